# Optimizing a Trainium2 kernel written in Bass

```python
import math
import jax, jax.numpy as jnp
from jax import lax
import numpy as np

D_MODEL = 1024
BATCH = 8
SEQ = 8192
DEPTH = 1

GRID_W = 64
N_Q_HEADS = 8
N_KV_HEADS = 2
HEAD_DIM = 128
Q_BLOCK = 128
ROPE_THETA = 10000.0
ROPE_AXIS_DIM = HEAD_DIM // 2
N_FOURIER_GROUPS = 4
FOURIER_GROUP_DIM = 128
FOURIER_DIM = N_FOURIER_GROUPS * FOURIER_GROUP_DIM
N_BRANCHES = 2
Q_DIM = N_Q_HEADS * HEAD_DIM
KV_DIM = N_KV_HEADS * HEAD_DIM
IN_COLS = FOURIER_DIM + Q_DIM + 2 * KV_DIM + N_BRANCHES * D_MODEL
N_EXPERTS = 32
TOP_K = 4
D_EXPERT = 1024
SWIGLU_LIMIT = 7.0
SWIGLU_ALPHA = 1.702
MOE_BLOCK = 128
N_MOD = 6
DEEPNORM_ALPHA = (2 * DEPTH) ** 0.25
DEEPNORM_BETA = (8 * DEPTH) ** -0.25
LN_EPS = 1e-5
ADA_EPS = 1e-6
QK_EPS = 1e-6

kernel_name = "hybrid_fourier_gqa_moe_deepnorm_block"


def layer_norm(x, g=None, b=None, eps=LN_EPS):
    xf = x.astype(jnp.float32)
    mu = jnp.mean(xf, axis=-1, keepdims=True)
    var = jnp.mean(jnp.square(xf - mu), axis=-1, keepdims=True)
    y = (xf - mu) * lax.rsqrt(var + eps)
    if g is not None:
        y = y * g.astype(jnp.float32) + b.astype(jnp.float32)
    return y.astype(x.dtype)


def rms_norm(x, g, eps=QK_EPS):
    xf = x.astype(jnp.float32)
    y = xf * lax.rsqrt(jnp.mean(jnp.square(xf), axis=-1, keepdims=True) + eps)
    return (y * g.astype(jnp.float32)).astype(x.dtype)


def axial_rope_angles(seq):
    rows = seq // GRID_W
    row_ids = jnp.repeat(jnp.arange(rows, dtype=jnp.float32), GRID_W)
    col_ids = jnp.tile(jnp.arange(GRID_W, dtype=jnp.float32), rows)
    freqs = ROPE_THETA ** (-jnp.arange(0, ROPE_AXIS_DIM, 2, dtype=jnp.float32) / ROPE_AXIS_DIM)
    return row_ids[:, None] * freqs, col_ids[:, None] * freqs


def _rotate_half(x, ang):
    cos = jnp.cos(ang)[:, None, :].astype(x.dtype)
    sin = jnp.sin(ang)[:, None, :].astype(x.dtype)
    x1, x2 = jnp.split(x, 2, axis=-1)
    return jnp.concatenate([x1 * cos - x2 * sin, x2 * cos + x1 * sin], axis=-1)


def apply_axial_rope(x, ang_r, ang_c):
    return jnp.concatenate([_rotate_half(x[..., :ROPE_AXIS_DIM], ang_r),
                            _rotate_half(x[..., ROPE_AXIS_DIM:], ang_c)], axis=-1)


def fourier_mix(f):
    b, s, _ = f.shape
    g = f.astype(jnp.float32).reshape(b, s, N_FOURIER_GROUPS, FOURIER_GROUP_DIM)
    y = jnp.fft.fftn(g, axes=(1, 3), norm="ortho").real
    return y.reshape(b, s, FOURIER_DIM).astype(f.dtype)


def blocked_gqa(q, k, v):
    b, s, _, _ = q.shape
    grp = N_Q_HEADS // N_KV_HEADS
    nblk = s // Q_BLOCK
    scale = 1.0 / math.sqrt(HEAD_DIM)
    qb = q.reshape(b, nblk, Q_BLOCK, N_KV_HEADS, grp, HEAD_DIM).transpose(1, 0, 2, 3, 4, 5)

    def one_block(q_blk):
        sc = jnp.einsum('bqhgd,bkhd->bhgqk', q_blk, k, preferred_element_type=jnp.float32) * scale
        p = jax.nn.softmax(sc, axis=-1).astype(v.dtype)
        return jnp.einsum('bhgqk,bkhd->bqhgd', p, v)

    o = lax.map(one_block, qb)
    return o.transpose(1, 0, 2, 3, 4, 5).reshape(b, s, Q_DIM)


def mixing_sublayer(u, w_in, q_norm_g, k_norm_g, w_fourier, w_attn_o, w_out, ang_r, ang_c):
    b, s, _ = u.shape
    z = u @ w_in
    cuts = np.cumsum([FOURIER_DIM, Q_DIM, KV_DIM, KV_DIM]).tolist()
    f, q, k, v, gates = jnp.split(z, cuts, axis=-1)
    y_f = fourier_mix(f) @ w_fourier
    q = apply_axial_rope(rms_norm(q.reshape(b, s, N_Q_HEADS, HEAD_DIM), q_norm_g), ang_r, ang_c)
    k = apply_axial_rope(rms_norm(k.reshape(b, s, N_KV_HEADS, HEAD_DIM), k_norm_g), ang_r, ang_c)
    v = v.reshape(b, s, N_KV_HEADS, HEAD_DIM)
    y_a = blocked_gqa(q, k, v) @ w_attn_o
    g_f, g_a = jnp.split(gates, N_BRANCHES, axis=-1)
    m = jax.nn.sigmoid(g_f) * y_f + jax.nn.sigmoid(g_a) * y_a
    return m @ w_out


def moe_sublayer(u, w_router, b_router, w_gate_up, b_gate_up, w_down, b_down):
    b, s, d = u.shape
    n = b * s
    xf = u.reshape(n, d)
    logits = (xf @ w_router + b_router).astype(jnp.float32)
    top_vals, top_idx = lax.top_k(logits, TOP_K)
    top_w = jax.nn.softmax(top_vals, axis=-1)

    n_slots = n * TOP_K
    e_flat = top_idx.reshape(n_slots).astype(jnp.int32)
    w_flat = top_w.reshape(n_slots)
    tok_flat = jnp.arange(n_slots, dtype=jnp.int32) // TOP_K

    order = jnp.argsort(e_flat)
    sorted_e = e_flat[order]
    counts = jnp.zeros((N_EXPERTS,), jnp.int32).at[e_flat].add(1)
    starts = jnp.cumsum(counts) - counts
    pcounts = ((counts + MOE_BLOCK - 1) // MOE_BLOCK) * MOE_BLOCK
    pends = jnp.cumsum(pcounts)
    pstarts = pends - pcounts
    dest = pstarts[sorted_e] + (jnp.arange(n_slots, dtype=jnp.int32) - starts[sorted_e])

    n_pad = n_slots + N_EXPERTS * MOE_BLOCK
    nblk = n_pad // MOE_BLOCK
    buf_tok = jnp.zeros((n_pad,), jnp.int32).at[dest].set(tok_flat[order])
    buf_w = jnp.zeros((n_pad,), jnp.float32).at[dest].set(w_flat[order])
    buf_valid = jnp.zeros((n_pad,), jnp.bool_).at[dest].set(True)
    block_starts = jnp.arange(nblk, dtype=jnp.int32) * MOE_BLOCK
    block_e = jnp.clip(jnp.searchsorted(pends, block_starts, side='right'), 0, N_EXPERTS - 1)

    def expert_block(args):
        e, rows = args
        xb = xf[rows]
        h = xb @ w_gate_up[e] + b_gate_up[e]
        gate, up = h[:, :D_EXPERT], h[:, D_EXPERT:]
        gate = jnp.minimum(gate, SWIGLU_LIMIT)
        up = jnp.clip(up, -SWIGLU_LIMIT, SWIGLU_LIMIT)
        act = (up + 1.0) * gate * jax.nn.sigmoid(SWIGLU_ALPHA * gate)
        return act @ w_down[e] + b_down[e]

    y = lax.map(expert_block, (block_e, buf_tok.reshape(nblk, MOE_BLOCK))).reshape(n_pad, d)
    contrib = jnp.where(buf_valid[:, None], y.astype(jnp.float32) * buf_w[:, None], 0.0)
    out = jnp.zeros((n, d), jnp.float32).at[buf_tok].add(contrib)
    return out.reshape(b, s, d).astype(u.dtype)


def setup_inputs(seed: int = 0) -> dict:
    key = jax.random.key(seed)
    ks = jax.random.split(key, 20)
    f32 = jnp.float32
    nrm = lambda k, shp: jax.random.normal(k, shp, f32)
    L, D, E, F = DEPTH, D_MODEL, N_EXPERTS, D_EXPERT
    return {
        "x": nrm(ks[0], (BATCH, SEQ, D)),
        "c": nrm(ks[1], (BATCH, D)),
        "w_ada": nrm(ks[2], (L, D, N_MOD * D)) * D ** -0.5,
        "b_ada": nrm(ks[3], (L, N_MOD * D)) * 0.02,
        "w_in": nrm(ks[4], (L, D, IN_COLS)) * D ** -0.5,
        "q_norm_g": 1.0 + 0.02 * nrm(ks[5], (L, HEAD_DIM)),
        "k_norm_g": 1.0 + 0.02 * nrm(ks[6], (L, HEAD_DIM)),
        "w_fourier": nrm(ks[7], (L, FOURIER_DIM, D)) * FOURIER_DIM ** -0.5,
        "w_attn_o": nrm(ks[8], (L, Q_DIM, D)) * Q_DIM ** -0.5,
        "w_out": nrm(ks[9], (L, D, D)) * (D ** -0.5 * DEEPNORM_BETA),
        "ln1_g": 1.0 + 0.02 * nrm(ks[10], (L, D)),
        "ln1_b": 0.02 * nrm(ks[11], (L, D)),
        "w_router": nrm(ks[12], (L, D, E)) * D ** -0.5,
        "b_router": nrm(ks[13], (L, E)) * 0.01,
        "w_gate_up": nrm(ks[14], (L, E, D, 2 * F)) * D ** -0.5,
        "b_gate_up": nrm(ks[15], (L, E, 2 * F)) * 0.02,
        "w_down": nrm(ks[16], (L, E, F, D)) * (F ** -0.5 * DEEPNORM_BETA),
        "b_down": nrm(ks[17], (L, E, D)) * 0.02,
        "ln2_g": 1.0 + 0.02 * nrm(ks[18], (L, D)),
        "ln2_b": 0.02 * nrm(ks[19], (L, D)),
    }


def reference(x, c, w_ada, b_ada, w_in, q_norm_g, k_norm_g, w_fourier, w_attn_o, w_out,
              ln1_g, ln1_b, w_router, b_router, w_gate_up, b_gate_up, w_down, b_down,
              ln2_g, ln2_b):
    seq = x.shape[1]
    ang_r, ang_c = axial_rope_angles(seq)
    cond = jax.nn.silu(c)
    for l in range(DEPTH):
        mod = cond @ w_ada[l] + b_ada[l]
        sh1, sc1, g1, sh2, sc2, g2 = [m[:, None, :] for m in jnp.split(mod, N_MOD, axis=-1)]
        u = layer_norm(x, eps=ADA_EPS) * (1.0 + sc1) + sh1
        h = mixing_sublayer(u, w_in[l], q_norm_g[l], k_norm_g[l], w_fourier[l],
                            w_attn_o[l], w_out[l], ang_r, ang_c)
        x = layer_norm(DEEPNORM_ALPHA * x + g1 * h, ln1_g[l], ln1_b[l])
        u = layer_norm(x, eps=ADA_EPS) * (1.0 + sc2) + sh2
        h = moe_sublayer(u, w_router[l], b_router[l], w_gate_up[l], b_gate_up[l],
                         w_down[l], b_down[l])
        x = layer_norm(DEEPNORM_ALPHA * x + g2 * h, ln2_g[l], ln2_b[l])
    return x
```

```python
import numpy as np
import ml_dtypes
from contextlib import ExitStack
import concourse.bass as bass
import concourse.mybir as mybir
from concourse.bass_utils import run_bass_kernel_spmd

F32 = mybir.dt.float32
BF16 = mybir.dt.bfloat16
I32 = mybir.dt.int32
AF = mybir.ActivationFunctionType
ALU = mybir.AluOpType
AX = mybir.AxisListType

S = 8192
D = 1024
NT = S // 128
NB = S // 512
NE = 32
BS = 512
NSUB = BS // 128
NBLK = (S * 4 + NE * (BS - 1)) // BS
NPAD = NBLK * BS
ALPHA = 2.0 ** 0.25


class Sem:
    def __init__(self, h, is_dma):
        self.h = h
        self.v = 0
        self.is_dma = is_dma


class Tok:
    __slots__ = ("sem", "val")

    def __init__(self, sem, val):
        self.sem = sem
        self.val = val


class Buf:
    def __init__(self, name=""):
        self.name = name
        self.w = None
        self.r = {}


class KB:
    def __init__(self, nc, es):
        self.nc = nc
        self.es = es
        self.engs = {"pe": nc.tensor, "act": nc.scalar, "dve": nc.vector, "pool": nc.gpsimd, "sp": nc.sync}
        self.sems = []
        self.esem = {}
        for e in ("pe", "act", "dve", "pool"):
            self.esem[e] = self.sem("e_" + e, is_dma=False)
        self.waited = {e: {} for e in self.engs}
        self.n_ins = 0

    def sem(self, name, is_dma=True):
        s = Sem(self.es.enter_context(self.nc.semaphore(name)), is_dma)
        self.sems.append(s)
        return s

    def wait(self, eng, tok):
        if tok is None:
            return
        val = tok.sem.v if tok.sem.is_dma else tok.val
        w = self.waited[eng]
        if w.get(id(tok.sem), 0) >= val:
            return
        self.engs[eng].wait_ge(tok.sem.h, val)
        w[id(tok.sem)] = val

    def _deps(self, eng, reads, writes):
        for b in reads:
            self.wait(eng, b.w)
        for b in writes:
            self.wait(eng, b.w)
            for t in b.r.values():
                self.wait(eng, t)

    def _done(self, tok, reads, writes):
        for b in reads:
            b.r[id(tok.sem)] = tok
        for b in writes:
            b.w = tok
            b.r = {}

    def op(self, eng, fn, reads=(), writes=(), dsem=None):
        self._deps(eng, reads, writes)
        ins = fn(self.engs[eng])
        self.n_ins += 1
        if dsem is not None:
            ins.then_inc(dsem.h, 16)
            dsem.v += 16
            tok = Tok(dsem, dsem.v)
        else:
            s = self.esem[eng]
            ins.then_inc(s.h, 1)
            s.v += 1
            tok = Tok(s, s.v)
        self._done(tok, reads, writes)
        return tok

    def group(self, eng, fns, reads=(), writes=()):
        self._deps(eng, reads, writes)
        ins = None
        for fn in fns:
            ins = fn(self.engs[eng])
            self.n_ins += 1
        s = self.esem[eng]
        ins.then_inc(s.h, 1)
        s.v += 1
        tok = Tok(s, s.v)
        self._done(tok, reads, writes)
        return tok

    def barrier(self):
        for e in self.engs:
            for s in self.sems:
                if s.v > 0:
                    self.wait(e, Tok(s, s.v))


def run_interleaved(gens, width=2):
    items = []
    for g in gens:
        items.append(g if isinstance(g, tuple) else (None, g, ()))
    done = set()
    active = []
    pos = 0
    while True:
        while len(active) < width and pos < len(items):
            name, g, deps = items[pos]
            if any(d not in done for d in deps):
                break
            active.append((name, g))
            pos += 1
        if not active:
            assert pos >= len(items), "interleave deadlock"
            break
        for ent in list(active):
            try:
                next(ent[1])
            except StopIteration:
                active.remove(ent)
                if ent[0] is not None:
                    done.add(ent[0])


def build(debug=None):
    debug = debug or {}
    stop_after = debug.get("stop_after", 99)
    nc = bass.Bass("TRN2", target_bir_lowering=False)
    I = {}

    def inp(name, shape, dt=F32):
        I[name] = nc.dram_tensor(name, shape, dt, kind="ExternalInput").ap()
        return I[name]

    inp("x", [S, D]); inp("c_fm", [128, 8]); inp("w_ada", [D, 6 * D]); inp("b_ada", [1, 6 * D])
    inp("w_in", [D, 4096]); inp("q_norm_g", [1, 128]); inp("k_norm_g", [1, 128])
    inp("w_fourier", [512, D]); inp("w_attn_o", [D, D]); inp("w_out", [D, D])
    inp("ln1_g", [1, D]); inp("ln1_b", [1, D]); inp("w_router", [D, NE]); inp("b_router", [1, NE])
    if stop_after >= 7:
        inp("w_gate_up", [NE, D, 2 * D]); inp("bgu_fm", [NE * 128, 16]); inp("w_down", [NE, D, D]); inp("b_down", [NE, D])
    inp("ln2_g", [1, D]); inp("ln2_b", [1, D])
    inp("rope_tab", [S, 256])
    inp("fft_e", [128, 64, 2, 128], BF16)
    inp("fft_w3", [128, 128], BF16)
    inp("dft_c", [128, 2, 128], BF16)
    out = nc.dram_tensor("out", [S, D], F32, kind="ExternalOutput").ap()

    dbg_out = {}

    def scratch(name, shape, dt):
        if name in debug.get("dump", ()):
            t = nc.dram_tensor(name, shape, dt, kind="ExternalOutput").ap()
            dbg_out[name] = t
            return t
        return nc.dram_tensor(name, shape, dt, kind="Internal").ap()

    f_dram = scratch("f_dram", [S, 512], BF16)
    qT_dram = scratch("qT_dram", [8, 128, S], BF16)
    kT_dram = scratch("kT_dram", [2, 128, S], BF16)
    v_dram = scratch("v_dram", [S, 256], BF16)
    gT_dram = scratch("gT_dram", [2048, S], BF16)
    Ys_dram = scratch("Ys_dram", [2, 64, 128, 512], BF16)
    XT_dram = scratch("XT_dram", [1024, S], BF16)
    OT_dram = scratch("OT_dram", [1024, S], BF16)
    x1_dram = scratch("x1_dram", [S, D], F32)
    u2_dram = scratch("u2_dram", [S, D], BF16)
    xs_dram = scratch("xs_dram", [NPAD, D], BF16)
    ys_dram = scratch("ys_dram", [NPAD, D], BF16)
    wgu_bf = scratch("wgu_bf", [NE * 128, 8 * 2 * D], BF16)
    wd_bf = scratch("wd_bf", [NE * 128, 8 * D], BF16)
    bgu_bf = scratch("bgu_bf", [NE, 2 * D], BF16)
    bd_bf = scratch("bd_bf", [NE, D], BF16)
    dbg6 = None
    if "dbg6" in debug.get("dump", ()):
        dbg6 = {"L": nc.dram_tensor("d6_L", [S, NE], F32, kind="ExternalOutput").ap(),
                "dest": nc.dram_tensor("d6_dest", [128, NT * 4], I32, kind="ExternalOutput").ap(),
                "blk": nc.dram_tensor("d6_blk", [1, NBLK], I32, kind="ExternalOutput").ap(),
                "chg": nc.dram_tensor("d6_chg", [1, NBLK], I32, kind="ExternalOutput").ap(),
                "w4": nc.dram_tensor("d6_w4", [128, NT * 4], F32, kind="ExternalOutput").ap()}
    mod_dump = scratch("mod_dump", [128, 6 * D], F32) if "mod_dump" in debug.get("dump", ()) else None

    with ExitStack() as top:
        kb = KB(nc, top)

        def T(es, name, shape, dt):
            return es.enter_context(nc.sbuf_tensor(name, shape, dt))

        def P(es, name, shape, dt):
            return es.enter_context(nc.psum_tensor(name, shape, dt))

        mod_bc = T(top, "mod_bc", [128, 6 * D], F32)
        b_mod = Buf("mod_bc")
        ident = T(top, "ident", [128, 128], BF16)
        b_ident = Buf("ident")
        kb.op("pool", lambda e: e.memset(ident[:], 0.0), writes=[b_ident])
        kb.op("pool", lambda e: e.affine_select(out=ident[:], in_=ident[:], pattern=[[-1, 128]], compare_op=ALU.not_equal,
                                                fill=1.0, base=0, channel_multiplier=1), writes=[b_ident])
        mhalf = T(top, "mhalf", [128, 16], F32)
        b_mhalf = Buf("mhalf")
        kb.op("pool", lambda e: e.memset(mhalf[:], -0.5), writes=[b_mhalf])

        with ExitStack() as es:
            c_sb = T(es, "c_sb", [128, 8], F32); cond = T(es, "cond", [128, 8], F32)
            cond_bc = T(es, "cond_bc", [128, 8, 128], F32)
            wblk = [T(es, f"wblk{i}", [128, 8, 512], F32) for i in range(2)]
            bada = T(es, "bada", [1, 6 * D], F32); ones1 = T(es, "ones1", [1, 128], F32)
            ps0 = [P(es, f"ps0_{i}", [128, 512], F32) for i in range(2)]
            b_c = Buf(); b_cond = Buf(); b_cbc = Buf(); b_wblk = [Buf(), Buf()]; b_bada = Buf(); b_ones1 = Buf(); b_ps0 = [Buf(), Buf()]
            s_c = kb.sem("p0c"); s_w = [kb.sem("p0w0"), kb.sem("p0w1")]
            kb.op("sp", lambda e: e.dma_start(out=c_sb[:], in_=I["c_fm"][:, :]), writes=[b_c], dsem=s_c)
            kb.op("sp", lambda e: e.dma_start(out=bada[:], in_=I["b_ada"][:, :]), writes=[b_bada], dsem=s_c)
            kb.op("pool", lambda e: e.memset(ones1[:], 1.0), writes=[b_ones1])
            kb.op("act", lambda e: e.activation(out=cond[:], in_=c_sb[:], func=AF.Silu), reads=[b_c], writes=[b_cond])
            kb.op("dve", lambda e: e.tensor_copy(out=cond_bc[:], in_=cond[:].unsqueeze(2).to_broadcast([128, 8, 128])),
                  reads=[b_cond], writes=[b_cbc])
            w_ada_v = I["w_ada"].rearrange("(dc p) c -> p dc c", p=128)
            for cb in range(12):
                sl = cb % 2
                kb.op("sp", lambda e: e.dma_start(out=wblk[sl][:], in_=w_ada_v[:, :, cb * 512:(cb + 1) * 512]),
                      writes=[b_wblk[sl]], dsem=s_w[sl])
                fns = []
                for dc in range(8):
                    fns.append(lambda e, dc=dc: e.matmul(ps0[sl][:], lhsT=cond_bc[:, dc, :], rhs=wblk[sl][:, dc, :],
                                                         start=(dc == 0), stop=False))
                fns.append(lambda e: e.matmul(ps0[sl][:], lhsT=ones1[0:1, :], rhs=bada[0:1, cb * 512:(cb + 1) * 512],
                                              start=False, stop=True))
                kb.group("pe", fns, reads=[b_cbc, b_wblk[sl], b_ones1, b_bada], writes=[b_ps0[sl]])
                addv = 1.0 if (cb // 2) in (1, 4) else 0.0
                kb.op("dve", lambda e: e.tensor_scalar(out=mod_bc[:, cb * 512:(cb + 1) * 512], in0=ps0[sl][:], scalar1=addv,
                                                       scalar2=None, op0=ALU.add), reads=[b_ps0[sl]], writes=[b_mod])
            if mod_dump is not None:
                s_d = kb.sem("p0d")
                kb.op("sp", lambda e: e.dma_start(out=mod_dump[:, :], in_=mod_bc[:]), reads=[b_mod], dsem=s_d)
            kb.barrier()
        SH1, SC1, G1, SH2, SC2, G2 = [mod_bc[:, i * D:(i + 1) * D] for i in range(6)]

        def ln_tile_g(src, b_src, dst, b_dst, st_, b_st_, mv_, b_mv_, rstd_, b_rstd_, eps, mul_ap, add_ap, b_aff):
            kb.op("dve", lambda e: e.bn_stats(out=st_[:, 0, :], in_=src[:, 0:512]), reads=[b_src], writes=[b_st_]); yield
            kb.op("dve", lambda e: e.bn_stats(out=st_[:, 1, :], in_=src[:, 512:1024]), reads=[b_src], writes=[b_st_]); yield
            kb.op("dve", lambda e: e.bn_aggr(out=mv_[:], in_=st_[:].rearrange("p a b -> p (a b)")), reads=[b_st_], writes=[b_mv_]); yield
            kb.op("pool", lambda e: e.tensor_scalar(out=rstd_[:], in0=mv_[:, 1:2], scalar1=1.0, scalar2=float(eps), op0=ALU.mult, op1=ALU.add),
                  reads=[b_mv_], writes=[b_rstd_]); yield
            kb.op("pool", lambda e: e.tensor_tensor(out=rstd_[:], in0=rstd_[:], in1=mhalf[:, 0:1], op=ALU.pow), reads=[b_rstd_, b_mhalf], writes=[b_rstd_]); yield
            kb.op("dve", lambda e: e.tensor_scalar(out=src[:], in0=src[:], scalar1=mv_[:, 0:1], scalar2=rstd_[:, 0:1], op0=ALU.subtract, op1=ALU.mult),
                  reads=[b_src, b_mv_, b_rstd_], writes=[b_src]); yield
            kb.op("dve", lambda e: e.tensor_tensor(out=src[:], in0=src[:], in1=mul_ap, op=ALU.mult), reads=[b_src, b_aff], writes=[b_src]); yield
            kb.op("dve", lambda e: e.tensor_tensor(out=dst[:], in0=src[:], in1=add_ap, op=ALU.add), reads=[b_src, b_aff], writes=[b_dst]); yield

        def ln_tile(src, b_src, dst, b_dst, st_, b_st_, mv_, b_mv_, rstd_, b_rstd_, eps, mul_ap, add_ap, b_aff, mul_first_pool):
            kb.op("dve", lambda e: e.bn_stats(out=st_[:, 0, :], in_=src[:, 0:512]), reads=[b_src], writes=[b_st_])
            kb.op("dve", lambda e: e.bn_stats(out=st_[:, 1, :], in_=src[:, 512:1024]), reads=[b_src], writes=[b_st_])
            kb.op("dve", lambda e: e.bn_aggr(out=mv_[:], in_=st_[:].rearrange("p a b -> p (a b)")), reads=[b_st_], writes=[b_mv_])
            kb.op("pool", lambda e: e.tensor_scalar(out=rstd_[:], in0=mv_[:, 1:2], scalar1=1.0, scalar2=float(eps), op0=ALU.mult, op1=ALU.add),
                  reads=[b_mv_], writes=[b_rstd_])
            kb.op("pool", lambda e: e.tensor_tensor(out=rstd_[:], in0=rstd_[:], in1=mhalf[:, 0:1], op=ALU.pow), reads=[b_rstd_, b_mhalf], writes=[b_rstd_])
            kb.op("dve", lambda e: e.tensor_scalar(out=src[:], in0=src[:], scalar1=mv_[:, 0:1], scalar2=rstd_[:, 0:1], op0=ALU.subtract, op1=ALU.mult),
                  reads=[b_src, b_mv_, b_rstd_], writes=[b_src])
            kb.op("dve", lambda e: e.tensor_tensor(out=src[:], in0=src[:], in1=mul_ap, op=ALU.mult), reads=[b_src, b_aff], writes=[b_src])
            kb.op("dve", lambda e: e.tensor_tensor(out=dst[:], in0=src[:], in1=add_ap, op=ALU.add), reads=[b_src, b_aff], writes=[b_dst])

        if stop_after >= 1:
          with ExitStack() as es:
            win = T(es, "win", [128, 8, 4096], BF16); b_win = Buf()
            xt = [T(es, f"xt{i}", [128, D], F32) for i in range(2)]; b_xt = [Buf(), Buf()]
            tab = [T(es, f"tab{i}", [128, 256], F32) for i in range(2)]; b_tab = [Buf(), Buf()]
            st_ = [T(es, f"st{i}", [128, 2, 6], F32) for i in range(2)]; mv_ = [T(es, f"mv{i}", [128, 2], F32) for i in range(2)]; rstd_ = [T(es, f"rstd{i}", [128, 1], F32) for i in range(2)]
            b_st_ = [Buf(), Buf()]; b_mv_ = [Buf(), Buf()]; b_rstd_ = [Buf(), Buf()]
            xn_ = [T(es, f"xn{i}", [128, D], F32) for i in range(2)]; b_xn_ = [Buf(), Buf()]
            ub = [T(es, f"ub{i}", [128, D], BF16) for i in range(2)]; b_ub = [Buf(), Buf()]
            uT = [T(es, f"uT{i}", [128, 8, 512], BF16) for i in range(2)]; b_uT = [Buf(), Buf()]
            qk_ = [T(es, f"qk{i}", [128, 1280], F32) for i in range(2)]; b_qk_ = [Buf(), Buf()]

            ss_ = [T(es, f"ss{i}", [128, 10], F32) for i in range(2)]; b_ss_ = [Buf(), Buf()]
            rs_ = [T(es, f"rs{i}", [128, 10], F32) for i in range(2)]; b_rs_ = [Buf(), Buf()]
            qn_ = [T(es, f"qn{i}", [128, 1280], F32) for i in range(2)]; b_qn_ = [Buf(), Buf()]
            t1_ = [T(es, f"t1{i}", [128, 1280], F32) for i in range(2)]; b_t1_ = [Buf(), Buf()]
            t2_ = [T(es, f"t2{i}", [128, 1280], F32) for i in range(2)]; b_t2_ = [Buf(), Buf()]
            rot_ = [T(es, f"rot{i}", [128, 1280], BF16) for i in range(2)]; b_rot_ = [Buf(), Buf()]
            gain = T(es, "gain", [128, 1280], F32); b_gain = Buf()
            fst = [T(es, f"fst{i}", [128, 512], BF16) for i in range(2)]; b_fst = [Buf(), Buf()]
            vst = [T(es, f"vst{i}", [128, 256], BF16) for i in range(2)]; b_vst = [Buf(), Buf()]
            qTs_ = [T(es, f"qTs{i}", [128, 8, 512], BF16) for i in range(2)]; b_qTs_ = [Buf(), Buf()]
            kTs_ = [T(es, f"kTs{i}", [128, 2, 512], BF16) for i in range(2)]; b_kTs_ = [Buf(), Buf()]
            gst = [T(es, f"gst{i}", [128, 4, 512], BF16) for i in range(2)]; b_gst = [Buf(), Buf()]
            pst = P(es, "pst", [128, 8, 128], BF16); b_pst = Buf()
            psz = P(es, "psz", [128, 2048], F32); b_psz = [Buf() for _ in range(4)]
            pstq = P(es, "pstq", [128, 8, 128], BF16); b_pstq = Buf()
            pstk = P(es, "pstk", [128, 2, 128], BF16); b_pstk = Buf()
            psg = P(es, "psg", [128, 512], F32); b_psg = Buf()
            s_win = kb.sem("p1win"); s_x = [kb.sem("p1x0"), kb.sem("p1x1")]; s_g = kb.sem("p1g")
            s_f = [kb.sem("p1f0"), kb.sem("p1f1")]; s_v = [kb.sem("p1v0"), kb.sem("p1v1")]
            s_q = kb.sem("p1q"); s_k = kb.sem("p1k"); s_gs = [kb.sem("p1gs0"), kb.sem("p1gs1")]
            w_in_v = I["w_in"].rearrange("(dc p) c -> p dc c", p=128)
            for dc in range(8):
                for hh in range(2):
                    kb.op("pool", lambda e, dc=dc, hh=hh: e.dma_start(out=win[:, dc, hh * 2048:(hh + 1) * 2048],
                                                                      in_=w_in_v[:, dc, hh * 2048:(hh + 1) * 2048]),
                          writes=[b_win], dsem=s_win)
            for h in range(10):
                src = I["q_norm_g"] if h < 8 else I["k_norm_g"]
                kb.op("sp", lambda e, h=h, src=src: e.dma_start(out=gain[:, h * 128:(h + 1) * 128],
                                                               in_=src[0:1, :].partition_broadcast(128)),
                      writes=[b_gain], dsem=s_g)
            kb.op("dve", lambda e: e.tensor_scalar(out=gain[:, 0:1024], in0=gain[:, 0:1024], scalar1=float(128.0 ** -0.5),
                                                   scalar2=None, op0=ALU.mult), reads=[b_gain], writes=[b_gain])
            x_v = I["x"].rearrange("(t p) d -> t p d", p=128)
            tab_v = I["rope_tab"].rearrange("(t p) d -> t p d", p=128)
            qT_v = qT_dram.rearrange("h d s -> d h s")
            kT_v = kT_dram.rearrange("h d s -> d h s")
            gT_v = gT_dram.rearrange("(g p) s -> p g s", p=128)

            s_tb = [kb.sem("p1t0"), kb.sem("p1t1")]

            def load_xt(t):
                sl = t % 2
                kb.op("sp", lambda e: e.dma_start(out=xt[sl][:], in_=x_v[t]), writes=[b_xt[sl]], dsem=s_x[sl])

            def load_tab(t):
                sl = t % 2
                kb.op("sp", lambda e: e.dma_start(out=tab[sl][:], in_=tab_v[t]), writes=[b_tab[sl]], dsem=s_tb[sl])

            load_xt(0); load_xt(1); load_tab(0); load_tab(1)
            def tile1(t):
                    sl = t % 2; blk = t // 4; sl4 = t % 4; ub_ = ub[sl]; uT_ = uT[blk % 2]
                    st = st_[sl]; mv = mv_[sl]; rstd = rstd_[sl]; b_st = b_st_[sl]; b_mv = b_mv_[sl]; b_rstd = b_rstd_[sl]
                    xn = xn_[sl]; b_xn = b_xn_[sl]; qk = qk_[sl]; b_qk = b_qk_[sl]; ss = ss_[sl]; b_ss = b_ss_[sl]; rs = rs_[sl]; b_rs = b_rs_[sl]
                    qn = qn_[sl]; b_qn = b_qn_[sl]; t1 = t1_[sl]; b_t1 = b_t1_[sl]; t2 = t2_[sl]; b_t2 = b_t2_[sl]; rot = rot_[sl]; b_rot = b_rot_[sl]
                    sq = t2; b_sq = b_t2
                    qTs = qTs_[blk % 2]; b_qTs = b_qTs_[blk % 2]; kTs = kTs_[blk % 2]; b_kTs = b_kTs_[blk % 2]
                    x_ = xt[sl]
                    kb.op("dve", lambda e: e.bn_stats(out=st[:, 0, :], in_=x_[:, 0:512]), reads=[b_xt[sl]], writes=[b_st])
                    yield
                    kb.op("dve", lambda e: e.bn_stats(out=st[:, 1, :], in_=x_[:, 512:1024]), reads=[b_xt[sl]], writes=[b_st])
                    yield
                    kb.op("dve", lambda e: e.bn_aggr(out=mv[:], in_=st[:].rearrange("p a b -> p (a b)")), reads=[b_st], writes=[b_mv])
                    yield
                    kb.op("pool", lambda e: e.tensor_scalar(out=rstd[:], in0=mv[:, 1:2], scalar1=1.0, scalar2=1e-6, op0=ALU.mult, op1=ALU.add),
                          reads=[b_mv], writes=[b_rstd])
                    yield
                    kb.op("pool", lambda e: e.tensor_tensor(out=rstd[:], in0=rstd[:], in1=mhalf[:, 0:1], op=ALU.pow),
                          reads=[b_rstd, b_mhalf], writes=[b_rstd])
                    yield
                    kb.op("dve", lambda e: e.tensor_scalar(out=xn[:], in0=x_[:], scalar1=mv[:, 0:1], scalar2=rstd[:, 0:1],
                                                           op0=ALU.subtract, op1=ALU.mult), reads=[b_xt[sl], b_mv, b_rstd], writes=[b_xn])
                    if t + 2 < NT:
                        load_xt(t + 2)
                    yield
                    kb.op("dve", lambda e: e.tensor_tensor(out=xn[:], in0=xn[:], in1=SC1, op=ALU.mult), reads=[b_xn, b_mod], writes=[b_xn])
                    yield
                    kb.op("dve", lambda e: e.tensor_tensor(out=ub_[:], in0=xn[:], in1=SH1, op=ALU.add), reads=[b_xn, b_mod], writes=[b_ub[sl]])
                    yield
                    kb.group("pe", [lambda e, dc=dc: e.transpose(out=pst[:, dc, :], in_=ub_[:, dc * 128:(dc + 1) * 128], identity=ident[:])
                                    for dc in range(8)], reads=[b_ub[sl], b_ident], writes=[b_pst])
                    kb.op("act", lambda e: e.copy(out=uT_[:, :, sl4 * 128:(sl4 + 1) * 128], in_=pst[:]), reads=[b_pst], writes=[b_uT[blk % 2]])
                    yield
                    for cc in range(4):
                        kb.group("pe", [lambda e, dc=dc, cc=cc: e.matmul(psz[:, cc * 512:(cc + 1) * 512], lhsT=uT_[:, dc, sl4 * 128:(sl4 + 1) * 128],
                                                                         rhs=win[:, dc, cc * 512:(cc + 1) * 512], start=(dc == 0), stop=(dc == 7))
                                        for dc in range(8)], reads=[b_uT[blk % 2], b_win], writes=[b_psz[cc]])
                    kb.op("act", lambda e: e.copy(out=fst[sl][:], in_=psz[:, 0:512]), reads=[b_psz[0]], writes=[b_fst[sl]])
                    kb.op("sp", lambda e: e.dma_start(out=f_dram[t * 128:(t + 1) * 128, :], in_=fst[sl][:]), reads=[b_fst[sl]], dsem=s_f[sl])
                    kb.op("act", lambda e: e.copy(out=vst[sl][:], in_=psz[:, 1792:2048]), reads=[b_psz[3]], writes=[b_vst[sl]])
                    kb.op("sp", lambda e: e.dma_start(out=v_dram[t * 128:(t + 1) * 128, :], in_=vst[sl][:]), reads=[b_vst[sl]], dsem=s_v[sl])
                    kb.op("act", lambda e: e.copy(out=qk[:], in_=psz[:, 512:1792]), reads=[b_psz[1], b_psz[2], b_psz[3]], writes=[b_qk])
                    yield
                    kb.op("dve", lambda e: e.tensor_tensor(out=sq[:], in0=qk[:], in1=qk[:], op=ALU.mult), reads=[b_qk], writes=[b_sq])
                    yield
                    kb.op("dve", lambda e: e.tensor_reduce(out=ss[:], in_=sq[:].rearrange("p (h d) -> p h d", d=128), axis=AX.X, op=ALU.add),
                          reads=[b_sq], writes=[b_ss])
                    yield
                    kb.op("pool", lambda e: e.tensor_scalar(out=rs[:], in0=ss[:], scalar1=1.0 / 128.0, scalar2=1e-6, op0=ALU.mult, op1=ALU.add),
                          reads=[b_ss], writes=[b_rs])
                    yield
                    kb.op("pool", lambda e: e.tensor_tensor(out=rs[:], in0=rs[:], in1=mhalf[:, 0:10], op=ALU.pow), reads=[b_rs, b_mhalf], writes=[b_rs])
                    yield
                    kb.op("dve", lambda e: e.tensor_tensor(out=qn[:].rearrange("p (h d) -> p h d", d=128), in0=qk[:].rearrange("p (h d) -> p h d", d=128),
                                                           in1=rs[:].unsqueeze(2).to_broadcast([128, 10, 128]), op=ALU.mult),
                          reads=[b_qk, b_rs], writes=[b_qn])
                    yield
                    kb.op("dve", lambda e: e.tensor_tensor(out=qn[:], in0=qn[:], in1=gain[:], op=ALU.mult), reads=[b_qn, b_gain], writes=[b_qn])
                    yield
                    tb_ = tab[sl]
                    kb.op("dve", lambda e: e.tensor_tensor(out=t1[:].rearrange("p (h d) -> p h d", d=128), in0=qn[:].rearrange("p (h d) -> p h d", d=128),
                                                           in1=tb_[:, 0:128].unsqueeze(1).to_broadcast([128, 10, 128]), op=ALU.mult),
                          reads=[b_qn, b_tab[sl]], writes=[b_t1])
                    yield
                    qn5 = qn[:].rearrange("p (h a t d) -> p h a t d", h=10, a=2, t=2, d=32)
                    t25 = t2[:].rearrange("p (h a t d) -> p h a t d", h=10, a=2, t=2, d=32)
                    sn4 = tb_[:, 128:256].rearrange("p (a t d) -> p a t d", a=2, t=2, d=32)
                    for half in range(2):
                        kb.op("pool", lambda e, half=half: e.tensor_tensor(out=t25[:, :, :, half, :], in0=qn5[:, :, :, 1 - half, :],
                                                                           in1=sn4[:, :, half, :].unsqueeze(1).to_broadcast([128, 10, 2, 32]), op=ALU.mult),
                              reads=[b_qn, b_tab[sl]], writes=[b_t2])
                        yield
                    if t + 2 < NT:
                        load_tab(t + 2)
                    kb.op("dve", lambda e: e.tensor_tensor(out=rot[:], in0=t1[:], in1=t2[:], op=ALU.add), reads=[b_t1, b_t2], writes=[b_rot])
                    yield
                    kb.group("pe", [lambda e, h=h: e.transpose(out=pstq[:, h, :], in_=rot[:, h * 128:(h + 1) * 128], identity=ident[:]) for h in range(8)],
                             reads=[b_rot, b_ident], writes=[b_pstq])
                    kb.group("pe", [lambda e, h=h: e.transpose(out=pstk[:, h, :], in_=rot[:, (8 + h) * 128:(9 + h) * 128], identity=ident[:]) for h in range(2)],
                             reads=[b_rot, b_ident], writes=[b_pstk])
                    kb.op("act", lambda e: e.copy(out=qTs[:, :, sl4 * 128:(sl4 + 1) * 128], in_=pstq[:]), reads=[b_pstq], writes=[b_qTs])
                    kb.op("act", lambda e: e.copy(out=kTs[:, :, sl4 * 128:(sl4 + 1) * 128], in_=pstk[:]), reads=[b_pstk], writes=[b_kTs])
                    yield
                    if sl4 == 3:
                        kb.op("sp", lambda e: e.dma_start(out=qT_v[:, :, blk * 512:(blk + 1) * 512], in_=qTs[:]), reads=[b_qTs], dsem=s_q)
                        yield
                        kb.op("sp", lambda e: e.dma_start(out=kT_v[:, :, blk * 512:(blk + 1) * 512], in_=kTs[:]), reads=[b_kTs], dsem=s_k)
                        yield
                        for gq in range(4):
                            gsl = gq % 2
                            for gi in range(4):
                                gc = gq * 4 + gi
                                kb.group("pe", [lambda e, dc=dc, gc=gc: e.matmul(psg[:], lhsT=win[:, dc, 2048 + gc * 128:2048 + (gc + 1) * 128],
                                                                                 rhs=uT_[:, dc, :], start=(dc == 0), stop=(dc == 7)) for dc in range(8)],
                                         reads=[b_uT[blk % 2], b_win], writes=[b_psg])
                                kb.op("act", lambda e, gi=gi: e.activation(out=gst[gsl][:, gi, :], in_=psg[:], func=AF.Sigmoid),
                                      reads=[b_psg], writes=[b_gst[gsl]])
                                yield
                            kb.op("sp", lambda e, gq=gq: e.dma_start(out=gT_v[:, gq * 4:(gq + 1) * 4, blk * 512:(blk + 1) * 512], in_=gst[gsl][:]),
                                  reads=[b_gst[gsl]], dsem=s_gs[gsl])
                            yield
            run_interleaved((tile1(t) for t in range(NT)), debug.get("il1", 2))
            kb.barrier()


        if stop_after >= 2:
          with ExitStack() as es:
            E = T(es, "fftE", [128, 64, 2, 128], BF16); b_E = Buf()
            f1 = [T(es, f"f1_{i}", [128, 8, 512], BF16) for i in range(2)]; b_f1 = [Buf(), Buf()]
            Ysb = [T(es, f"Ysb{i}", [128, 8, 2, 512], BF16) for i in range(2)]; b_Ysb = [Buf(), Buf()]
            psY = [P(es, f"psY{i}", [128, 512], F32) for i in range(4)]; b_psY = [Buf() for _ in range(4)]
            s_E = kb.sem("p2E"); s_f1 = [kb.sem("p2f0"), kb.sem("p2f1")]; s_y = [kb.sem("p2y0"), kb.sem("p2y1")]
            kb.op("sp", lambda e: e.dma_start(out=E[:], in_=I["fft_e"][:, :, :, :]), writes=[b_E], dsem=s_E)
            f1_v = f_dram.rearrange("(p n) c -> p n c", n=64)
            cnt = 0
            for ch in range(8):
                sl = ch % 2
                kb.op("sp", lambda e: e.dma_start(out=f1[sl][:], in_=f1_v[:, ch * 8:(ch + 1) * 8, :]), writes=[b_f1[sl]], dsem=s_f1[sl])
                for j in range(8):
                    n2 = ch * 8 + j
                    for ri in range(2):
                        pb = cnt % 4; cnt += 1
                        kb.op("pe", lambda e: e.matmul(psY[pb][:], lhsT=E[:, n2, ri, :], rhs=f1[sl][:, j, :], start=True, stop=True),
                              reads=[b_E, b_f1[sl]], writes=[b_psY[pb]])
                        if ri == 0:
                            kb.op("act", lambda e: e.copy(out=Ysb[sl][:, j, ri, :], in_=psY[pb][:]), reads=[b_psY[pb]], writes=[b_Ysb[sl]])
                        else:
                            kb.op("dve", lambda e: e.tensor_copy(out=Ysb[sl][:, j, ri, :], in_=psY[pb][:]), reads=[b_psY[pb]], writes=[b_Ysb[sl]])
                for ri in range(2):
                    kb.op("sp", lambda e, ri=ri: e.dma_start(out=Ys_dram[ri, ch * 8:(ch + 1) * 8].rearrange("n k c -> k n c"), in_=Ysb[sl][:, :, ri, :]),
                          reads=[b_Ysb[sl]], dsem=s_y[sl])
            kb.barrier()
          with ExitStack() as es:
            W3 = T(es, "W3", [128, 128], BF16); b_W3 = Buf()
            Y2 = [T(es, f"Y2_{i}", [128, 8, 256], BF16) for i in range(2)]; b_Y2 = [Buf(), Buf()]
            XT = T(es, "XT", [128, 2, 2, S], BF16); b_XT = Buf()
            psX = [P(es, f"psX{i}", [128, 4, 128], F32) for i in range(2)]; b_psX = [Buf(), Buf()]
            s_w3 = kb.sem("p2w3"); s_y2 = [kb.sem("p2y20"), kb.sem("p2y21")]; s_xo = kb.sem("p2xo")
            kb.op("sp", lambda e: e.dma_start(out=W3[:], in_=I["fft_w3"][:, :]), writes=[b_W3], dsem=s_w3)
            Y2_v = Ys_dram.rearrange("r n k c -> (r n) k c")
            cnt = 0
            for pp in range(2):
                for kc in range(16):
                    sl = kc % 2
                    kb.op("sp", lambda e: e.dma_start(out=Y2[sl][:], in_=Y2_v[:, kc * 8:(kc + 1) * 8, pp * 256:(pp + 1) * 256]),
                          writes=[b_Y2[sl]], dsem=s_y2[sl])
                    for ccl in range(2):
                        for g in range(2):
                            pb = cnt % 2; cnt += 1
                            kb.group("pe", [lambda e, j=j: e.matmul(psX[pb][:, j, :], lhsT=Y2[sl][:, g * 4 + j, ccl * 128:(ccl + 1) * 128], rhs=W3[:],
                                                                    start=True, stop=True) for j in range(4)],
                                     reads=[b_Y2[sl], b_W3], writes=[b_psX[pb]])
                            k10 = kc * 8 + g * 4
                            o_ap = XT[:, ccl, :, :].rearrange("c x (k2 k1) -> c x k2 k1", k1=128)[:, :, :, k10:k10 + 4]
                            i_ap = psX[pb][:].rearrange("c j (x k2) -> c x k2 j", x=2)
                            if cnt % 2 == 0:
                                kb.op("act", lambda e: e.copy(out=o_ap, in_=i_ap), reads=[b_psX[pb]], writes=[b_XT])
                            else:
                                kb.op("dve", lambda e: e.tensor_copy(out=o_ap, in_=i_ap), reads=[b_psX[pb]], writes=[b_XT])
                for ccl in range(2):
                    for xr in range(2):
                        r0 = xr * 512 + (pp * 2 + ccl) * 128
                        kb.op("sp", lambda e, ccl=ccl, xr=xr, r0=r0: e.dma_start(out=XT_dram[r0:r0 + 128, :], in_=XT[:, ccl, xr, :]),
                              reads=[b_XT], dsem=s_xo)
            kb.barrier()

        if stop_after >= 4:
          with ExitStack() as es:
            kT = T(es, "kT", [128, 2, S], BF16); b_kT = Buf()
            V = T(es, "V", [128, NT, 256], BF16); b_V = Buf()
            ones = T(es, "ones", [128, 128], BF16); b_ones = Buf()
            gqk = T(es, "gqk", [128, 2, 128], F32); b_gqk = Buf()
            gmx = T(es, "gmx", [128, 2], F32); b_gmx = Buf()
            nbias = T(es, "nbias", [128, 1], F32); b_nb = Buf()
            qb = [T(es, f"qb{i}", [128, 8, 512], BF16) for i in range(2)]; b_qb = [Buf(), Buf()]
            PT = [T(es, f"PT{i}", [128, 1024], BF16) for i in range(4)]; b_PT = [Buf() for _ in range(4)]
            acc1 = T(es, "acc1", [128, 512], F32); b_acc1 = Buf()
            Osb = T(es, "Osb", [128, 512], F32); b_Osb = Buf()
            ones32 = T(es, "ones32", [128, 32], BF16); b_ones32 = Buf()
            kb.op("pool", lambda e: e.memset(ones32[:], 1.0), writes=[b_ones32])
            onesF = T(es, "onesF", [128, 128], F32); b_onesF = Buf()
            rec = T(es, "rec", [128, 512], F32); b_rec = Buf()
            OTs = [T(es, f"OTs{i}", [128, 512], BF16) for i in range(2)]; b_OTs = [Buf(), Buf()]
            psS = [P(es, f"psS{i}", [128, 1024], F32) for i in range(3)]; b_psS = [Buf() for _ in range(3)]
            psO = [P(es, "psO0", [128, 512], F32)]; b_psO = [Buf()]
            psL = [P(es, "psL0", [128, 512], F32)]; b_psL = [Buf()]
            s_kv = kb.sem("p4kv"); s_qb = [kb.sem("p4q0"), kb.sem("p4q1")]; s_o = [kb.sem("p4o0"), kb.sem("p4o1")]; s_gq = kb.sem("p4g")
            kb.op("sp", lambda e: e.dma_start(out=kT[:], in_=kT_dram.rearrange("h d s -> d h s")), writes=[b_kT], dsem=s_kv)
            kb.op("sp", lambda e: e.dma_start(out=V[:], in_=v_dram.rearrange("(t p) c -> p t c", p=128)), writes=[b_V], dsem=s_kv)
            kb.op("pool", lambda e: e.memset(onesF[:], 1.0 / 32.0), writes=[b_onesF])
            kb.op("sp", lambda e: e.dma_start(out=gqk[:, 0, :], in_=I["q_norm_g"][0:1, :].partition_broadcast(128)), writes=[b_gqk], dsem=s_gq)
            kb.op("sp", lambda e: e.dma_start(out=gqk[:, 1, :], in_=I["k_norm_g"][0:1, :].partition_broadcast(128)), writes=[b_gqk], dsem=s_gq)
            gmn = T(es, "gmn", [128, 2], F32); b_gmn = Buf()
            kb.op("dve", lambda e: e.tensor_reduce(out=gmx[:], in_=gqk[:], axis=AX.X, op=ALU.max), reads=[b_gqk], writes=[b_gmx])
            kb.op("dve", lambda e: e.tensor_reduce(out=gmn[:], in_=gqk[:], axis=AX.X, op=ALU.min, negate=True), reads=[b_gqk], writes=[b_gmn])
            kb.op("dve", lambda e: e.tensor_tensor(out=gmx[:], in0=gmx[:], in1=gmn[:], op=ALU.max), reads=[b_gmx, b_gmn], writes=[b_gmx])
            kb.op("dve", lambda e: e.tensor_tensor(out=nbias[:], in0=gmx[:, 0:1], in1=gmx[:, 1:2], op=ALU.mult), reads=[b_gmx], writes=[b_nb])
            kb.op("dve", lambda e: e.tensor_scalar(out=nbias[:], in0=nbias[:], scalar1=float(-(128.0 ** 0.5)), scalar2=None, op0=ALU.mult),
                  reads=[b_nb], writes=[b_nb])
            qT_v2 = qT_dram.rearrange("h d s -> d h s")
            kb.op("sp", lambda e: e.dma_start(out=qb[0][:], in_=qT_v2[:, :, 0:512]), writes=[b_qb[0]], dsem=s_qb[0])
            pc_jobs = []
            if stop_after >= 7:
                stg = [T(es, f"stg{i}", [128, 4096], F32) for i in range(2)]; b_stg = [Buf(), Buf()]
                stb = [T(es, f"stb{i}", [128, 4096], BF16) for i in range(2)]; b_stb = [Buf(), Buf()]
                s_ci = [kb.sem("p3i0"), kb.sem("p3i1")]; s_co = [kb.sem("p3o0"), kb.sem("p3o1")]
                for ex in range(NE):
                    gsrc = I["w_gate_up"][ex].rearrange("(r p) f -> p r f", p=128)
                    dsrc = I["w_down"][ex].rearrange("(r p) f -> p r f", p=128)
                    for q in range(4):
                        pc_jobs.append((gsrc[:, q * 2:(q + 1) * 2, :], wgu_bf[ex * 128:(ex + 1) * 128, q * 4096:(q + 1) * 4096], 2, 128, 4096))
                    for q in range(2):
                        pc_jobs.append((dsrc[:, q * 4:(q + 1) * 4, :], wd_bf[ex * 128:(ex + 1) * 128, q * 4096:(q + 1) * 4096], 4, 128, 4096))
                pc_jobs.append((I["b_down"][:, :], bd_bf[:, :], 1, NE, D))
            pc_state = [0]

            def precast_step(n):
                for _ in range(n):
                    ci = pc_state[0]
                    if ci >= len(pc_jobs):
                        return
                    src_ap, dst_ap, nr, npart, w_ = pc_jobs[ci]
                    sl = ci % 2
                    o_ap = stg[sl][0:npart, 0:w_]
                    if nr > 1:
                        o_ap = o_ap.rearrange("p (r f) -> p r f", r=nr)
                    kb.op("sp", lambda e: e.dma_start(out=o_ap, in_=src_ap), writes=[b_stg[sl]], dsem=s_ci[sl])
                    ce = ("dve", "pool")[ci % 2]
                    kb.op(ce, lambda e: e.tensor_copy(out=stb[sl][0:npart, 0:w_], in_=stg[sl][0:npart, 0:w_]), reads=[b_stg[sl]], writes=[b_stb[sl]])
                    kb.op("sp", lambda e: e.dma_start(out=dst_ap, in_=stb[sl][0:npart, 0:w_]), reads=[b_stb[sl]], dsem=s_co[sl])
                    pc_state[0] += 1
            i_u = 0
            nblk4 = debug.get("p4_blocks", NB)
            NU = NT // 2
            for blk in range(nblk4):
                bs = blk % 2
                if blk + 1 < nblk4:
                    kb.op("sp", lambda e: e.dma_start(out=qb[1 - bs][:], in_=qT_v2[:, :, (blk + 1) * 512:(blk + 2) * 512]),
                          writes=[b_qb[1 - bs]], dsem=s_qb[1 - bs])
                for h in range(8):
                    kvh = h // 4
                    base = i_u

                    def emit_S(u):
                        ii = base + u
                        kb.group("pe", [lambda e, c=c: e.matmul(psS[ii % 3][:, c * 512:(c + 1) * 512], lhsT=kT[:, kvh, (2 * u + c) * 128:(2 * u + c + 1) * 128],
                                                                rhs=qb[bs][:, h, :], start=True, stop=True) for c in range(2)],
                                 reads=[b_kT, b_qb[bs]], writes=[b_psS[ii % 3]])
                    emit_S(0); emit_S(1)
                    for u in range(NU):
                        ii = base + u
                        pt = PT[ii % 4]
                        kb.op("act", lambda e: e.activation(out=pt[:], in_=psS[ii % 3][:], func=AF.Exp, bias=nbias[:, 0:1], scale=1.0),
                              reads=[b_psS[ii % 3], b_nb], writes=[b_PT[ii % 4]])
                        if u + 2 < NU:
                            emit_S(u + 2)
                        first = (u == 0); last = (u == NU - 1)
                        kb.group("pe", [lambda e, c=c: e.matmul(psO[0][:], lhsT=V[:, 2 * u + c, kvh * 128:(kvh + 1) * 128], rhs=pt[:, c * 512:(c + 1) * 512],
                                                                start=(first and c == 0), stop=(last and c == 1)) for c in range(2)],
                                 reads=[b_V, b_PT[ii % 4]], writes=([b_psO[0]] if (first or last) else []))
                        if u % 2 == 1:
                            ptp = PT[(ii - 1) % 4]
                            srcs = [ptp[:, 0:512], ptp[:, 512:1024], pt[:, 0:512], pt[:, 512:1024]]
                            kb.group("pe", [lambda e, jc=jc: e.matmul(psL[0][32 * jc:32 * (jc + 1), :], lhsT=ones32[:, :], rhs=srcs[jc],
                                                                      start=(u == 1), stop=(u == NU - 1), tile_position=(0, 32 * jc)) for jc in range(4)],
                                     reads=[b_ones32, b_PT[(ii - 1) % 4], b_PT[ii % 4]], writes=([b_psL[0]] if (u == 1 or u == NU - 1) else []))
                    i_u += NU
                    kb.op("dve", lambda e: e.tensor_copy(out=acc1[:], in_=psL[0][:]), reads=[b_psL[0]], writes=[b_acc1])
                    kb.op("dve", lambda e: e.tensor_copy(out=Osb[:], in_=psO[0][:]), reads=[b_psO[0]], writes=[b_Osb])
                    kb.op("pe", lambda e: e.matmul(psL[0][:], lhsT=onesF[:], rhs=acc1[:], start=True, stop=True), reads=[b_onesF, b_acc1], writes=[b_psL[0]])
                    jo = (blk * 8 + h) % 2
                    kb.op("dve", lambda e: e.reciprocal(out=rec[:], in_=psL[0][:]), reads=[b_psL[0]], writes=[b_rec])
                    kb.op("dve", lambda e: e.tensor_tensor(out=OTs[jo][:], in0=Osb[:], in1=rec[:], op=ALU.mult), reads=[b_Osb, b_rec], writes=[b_OTs[jo]])
                    kb.op("sp", lambda e: e.dma_start(out=OT_dram[h * 128:(h + 1) * 128, blk * 512:(blk + 1) * 512], in_=OTs[jo][:]),
                          reads=[b_OTs[jo]], dsem=s_o[jo])
                    precast_step(2)
            precast_step(len(pc_jobs))
            kb.barrier()

        if stop_after >= 5:
          with ExitStack() as es:
            Wf = T(es, "Wf", [128, 8, D], BF16); b_Wf = Buf()
            wao = T(es, "wao", [128, 8, D], BF16); b_wao = Buf()
            wout = T(es, "wout", [128, 8, D], BF16); b_wout = Buf()
            lng = T(es, "lng", [128, D], F32); lnb = T(es, "lnb", [128, D], F32); b_ln = Buf()
            s_w5 = kb.sem("p5w"); s_ln = kb.sem("p5ln")
            kb.op("sp", lambda e: e.dma_start(out=lng[:], in_=I["ln1_g"][0:1, :].partition_broadcast(128)), writes=[b_ln], dsem=s_ln)
            kb.op("sp", lambda e: e.dma_start(out=lnb[:], in_=I["ln1_b"][0:1, :].partition_broadcast(128)), writes=[b_ln], dsem=s_ln)
            wao_v = I["w_attn_o"].rearrange("(kc p) d -> p kc d", p=128); wout_v = I["w_out"].rearrange("(kc p) d -> p kc d", p=128)
            for kc in range(8):
                kb.op("pool", lambda e, kc=kc: e.dma_start(out=wao[:, kc, :], in_=wao_v[:, kc, :]), writes=[b_wao], dsem=s_w5)
                kb.op("pool", lambda e, kc=kc: e.dma_start(out=wout[:, kc, :], in_=wout_v[:, kc, :]), writes=[b_wout], dsem=s_w5)
            psF = [P(es, f"psF{i}", [128, 512], F32) for i in range(2)]; b_psF = [Buf(), Buf()]
            psA = [P(es, f"psA{i}", [128, 512], F32) for i in range(2)]; b_psA = [Buf(), Buf()]
            psH = P(es, "psH", [128, D], F32); b_psH = Buf()
            with ExitStack() as es2:
                wf_sb = T(es2, "wf_sb", [128, 4, D], BF16); b_wf = Buf()
                dftc = T(es2, "dftc", [128, 2, 128], BF16); b_dftc = Buf()
                s_f5 = kb.sem("p5f")
                kb.op("pool", lambda e: e.dma_start(out=wf_sb[:], in_=I["w_fourier"].rearrange("(g p) d -> p g d", p=128)), writes=[b_wf], dsem=s_f5)
                kb.op("sp", lambda e: e.dma_start(out=dftc[:], in_=I["dft_c"][:, :, :]), writes=[b_dftc], dsem=s_f5)
                cnt = 0
                for xr in range(2):
                    for g in range(4):
                        for half in range(2):
                            pb = cnt % 2; cnt += 1
                            kb.op("pe", lambda e: e.matmul(psF[pb][:], lhsT=dftc[:, xr, :], rhs=wf_sb[:, g, half * 512:(half + 1) * 512], start=True, stop=True),
                                  reads=[b_dftc, b_wf], writes=[b_psF[pb]])
                            kb.op("dve", lambda e: e.tensor_copy(out=Wf[:, xr * 4 + g, half * 512:(half + 1) * 512], in_=psF[pb][:]),
                                  reads=[b_psF[pb]], writes=[b_Wf])
                kb.barrier()
            XTb = [T(es, f"XTb{i}", [128, 8, 512], BF16) for i in range(2)]; b_XTb = [Buf(), Buf()]
            OTb = [T(es, f"OTb{i}", [128, 8, 512], BF16) for i in range(2)]; b_OTb = [Buf(), Buf()]
            gTb = [T(es, f"gTb{i}", [128, 16, 512], BF16) for i in range(2)]; b_gTb = [Buf(), Buf()]
            mT = T(es, "mT", [128, 8, 512], BF16); b_mT = Buf()
            ta = T(es, "ta", [128, 512], F32); tb2 = T(es, "tb2", [128, 512], F32); b_ta = Buf(); b_tb2 = Buf()
            x5 = [T(es, f"x5_{i}", [128, D], F32) for i in range(2)]; b_x5 = [Buf(), Buf()]
            r5 = T(es, "r5", [128, D], F32); b_r5 = Buf()
            st5 = T(es, "st5", [128, 2, 6], F32); mv5 = T(es, "mv5", [128, 2], F32); rstd5 = T(es, "rstd5", [128, 1], F32)
            b_st5 = Buf(); b_mv5 = Buf(); b_rstd5 = Buf()
            y5 = [T(es, f"y5_{i}", [128, D], F32) for i in range(2)]; b_y5 = [Buf(), Buf()]
            s_blk = [kb.sem("p5b0"), kb.sem("p5b1")]; s_x5 = [kb.sem("p5x0"), kb.sem("p5x1")]; s_y5 = [kb.sem("p5y0"), kb.sem("p5y1")]
            XT_v = XT_dram.rearrange("(kc p) s -> p kc s", p=128); OT_v = OT_dram.rearrange("(kc p) s -> p kc s", p=128)
            gT_v5 = gT_dram.rearrange("(g p) s -> p g s", p=128)
            x_v5 = I["x"].rearrange("(t p) d -> t p d", p=128); x1_v = x1_dram.rearrange("(t p) d -> t p d", p=128)

            def load_blk(tb):
                sl = tb % 2
                kb.op("sp", lambda e: e.dma_start(out=XTb[sl][:], in_=XT_v[:, :, tb * 512:(tb + 1) * 512]), writes=[b_XTb[sl]], dsem=s_blk[sl])
                kb.op("sp", lambda e: e.dma_start(out=OTb[sl][:], in_=OT_v[:, :, tb * 512:(tb + 1) * 512]), writes=[b_OTb[sl]], dsem=s_blk[sl])
                kb.op("sp", lambda e: e.dma_start(out=gTb[sl][:], in_=gT_v5[:, :, tb * 512:(tb + 1) * 512]), writes=[b_gTb[sl]], dsem=s_blk[sl])

            mT_ = [mT, T(es, "mT1", [128, 8, 512], BF16)]; b_mT_ = [b_mT, Buf()]
            ta_ = [ta, T(es, "ta1", [128, 512], F32)]; tb2_ = [tb2, T(es, "tb21", [128, 512], F32)]; b_ta_ = [b_ta, Buf()]; b_tb2_ = [b_tb2, Buf()]
            r5_ = [r5, T(es, "r5_1", [128, D], F32)]; b_r5_ = [b_r5, Buf()]
            st5_ = [st5, T(es, "st5_1", [128, 2, 6], F32)]; mv5_ = [mv5, T(es, "mv5_1", [128, 2], F32)]; rstd5_ = [rstd5, T(es, "rstd5_1", [128, 1], F32)]
            b_st5_ = [b_st5, Buf()]; b_mv5_ = [b_mv5, Buf()]; b_rstd5_ = [b_rstd5, Buf()]

            def gate5(tb):
                sl = tb % 2
                load_blk(tb)
                yield
                for Dc in range(8):
                    pb = Dc % 2
                    kb.group("pe", [lambda e, kc=kc: e.matmul(psF[pb][:], lhsT=Wf[:, kc, Dc * 128:(Dc + 1) * 128], rhs=XTb[sl][:, kc, :],
                                                              start=(kc == 0), stop=(kc == 7)) for kc in range(8)],
                             reads=[b_Wf, b_XTb[sl]], writes=[b_psF[pb]])
                    kb.group("pe", [lambda e, kc=kc: e.matmul(psA[pb][:], lhsT=wao[:, kc, Dc * 128:(Dc + 1) * 128], rhs=OTb[sl][:, kc, :],
                                                              start=(kc == 0), stop=(kc == 7)) for kc in range(8)],
                             reads=[b_wao, b_OTb[sl]], writes=[b_psA[pb]])
                    kb.op("dve", lambda e: e.tensor_tensor(out=ta_[pb][:], in0=psF[pb][:], in1=gTb[sl][:, Dc, :], op=ALU.mult),
                          reads=[b_psF[pb], b_gTb[sl]], writes=[b_ta_[pb]])
                    kb.op("dve", lambda e: e.tensor_tensor(out=tb2_[pb][:], in0=psA[pb][:], in1=gTb[sl][:, 8 + Dc, :], op=ALU.mult),
                          reads=[b_psA[pb], b_gTb[sl]], writes=[b_tb2_[pb]])
                    yield
                    kb.op("dve", lambda e: e.tensor_tensor(out=mT_[sl][:, Dc, :], in0=ta_[pb][:], in1=tb2_[pb][:], op=ALU.add),
                          reads=[b_ta_[pb], b_tb2_[pb]], writes=[b_mT_[sl]])
                    yield

            def tile5(tb, tt):
                sl = tb % 2; t = tb * 4 + tt; xs_ = t % 2
                r5t = r5_[xs_]; b_r5t = b_r5_[xs_]
                kb.op("sp", lambda e: e.dma_start(out=x5[xs_][:], in_=x_v5[t]), writes=[b_x5[xs_]], dsem=s_x5[xs_])
                yield
                for half in range(2):
                    kb.group("pe", [lambda e, Dc=Dc: e.matmul(psH[:, half * 512:(half + 1) * 512], lhsT=mT_[sl][:, Dc, tt * 128:(tt + 1) * 128],
                                                              rhs=wout[:, Dc, half * 512:(half + 1) * 512], start=(Dc == 0), stop=(Dc == 7)) for Dc in range(8)],
                             reads=[b_mT_[sl], b_wout], writes=[b_psH])
                kb.op("dve", lambda e: e.tensor_tensor(out=r5t[:], in0=psH[:], in1=G1, op=ALU.mult), reads=[b_psH, b_mod], writes=[b_r5t])
                yield
                kb.op("dve", lambda e: e.scalar_tensor_tensor(out=r5t[:], in0=x5[xs_][:], scalar=float(ALPHA), in1=r5t[:], op0=ALU.mult, op1=ALU.add),
                      reads=[b_x5[xs_], b_r5t], writes=[b_r5t])
                yield
                yield from ln_tile_g(r5t, b_r5t, y5[xs_], b_y5[xs_], st5_[xs_], b_st5_[xs_], mv5_[xs_], b_mv5_[xs_], rstd5_[xs_], b_rstd5_[xs_],
                                     1e-5, lng[:], lnb[:], b_ln)
                kb.op("sp", lambda e: e.dma_start(out=x1_v[t], in_=y5[xs_][:]), reads=[b_y5[xs_]], dsem=s_y5[xs_])
                yield

            items5 = [("g0", gate5(0), ()), ("g1", gate5(1), ("g0",))]
            for tb in range(NB):
                for tt in range(4):
                    items5.append((f"t{tb}_{tt}", tile5(tb, tt), (f"g{tb}",)))
                if tb + 2 < NB:
                    items5.append((f"g{tb + 2}", gate5(tb + 2), (f"t{tb}_3", f"g{tb + 1}")))
            run_interleaved(items5, 2)
            kb.barrier()


        if stop_after >= 6:
          with ExitStack() as es68:
            w4_all = T(es68, "w4_all", [128, NT, 4], F32); b_w4 = Buf()
            dest_i = T(es68, "dest_i", [128, NT * 4], I32); b_desti = Buf()
            blk_i = T(es68, "blk_i", [128, NBLK], I32); chg_i = T(es68, "chg_i", [128, NBLK], I32); b_blk = Buf()
            idx_w = T(es68, "idx_w", [128, NBLK], I32); idx_b = T(es68, "idx_b", [128, NBLK], I32)
            with ExitStack() as es:
                identF = T(es, "identF", [128, 128], F32); b_identF = Buf()
                kb.op("pool", lambda e: e.memset(identF[:], 0.0), writes=[b_identF])
                kb.op("pool", lambda e: e.affine_select(out=identF[:], in_=identF[:], pattern=[[-1, 128]], compare_op=ALU.not_equal,
                                                        fill=1.0, base=0, channel_multiplier=1), writes=[b_identF])
                ustr = T(es, "ustr", [128, 128], BF16); b_ustr = Buf()
                kb.op("pool", lambda e: e.memset(ustr[:], 1.0), writes=[b_ustr])
                kb.op("pool", lambda e: e.affine_select(out=ustr[:], in_=ustr[:], pattern=[[1, 128]], compare_op=ALU.is_gt,
                                                        fill=0.0, base=0, channel_multiplier=-1), writes=[b_ustr])
                onesb = T(es, "onesb6", [128, 128], BF16); b_onesb = Buf()
                kb.op("pool", lambda e: e.memset(onesb[:], 1.0), writes=[b_onesb])
                ones1f = T(es, "ones1f", [1, 128], F32); b_ones1f = Buf()
                kb.op("pool", lambda e: e.memset(ones1f[:], 1.0), writes=[b_ones1f])
                wr = T(es, "wr", [128, 8, NE], F32); br = T(es, "br", [1, NE], F32); b_wr = Buf()
                s_wr = kb.sem("p6wr")
                kb.op("sp", lambda e: e.dma_start(out=wr[:], in_=I["w_router"].rearrange("(dc p) n -> p dc n", p=128)), writes=[b_wr], dsem=s_wr)
                kb.op("sp", lambda e: e.dma_start(out=br[:], in_=I["b_router"][:, :]), writes=[b_wr], dsem=s_wr)
                x6 = [T(es, f"x6_{i}", [128, D], F32) for i in range(2)]; b_x6 = [Buf(), Buf()]
                u2 = T(es, "u2", [128, D], F32); b_u2 = Buf()
                u2b = [T(es, f"u2b{i}", [128, D], BF16) for i in range(2)]; b_u2b = [Buf(), Buf()]
                u2T = T(es, "u2T", [128, 8, 128], F32); b_u2T = Buf()
                st6 = T(es, "st6", [128, 2, 6], F32); mv6 = T(es, "mv6", [128, 2], F32); rstd6 = T(es, "rstd6", [128, 1], F32)
                b_st6 = Buf(); b_mv6 = Buf(); b_rstd6 = Buf()
                L_all = T(es, "L_all", [128, NT, NE], F32); b_L = Buf()
                top8 = T(es, "top8", [128, 8], F32); b_top8 = Buf()
                top4_all = T(es, "top4_all", [128, NT, 4], F32); b_top4 = Buf()
                negmax = T(es, "negmax", [128, 1], F32); b_negmax = Buf()
                e4 = T(es, "e4", [128, 4], F32); den = T(es, "den", [128, 1], F32); b_e4 = Buf(); b_den = Buf()
                maskb = T(es, "maskb", [128, NE], BF16); b_maskb = Buf()
                pos_all = T(es, "pos_all", [128, NT, NE], F32); b_pos = Buf()
                runcnt = T(es, "runcnt", [128, NE], F32); b_run = Buf()
                kb.op("dve", lambda e: e.memset(runcnt[:], 0.0), writes=[b_run])
                psT6 = P(es, "psT6", [128, 8, 128], F32); b_psT6 = Buf()
                psLg = P(es, "psLg", [128, NE], F32); b_psLg = Buf()
                psPos = P(es, "psPos", [128, NE], F32); b_psPos = Buf()
                psCnt = P(es, "psCnt", [128, NE], F32); b_psCnt = Buf()
                s_x6 = [kb.sem("p6x0"), kb.sem("p6x1")]; s_u6 = [kb.sem("p6u0"), kb.sem("p6u1")]
                x1_v6 = x1_dram.rearrange("(t p) d -> t p d", p=128); u2_v = u2_dram.rearrange("(t p) d -> t p d", p=128)
                u2_ = [u2, T(es, "u2_1", [128, D], F32)]; b_u2_ = [b_u2, Buf()]
                u2T_ = [u2T, T(es, "u2T_1", [128, 8, 128], F32)]; b_u2T_ = [b_u2T, Buf()]
                st6_ = [st6, T(es, "st6_1", [128, 2, 6], F32)]; mv6_ = [mv6, T(es, "mv6_1", [128, 2], F32)]; rstd6_ = [rstd6, T(es, "rstd6_1", [128, 1], F32)]
                b_st6_ = [b_st6, Buf()]; b_mv6_ = [b_mv6, Buf()]; b_rstd6_ = [b_rstd6, Buf()]
                top8_ = [top8, T(es, "top8_1", [128, 8], F32)]; b_top8_ = [b_top8, Buf()]
                negmax_ = [negmax, T(es, "negmax_1", [128, 1], F32)]; b_negmax_ = [b_negmax, Buf()]
                e4_ = [e4, T(es, "e4_1", [128, 4], F32)]; den_ = [den, T(es, "den_1", [128, 1], F32)]; b_e4_ = [b_e4, Buf()]; b_den_ = [b_den, Buf()]
                maskb_ = [maskb, T(es, "maskb_1", [128, NE], BF16)]; b_maskb_ = [b_maskb, Buf()]
                for i6 in range(2):
                    kb.op("sp", lambda e, i6=i6: e.dma_start(out=x6[i6][:], in_=x1_v6[i6]), writes=[b_x6[i6]], dsem=s_x6[i6])

                def route_a(t):
                    sl = t % 2
                    u2c = u2_[sl]; b_u2c = b_u2_[sl]; u2Tc = u2T_[sl]; b_u2Tc = b_u2T_[sl]
                    t8 = top8_[sl]; b_t8 = b_top8_[sl]; nm = negmax_[sl]; b_nm = b_negmax_[sl]
                    e4c = e4_[sl]; b_e4c = b_e4_[sl]; dn = den_[sl]; b_dn = b_den_[sl]; mk = maskb_[sl]; b_mk = b_maskb_[sl]
                    yield from ln_tile_g(x6[sl], b_x6[sl], u2c, b_u2c, st6_[sl], b_st6_[sl], mv6_[sl], b_mv6_[sl], rstd6_[sl], b_rstd6_[sl], 1e-6, SC2, SH2, b_mod)
                    if t + 2 < NT:
                        kb.op("sp", lambda e: e.dma_start(out=x6[sl][:], in_=x1_v6[t + 2]), writes=[b_x6[sl]], dsem=s_x6[sl])
                    kb.op("act", lambda e: e.copy(out=u2b[sl][:], in_=u2c[:]), reads=[b_u2c], writes=[b_u2b[sl]])
                    yield
                    kb.op("sp", lambda e: e.dma_start(out=u2_v[t], in_=u2b[sl][:]), reads=[b_u2b[sl]], dsem=s_u6[sl])
                    yield
                    kb.group("pe", [lambda e, dc=dc: e.transpose(out=psT6[:, dc, :], in_=u2c[:, dc * 128:(dc + 1) * 128], identity=identF[:]) for dc in range(8)],
                             reads=[b_u2c, b_identF], writes=[b_psT6])
                    kb.op("dve", lambda e: e.tensor_copy(out=u2Tc[:], in_=psT6[:]), reads=[b_psT6], writes=[b_u2Tc])
                    yield
                    fns = [lambda e, dc=dc: e.matmul(psLg[:], lhsT=u2Tc[:, dc, :], rhs=wr[:, dc, :], start=(dc == 0), stop=False) for dc in range(8)]
                    fns.append(lambda e: e.matmul(psLg[:], lhsT=ones1f[0:1, :], rhs=br[0:1, :], start=False, stop=True))
                    kb.group("pe", fns, reads=[b_u2Tc, b_wr, b_ones1f], writes=[b_psLg])
                    Lt = L_all[:, t, :]
                    kb.op("dve", lambda e: e.tensor_copy(out=Lt, in_=psLg[:]), reads=[b_psLg], writes=[b_L])
                    yield
                    kb.op("dve", lambda e: e.max(out=t8[:], in_=Lt), reads=[b_L], writes=[b_t8])
                    yield
                    kb.op("dve", lambda e: e.tensor_copy(out=top4_all[:, t, :], in_=t8[:, 0:4]), reads=[b_t8], writes=[b_top4])
                    yield
                    kb.op("dve", lambda e: e.tensor_scalar(out=mk[:], in0=Lt, scalar1=t8[:, 3:4], scalar2=None, op0=ALU.is_ge),
                          reads=[b_L, b_t8], writes=[b_mk])
                    yield
                    kb.op("dve", lambda e: e.tensor_scalar(out=nm[:], in0=t8[:, 0:1], scalar1=-1.0, scalar2=None, op0=ALU.mult),
                          reads=[b_t8], writes=[b_nm])
                    yield
                    kb.op("act", lambda e: e.activation(out=e4c[:], in_=t8[:, 0:4], func=AF.Exp, bias=nm[:, 0:1], scale=1.0, accum_out=dn[:, 0:1]),
                          reads=[b_t8, b_nm], writes=[b_e4c, b_dn])
                    yield
                    kb.op("dve", lambda e: e.reciprocal(out=dn[:], in_=dn[:]), reads=[b_dn], writes=[b_dn])
                    yield
                    kb.op("dve", lambda e: e.tensor_scalar(out=w4_all[:, t, :], in0=e4c[:], scalar1=dn[:, 0:1], scalar2=None, op0=ALU.mult),
                          reads=[b_e4c, b_dn], writes=[b_w4])
                    yield

                def route_b(t):
                    mk = maskb_[t % 2]; b_mk = b_maskb_[t % 2]
                    kb.op("pe", lambda e: e.matmul(psPos[:], lhsT=ustr[:], rhs=mk[:], start=True, stop=True), reads=[b_ustr, b_mk], writes=[b_psPos])
                    kb.op("pe", lambda e: e.matmul(psCnt[:], lhsT=onesb[:], rhs=mk[:], start=True, stop=True), reads=[b_onesb, b_mk], writes=[b_psCnt])
                    kb.op("dve", lambda e: e.tensor_tensor(out=pos_all[:, t, :], in0=psPos[:], in1=runcnt[:], op=ALU.add), reads=[b_psPos, b_run], writes=[b_pos])
                    kb.op("dve", lambda e: e.tensor_tensor(out=runcnt[:], in0=psCnt[:], in1=runcnt[:], op=ALU.add), reads=[b_psCnt, b_run], writes=[b_run])
                    yield

                items6 = []
                for t in range(NT):
                    items6.append((f"a{t}", route_a(t), ((f"b{t - 2}",) if t >= 2 else ())))
                    if t >= 1:
                        items6.append((f"b{t - 1}", route_b(t - 1), (f"a{t - 1}",) + ((f"b{t - 2}",) if t >= 2 else ())))
                items6.append((f"b{NT - 1}", route_b(NT - 1), (f"a{NT - 1}", f"b{NT - 2}")))
                run_interleaved(items6, 2)
                thr_i = T(es, "thr_i", [128, 64], I32); thr = T(es, "thr", [128, 64], F32); b_thr = Buf()
                kb.op("pool", lambda e: e.iota(thr_i[:], pattern=[[BS, 64]], base=0, channel_multiplier=0), writes=[b_thr])
                kb.op("dve", lambda e: e.tensor_copy(out=thr[:], in_=thr_i[:]), reads=[b_thr], writes=[b_thr])
                jf_i = T(es, "jf_i", [128, NBLK], I32); jf = T(es, "jf", [128, NBLK], F32); b_jf = Buf()
                kb.op("pool", lambda e: e.iota(jf_i[:], pattern=[[1, NBLK]], base=0, channel_multiplier=0), writes=[b_jf])
                kb.op("dve", lambda e: e.tensor_copy(out=jf[:], in_=jf_i[:]), reads=[b_jf], writes=[b_jf])
                junk = T(es, "junk", [128, 64], F32); b_junk = Buf()
                nblk = T(es, "nblk", [128, NE], F32); b_nblk = Buf()
                zeros32 = T(es, "zeros32", [128, NE], F32); b_z32 = Buf()
                kb.op("dve", lambda e: e.memset(zeros32[:], 0.0), writes=[b_z32])
                for ex in range(NE):
                    kb.op("dve", lambda e, ex=ex: e.tensor_scalar(out=junk[:], in0=thr[:], scalar1=runcnt[:, ex:ex + 1], scalar2=0.0, op0=ALU.is_lt, op1=ALU.add,
                                                                  accum_out=nblk[:, ex:ex + 1]), reads=[b_thr, b_run], writes=[b_junk, b_nblk])
                pend = T(es, "pend", [128, NE], F32); b_pend = Buf()
                kb.op("dve", lambda e: e.tensor_tensor_scan(out=pend[:], data0=nblk[:], data1=zeros32[:], initial=0.0, op0=ALU.add, op1=ALU.add),
                      reads=[b_nblk, b_z32], writes=[b_pend])
                pstart = T(es, "pstart", [128, NE], F32); b_pstart = Buf()
                kb.op("dve", lambda e: e.tensor_tensor(out=pstart[:], in0=pend[:], in1=nblk[:], op=ALU.subtract), reads=[b_pend, b_nblk], writes=[b_pstart])
                kb.op("dve", lambda e: e.tensor_scalar(out=pstart[:], in0=pstart[:], scalar1=float(BS), scalar2=None, op0=ALU.mult), reads=[b_pstart], writes=[b_pstart])
                acc = T(es, "acc6", [128, NBLK], F32); b_acc = Buf()
                chg = T(es, "chg6", [128, NBLK], F32); b_chg = Buf()
                kb.op("dve", lambda e: e.memset(acc[:], 0.0), writes=[b_acc])
                for ex in range(NE - 1):
                    kb.op("dve", lambda e, ex=ex: e.scalar_tensor_tensor(out=acc[:], in0=jf[:], scalar=pend[:, ex:ex + 1], in1=acc[:], op0=ALU.is_ge, op1=ALU.add),
                          reads=[b_jf, b_pend, b_acc], writes=[b_acc])
                kb.op("dve", lambda e: e.memset(chg[:], 1.0), writes=[b_chg])
                kb.op("dve", lambda e: e.tensor_tensor(out=chg[:, 2:NBLK], in0=acc[:, 2:NBLK], in1=acc[:, 0:NBLK - 2], op=ALU.not_equal), reads=[b_acc, b_chg], writes=[b_chg])
                kb.op("dve", lambda e: e.tensor_copy(out=blk_i[:], in_=acc[:]), reads=[b_acc], writes=[b_blk])
                kb.op("dve", lambda e: e.tensor_copy(out=chg_i[:], in_=chg[:]), reads=[b_chg], writes=[b_blk])
                BIG = float(1 << 20)
                pio_i = T(es, "pio_i", [128, 1], I32); pio = T(es, "pio", [128, 1], F32); b_pio = Buf()
                kb.op("pool", lambda e: e.iota(pio_i[:], pattern=[[0, 1]], base=0, channel_multiplier=1), writes=[b_pio])
                kb.op("dve", lambda e: e.tensor_copy(out=pio[:], in_=pio_i[:]), reads=[b_pio], writes=[b_pio])
                idxf = T(es, "idxf", [128, NBLK], F32); b_idxf = Buf()
                kb.op("dve", lambda e: e.tensor_scalar(out=idxf[:], in0=acc[:], scalar1=128.0, scalar2=None, op0=ALU.mult), reads=[b_acc], writes=[b_idxf])
                kb.op("dve", lambda e: e.tensor_scalar(out=idxf[:], in0=idxf[:], scalar1=pio[:, 0:1], scalar2=None, op0=ALU.add), reads=[b_idxf, b_pio], writes=[b_idxf])
                kb.op("dve", lambda e: e.tensor_copy(out=idx_w[:], in_=idxf[:]), reads=[b_idxf], writes=[b_blk])
                kb.op("dve", lambda e: e.tensor_copy(out=idx_b[:], in_=acc[:]), reads=[b_acc], writes=[b_blk])
                destf = T(es, "destf", [128, NT * 4], F32); b_destf = Buf()
                A6 = T(es, "A6", [128, NE], F32); b_A6 = Buf()
                junk2 = T(es, "junk2", [128, NE], F32); b_junk2 = Buf()
                s_sc = [kb.sem("p6s0"), kb.sem("p6s1")]; s_ul = [kb.sem("p6l0"), kb.sem("p6l1")]
                A6_ = [A6, T(es, "A6_1", [128, NE], F32)]; b_A6_ = [b_A6, Buf()]
                b_dt = [Buf() for _ in range(NT)]
                for t in range(NT):
                    sl = t % 2
                    kb.op("dve", lambda e: e.tensor_tensor(out=A6_[sl][:], in0=pos_all[:, t, :], in1=pstart[:], op=ALU.add), reads=[b_pos, b_pstart], writes=[b_A6_[sl]])
                    for j in range(4):
                        kb.op("dve", lambda e, j=j: e.scalar_tensor_tensor(out=junk2[:], in0=L_all[:, t, :], scalar=top4_all[:, t, j:j + 1], in1=A6_[sl][:],
                                                                           op0=ALU.is_equal, op1=ALU.mult, accum_out=destf[:, t * 4 + j:t * 4 + j + 1]),
                              reads=[b_L, b_top4, b_A6_[sl]], writes=[b_junk2, b_destf])
                    kb.op("dve", lambda e: e.tensor_copy(out=dest_i[:, t * 4:(t + 1) * 4], in_=destf[:, t * 4:(t + 1) * 4]), reads=[b_destf], writes=[b_dt[t]])
                    kb.op("sp", lambda e: e.dma_start(out=u2b[sl][:], in_=u2_v[t]), writes=[b_u2b[sl]], dsem=s_ul[sl])
                    for j in range(4):
                        kb.op("pool", lambda e, j=j: e.indirect_dma_start(out=xs_dram[:, :], out_offset=bass.IndirectOffsetOnAxis(ap=dest_i[:, t * 4 + j:t * 4 + j + 1], axis=0),
                                                                          in_=u2b[sl][:], in_offset=None), reads=[b_u2b[sl], b_dt[t]], dsem=s_sc[sl])
                if dbg6 is not None:
                    s_d6 = kb.sem("p6dbg")
                    kb.op("sp", lambda e: e.dma_start(out=dbg6["L"].rearrange("(t p) n -> p t n", p=128), in_=L_all[:]), reads=[b_L], dsem=s_d6)
                    kb.op("sp", lambda e: e.dma_start(out=dbg6["dest"][:, :], in_=dest_i[:]), reads=b_dt, dsem=s_d6)
                    kb.op("sp", lambda e: e.dma_start(out=dbg6["blk"][:, :], in_=blk_i[0:1, :]), reads=[b_blk], dsem=s_d6)
                    kb.op("sp", lambda e: e.dma_start(out=dbg6["chg"][:, :], in_=chg_i[0:1, :]), reads=[b_blk], dsem=s_d6)
                    kb.op("sp", lambda e: e.dma_start(out=dbg6["w4"][:, :], in_=w4_all[:].rearrange("p t j -> p (t j)")), reads=[b_w4], dsem=s_d6)
                kb.barrier()

            if stop_after >= 7:
              with ExitStack() as es:
                wgu = [T(es, f"wgu{i}", [128, 8 * 2 * D], BF16) for i in range(2)]
                wd = [T(es, f"wd{i}", [128, 8 * D], BF16) for i in range(2)]
                bgf = [T(es, f"bgf{i}", [128, 16], F32) for i in range(2)]
                bd = [T(es, f"bd{i}", [2, D], BF16) for i in range(2)]
                b_w7 = [Buf(), Buf()]
                ones7 = T(es, "ones7", [1, 128], BF16); b_ones7 = Buf()
                kb.op("pool", lambda e: e.memset(ones7[:], 1.0), writes=[b_ones7])
                xsb = [T(es, f"xsb{i}", [128, NSUB, D], BF16) for i in range(2)]; b_xsb = [Buf(), Buf()]
                xsT = [T(es, f"xsT{i}", [128, 8, BS], BF16) for i in range(2)]; b_xsT = [Buf(), Buf()]
                actT = [T(es, f"actT{i}", [128, 8, BS], BF16) for i in range(2)]; b_actT = [Buf(), Buf()]
                g7 = [T(es, f"g7_{i}", [128, BS], F32) for i in range(2)]; b_g7 = [Buf(), Buf()]
                sg = [T(es, f"sg_{i}", [128, BS], F32) for i in range(2)]; b_sg = [Buf(), Buf()]
                u1 = [T(es, f"u1_{i}", [128, BS], F32) for i in range(2)]; b_u1 = [Buf(), Buf()]
                a1 = [T(es, f"a1_{i}", [128, BS], F32) for i in range(2)]; b_a1 = [Buf(), Buf()]
                ysb = [T(es, f"ysb{i}", [128, 512], BF16) for i in range(3)]; b_ysb = [Buf() for _ in range(3)]
                pstx = P(es, "pstx", [128, 8, 128], BF16); b_pstx = Buf()
                psG = [P(es, f"psG{i}", [128, BS], F32) for i in range(2)]; b_psG = [Buf(), Buf()]
                psU = [P(es, f"psU{i}", [128, BS], F32) for i in range(2)]; b_psU = [Buf(), Buf()]
                psYb = [P(es, f"psYb{i}", [128, 512], F32) for i in range(3)]; b_psYb = [Buf() for _ in range(3)]
                s_w7 = [kb.sem("p7w0"), kb.sem("p7w1")]; s_xs = [kb.sem("p7x0"), kb.sem("p7x1")]; s_ys = [kb.sem(f"p7y{i}") for i in range(3)]
                nblk7 = debug.get("p7_blocks", NBLK)
                xs_v = xs_dram.rearrange("(j t p) d -> j p t d", t=NSUB, p=128)

                def load_w(j):
                    sl = j % 2
                    for dst, srcT in ((wgu[sl][:, :], wgu_bf), (wd[sl][:, :], wd_bf), (bgf[sl][:, :], I["bgu_fm"])):
                        kb.op("pool", lambda e, dst=dst, srcT=srcT: e.indirect_dma_start(out=dst, out_offset=None, in_=srcT[:, :],
                                                                                        in_offset=bass.IndirectOffsetOnAxis(ap=idx_w[:, j:j + 1], axis=0)),
                              reads=[b_blk], writes=[b_w7[sl]], dsem=s_w7[sl])
                    kb.op("pool", lambda e: e.indirect_dma_start(out=bd[sl][0:2, :], out_offset=None, in_=bd_bf[:, :],
                                                                 in_offset=bass.IndirectOffsetOnAxis(ap=idx_b[0:2, j:j + 1], axis=0)),
                          reads=[b_blk], writes=[b_w7[sl]], dsem=s_w7[sl])
                    kb.op("sp", lambda e: e.dma_start(out=xsb[sl][:], in_=xs_v[j]), writes=[b_xsb[sl]], dsem=s_xs[sl])

                def emit_tx(jj):
                    s2 = jj % 2
                    for st in range(NSUB):
                        kb.group("pe", [lambda e, dc=dc: e.transpose(out=pstx[:, dc, :], in_=xsb[s2][:, st, dc * 128:(dc + 1) * 128], identity=ident[:]) for dc in range(8)],
                                 reads=[b_xsb[s2], b_ident], writes=[b_pstx])
                        kb.op("act", lambda e: e.copy(out=xsT[s2][:, :, st * 128:(st + 1) * 128], in_=pstx[:]), reads=[b_pstx], writes=[b_xsT[s2]])

                load_w(0)
                yk = 0
                for j in range(nblk7):
                    sl = j % 2
                    if j + 1 < nblk7:
                        load_w(j + 1)
                    if j == 0:
                        emit_tx(0)
                    for fc in range(8):
                        pb = fc % 2
                        kb.group("pe", [lambda e, r=r: e.matmul(psG[pb][:], lhsT=wgu[sl][:, r * 2048 + fc * 128:r * 2048 + (fc + 1) * 128], rhs=xsT[sl][:, r, :],
                                                                start=(r == 0), stop=(r == 7)) for r in range(8)],
                                 reads=[b_xsT[sl], b_w7[sl]], writes=[b_psG[pb]])
                        kb.group("pe", [lambda e, r=r: e.matmul(psU[pb][:], lhsT=wgu[sl][:, r * 2048 + 1024 + fc * 128:r * 2048 + 1024 + (fc + 1) * 128], rhs=xsT[sl][:, r, :],
                                                                start=(r == 0), stop=(r == 7)) for r in range(8)],
                                 reads=[b_xsT[sl], b_w7[sl]], writes=[b_psU[pb]])
                        kb.op("dve", lambda e: e.tensor_scalar(out=g7[pb][:], in0=psG[pb][:], scalar1=bgf[sl][:, fc:fc + 1], scalar2=7.0, op0=ALU.add, op1=ALU.min),
                              reads=[b_psG[pb], b_w7[sl]], writes=[b_g7[pb]])
                        kb.op("act", lambda e: e.activation(out=sg[pb][:], in_=g7[pb][:], func=AF.Sigmoid, scale=1.702), reads=[b_g7[pb]], writes=[b_sg[pb]])
                        kb.op("dve", lambda e: e.tensor_scalar(out=u1[pb][:], in0=psU[pb][:], scalar1=bgf[sl][:, 8 + fc:9 + fc], scalar2=7.0, op0=ALU.add, op1=ALU.min),
                              reads=[b_psU[pb], b_w7[sl]], writes=[b_u1[pb]])
                        kb.op("dve", lambda e: e.tensor_scalar(out=u1[pb][:], in0=u1[pb][:], scalar1=-7.0, scalar2=1.0, op0=ALU.max, op1=ALU.add),
                              reads=[b_u1[pb]], writes=[b_u1[pb]])
                        kb.op("dve", lambda e: e.tensor_tensor(out=a1[pb][:], in0=u1[pb][:], in1=g7[pb][:], op=ALU.mult), reads=[b_u1[pb], b_g7[pb]], writes=[b_a1[pb]])
                        kb.op("dve", lambda e: e.tensor_tensor(out=actT[sl][:, fc, :], in0=a1[pb][:], in1=sg[pb][:], op=ALU.mult), reads=[b_a1[pb], b_sg[pb]], writes=[b_actT[sl]])
                    if j + 1 < nblk7:
                        emit_tx(j + 1)
                    for st in range(NSUB):
                        r0 = j * BS + st * 128
                        for half in range(2):
                            yb = yk % 3; yk += 1
                            fns = [lambda e, fc=fc: e.matmul(psYb[yb][:], lhsT=actT[sl][:, fc, st * 128:(st + 1) * 128],
                                                             rhs=wd[sl][:, fc * 1024 + half * 512:fc * 1024 + (half + 1) * 512], start=(fc == 0), stop=False) for fc in range(8)]
                            fns.append(lambda e: e.matmul(psYb[yb][:], lhsT=ones7[0:1, :], rhs=bd[sl][0:1, half * 512:(half + 1) * 512], start=False, stop=True))
                            kb.group("pe", fns, reads=[b_actT[sl], b_w7[sl], b_ones7], writes=[b_psYb[yb]])
                            kb.op("act", lambda e: e.copy(out=ysb[yb][:], in_=psYb[yb][:]), reads=[b_psYb[yb]], writes=[b_ysb[yb]])
                            kb.op("act", lambda e: e.dma_start(out=ys_dram[r0:r0 + 128, half * 512:(half + 1) * 512], in_=ysb[yb][:]), reads=[b_ysb[yb]], dsem=s_ys[yb])
                kb.barrier()

            if stop_after >= 8:
              with ExitStack() as es:
                lng2 = T(es, "lng2", [128, D], F32); lnb2 = T(es, "lnb2", [128, D], F32); b_ln2 = Buf()
                s_ln2 = kb.sem("p8ln")
                kb.op("sp", lambda e: e.dma_start(out=lng2[:], in_=I["ln2_g"][0:1, :].partition_broadcast(128)), writes=[b_ln2], dsem=s_ln2)
                kb.op("sp", lambda e: e.dma_start(out=lnb2[:], in_=I["ln2_b"][0:1, :].partition_broadcast(128)), writes=[b_ln2], dsem=s_ln2)
                yg = [[T(es, f"yg{i}_{j}", [128, D], BF16) for j in range(4)] for i in range(2)]; b_yg = [[Buf() for _ in range(4)] for _ in range(2)]
                x8 = [T(es, f"x8_{i}", [128, D], F32) for i in range(2)]; b_x8 = [Buf(), Buf()]
                h8 = T(es, "h8", [128, D], F32); b_h8 = Buf()
                o8 = [T(es, f"o8_{i}", [128, D], F32) for i in range(2)]; b_o8 = [Buf(), Buf()]
                st8 = T(es, "st8", [128, 2, 6], F32); mv8 = T(es, "mv8", [128, 2], F32); rstd8 = T(es, "rstd8", [128, 1], F32)
                b_st8 = Buf(); b_mv8 = Buf(); b_rstd8 = Buf()
                s_g8 = [kb.sem("p8g0"), kb.sem("p8g1")]; s_x8 = [kb.sem("p8x0"), kb.sem("p8x1")]; s_o8 = [kb.sem("p8o0"), kb.sem("p8o1")]
                x1_v8 = x1_dram.rearrange("(t p) d -> t p d", p=128); out_v = out.rearrange("(t p) d -> t p d", p=128)

                def load8(t):
                    sl = t % 2
                    kb.op("sp", lambda e: e.dma_start(out=x8[sl][:], in_=x1_v8[t]), writes=[b_x8[sl]], dsem=s_x8[sl])
                    for j in range(4):
                        kb.op("pool", lambda e, j=j: e.indirect_dma_start(out=yg[sl][j][:], out_offset=None, in_=ys_dram[:, :],
                                                                          in_offset=bass.IndirectOffsetOnAxis(ap=dest_i[:, t * 4 + j:t * 4 + j + 1], axis=0)),
                              reads=[b_desti], writes=[b_yg[sl][j]], dsem=s_g8[sl])
                h8_ = [h8, T(es, "h8_1", [128, D], F32)]; b_h8_ = [b_h8, Buf()]
                st8_ = [st8, T(es, "st8_1", [128, 2, 6], F32)]; mv8_ = [mv8, T(es, "mv8_1", [128, 2], F32)]; rstd8_ = [rstd8, T(es, "rstd8_1", [128, 1], F32)]
                b_st8_ = [b_st8, Buf()]; b_mv8_ = [b_mv8, Buf()]; b_rstd8_ = [b_rstd8, Buf()]
                load8(0); load8(1)

                def tile8(t):
                    sl = t % 2
                    hh = h8_[sl]; b_hh = b_h8_[sl]
                    kb.op("dve", lambda e: e.tensor_scalar(out=hh[:], in0=yg[sl][0][:], scalar1=w4_all[:, t, 0:1], scalar2=None, op0=ALU.mult),
                          reads=[b_yg[sl][0], b_w4], writes=[b_hh])
                    yield
                    for j in range(1, 4):
                        kb.op("dve", lambda e, j=j: e.scalar_tensor_tensor(out=hh[:], in0=yg[sl][j][:], scalar=w4_all[:, t, j:j + 1], in1=hh[:], op0=ALU.mult, op1=ALU.add),
                              reads=[b_yg[sl][j], b_w4, b_hh], writes=[b_hh])
                        yield
                    kb.op("dve", lambda e: e.tensor_tensor(out=hh[:], in0=hh[:], in1=G2, op=ALU.mult), reads=[b_hh, b_mod], writes=[b_hh])
                    yield
                    kb.op("dve", lambda e: e.scalar_tensor_tensor(out=hh[:], in0=x8[sl][:], scalar=float(ALPHA), in1=hh[:], op0=ALU.mult, op1=ALU.add),
                          reads=[b_x8[sl], b_hh], writes=[b_hh])
                    if t + 2 < NT:
                        load8(t + 2)
                    yield
                    yield from ln_tile_g(hh, b_hh, o8[sl], b_o8[sl], st8_[sl], b_st8_[sl], mv8_[sl], b_mv8_[sl], rstd8_[sl], b_rstd8_[sl],
                                         1e-5, lng2[:], lnb2[:], b_ln2)
                    kb.op("sp", lambda e: e.dma_start(out=out_v[t], in_=o8[sl][:]), reads=[b_o8[sl]], dsem=s_o8[sl])
                    yield

                run_interleaved((tile8(t) for t in range(NT)), 2)
                kb.barrier()

        kb.barrier()
    return nc, dbg_out, I


def _tables():
    rows = S // 64
    row_ids = np.repeat(np.arange(rows, dtype=np.float32), 64)
    col_ids = np.tile(np.arange(64, dtype=np.float32), rows)
    freqs = (np.float32(10000.0) ** (-np.arange(0, 64, 2, dtype=np.float32) / np.float32(64))).astype(np.float32)
    ang_r = (row_ids[:, None] * freqs).astype(np.float32)
    ang_c = (col_ids[:, None] * freqs).astype(np.float32)
    cr, sr, cc, sc = np.cos(ang_r), np.sin(ang_r), np.cos(ang_c), np.sin(ang_c)
    rope = np.concatenate([cr, cr, cc, cc, -sr, sr, -sc, sc], axis=1).astype(np.float32)
    n1 = np.arange(128)[:, None, None]; n2 = np.arange(64)[None, :, None]; k1 = np.arange(128)[None, None, :]
    ang = 2.0 * np.pi * ((k1 * (64 * n1 + n2)) % 8192) / 8192.0
    fft_e = np.stack([np.cos(ang), -np.sin(ang)], axis=2)
    n2v = np.arange(64)[:, None]; k2 = np.arange(64)[None, :]
    th = 2.0 * np.pi * ((n2v * k2) % 64) / 64.0
    sc_ = 1.0 / np.sqrt(8192.0)
    w3 = np.zeros((128, 128))
    w3[0:64, 0:64] = np.cos(th); w3[64:128, 0:64] = np.sin(th)
    w3[0:64, 64:128] = -np.sin(th); w3[64:128, 64:128] = np.cos(th)
    w3 *= sc_
    cch = np.arange(128)[:, None] * np.arange(128)[None, :]
    thc = 2.0 * np.pi * (cch % 128) / 128.0
    dft_c = np.stack([np.cos(thc), np.sin(thc)], axis=1) / np.sqrt(128.0)
    bf = ml_dtypes.bfloat16
    return {"rope_tab": rope, "fft_e": fft_e.astype(np.float32).astype(bf), "fft_w3": w3.astype(np.float32).astype(bf),
            "dft_c": dft_c.astype(np.float32).astype(bf)}


def make_in_maps(inputs):
    tabs = _tables()
    shared = {}
    for k in ("w_ada", "w_in", "w_fourier", "w_attn_o", "w_out", "w_router", "w_gate_up", "w_down"):
        shared[k] = np.ascontiguousarray(np.asarray(inputs[k], dtype=np.float32)[0])
    for k in ("b_ada", "q_norm_g", "k_norm_g", "ln1_g", "ln1_b", "b_router", "ln2_g", "ln2_b"):
        shared[k] = np.ascontiguousarray(np.asarray(inputs[k], dtype=np.float32)[0][None, :])
    shared["b_down"] = np.ascontiguousarray(np.asarray(inputs["b_down"], dtype=np.float32)[0])
    shared["bgu_fm"] = np.ascontiguousarray(np.asarray(inputs["b_gate_up"], dtype=np.float32)[0].reshape(NE, 16, 128).transpose(0, 2, 1).reshape(NE * 128, 16))
    shared.update(tabs)
    x = np.asarray(inputs["x"], dtype=np.float32)
    c = np.asarray(inputs["c"], dtype=np.float32)
    maps = []
    for b in range(8):
        m = dict(shared)
        m["x"] = np.ascontiguousarray(x[b])
        m["c_fm"] = np.ascontiguousarray(c[b].reshape(8, 128).T)
        maps.append(m)
    return maps


def kernel(**inputs):
    nc, _, _ = build()
    maps = make_in_maps(inputs)
    res = run_bass_kernel_spmd(nc, maps, core_ids=list(range(8)))
    return np.stack([np.asarray(r["out"], dtype=np.float32) for r in res.results], axis=0)
```

```python
import numpy as np
import ml_dtypes
from contextlib import ExitStack
import concourse.bass as bass
import concourse.mybir as mybir
from concourse.bass_utils import run_bass_kernel_spmd

F32 = mybir.dt.float32
BF16 = mybir.dt.bfloat16
I32 = mybir.dt.int32
AF = mybir.ActivationFunctionType
ALU = mybir.AluOpType
AX = mybir.AxisListType

S = 8192
D = 1024
NT = S // 128
NB = S // 512
NE = 32
BS = 512
NSUB = BS // 128
NBLK = (S * 4 + NE * (BS - 1)) // BS
NPAD = NBLK * BS
ALPHA = 2.0 ** 0.25


class Sem:
    def __init__(self, h, is_dma):
        self.h = h
        self.v = 0
        self.is_dma = is_dma


class Tok:
    __slots__ = ("sem", "val")

    def __init__(self, sem, val):
        self.sem = sem
        self.val = val


class Buf:
    def __init__(self, name=""):
        self.name = name
        self.w = None
        self.r = {}


class KB:
    def __init__(self, nc, es):
        self.nc = nc
        self.es = es
        self.engs = {"pe": nc.tensor, "act": nc.scalar, "dve": nc.vector, "pool": nc.gpsimd, "sp": nc.sync}
        self.sems = []
        self.esem = {}
        for e in ("pe", "act", "dve", "pool"):
            self.esem[e] = self.sem("e_" + e, is_dma=False)
        self.waited = {e: {} for e in self.engs}
        self.n_ins = 0

    def sem(self, name, is_dma=True):
        s = Sem(self.es.enter_context(self.nc.semaphore(name)), is_dma)
        self.sems.append(s)
        return s

    def wait(self, eng, tok):
        if tok is None:
            return
        val = tok.sem.v if tok.sem.is_dma else tok.val
        w = self.waited[eng]
        if w.get(id(tok.sem), 0) >= val:
            return
        self.engs[eng].wait_ge(tok.sem.h, val)
        w[id(tok.sem)] = val

    def _deps(self, eng, reads, writes):
        for b in reads:
            self.wait(eng, b.w)
        for b in writes:
            self.wait(eng, b.w)
            for t in b.r.values():
                self.wait(eng, t)

    def _done(self, tok, reads, writes):
        for b in reads:
            b.r[id(tok.sem)] = tok
        for b in writes:
            b.w = tok
            b.r = {}

    def op(self, eng, fn, reads=(), writes=(), dsem=None):
        self._deps(eng, reads, writes)
        ins = fn(self.engs[eng])
        self.n_ins += 1
        if dsem is not None:
            ins.then_inc(dsem.h, 16)
            dsem.v += 16
            tok = Tok(dsem, dsem.v)
        else:
            s = self.esem[eng]
            ins.then_inc(s.h, 1)
            s.v += 1
            tok = Tok(s, s.v)
        self._done(tok, reads, writes)
        return tok

    def group(self, eng, fns, reads=(), writes=()):
        self._deps(eng, reads, writes)
        ins = None
        for fn in fns:
            ins = fn(self.engs[eng])
            self.n_ins += 1
        s = self.esem[eng]
        ins.then_inc(s.h, 1)
        s.v += 1
        tok = Tok(s, s.v)
        self._done(tok, reads, writes)
        return tok

    def barrier(self):
        for e in self.engs:
            for s in self.sems:
                if s.v > 0:
                    self.wait(e, Tok(s, s.v))


def run_interleaved(gens, width=2):
    items = []
    for g in gens:
        items.append(g if isinstance(g, tuple) else (None, g, ()))
    done = set()
    active = []
    pos = 0
    while True:
        while len(active) < width and pos < len(items):
            name, g, deps = items[pos]
            if any(d not in done for d in deps):
                break
            active.append((name, g))
            pos += 1
        if not active:
            assert pos >= len(items), "interleave deadlock"
            break
        for ent in list(active):
            try:
                next(ent[1])
            except StopIteration:
                active.remove(ent)
                if ent[0] is not None:
                    done.add(ent[0])


def build(debug=None):
    debug = debug or {}
    stop_after = debug.get("stop_after", 99)
    nc = bass.Bass("TRN2", target_bir_lowering=False)
    I = {}

    def inp(name, shape, dt=F32):
        I[name] = nc.dram_tensor(name, shape, dt, kind="ExternalInput").ap()
        return I[name]

    inp("x", [S, D]); inp("c_fm", [128, 8]); inp("w_ada", [D, 6 * D]); inp("b_ada", [1, 6 * D])
    inp("w_in", [D, 4096]); inp("q_norm_g", [1, 128]); inp("k_norm_g", [1, 128])
    inp("w_fourier", [512, D]); inp("w_attn_o", [D, D]); inp("w_out", [D, D])
    inp("ln1_g", [1, D]); inp("ln1_b", [1, D]); inp("w_router", [D, NE]); inp("b_router", [1, NE])
    if stop_after >= 7:
        inp("w_gate_up", [NE, D, 2 * D]); inp("bgu_fm", [NE * 128, 16]); inp("w_down", [NE, D, D]); inp("b_down", [NE, D])
    inp("ln2_g", [1, D]); inp("ln2_b", [1, D])
    inp("rope_tab", [S, 256])
    inp("fft_e", [128, 64, 2, 128], BF16)
    inp("fft_w3", [128, 128], BF16)
    inp("dft_c", [128, 2, 128], BF16)
    out = nc.dram_tensor("out", [S, D], F32, kind="ExternalOutput").ap()

    dbg_out = {}

    def scratch(name, shape, dt):
        if name in debug.get("dump", ()):
            t = nc.dram_tensor(name, shape, dt, kind="ExternalOutput").ap()
            dbg_out[name] = t
            return t
        return nc.dram_tensor(name, shape, dt, kind="Internal").ap()

    f_dram = scratch("f_dram", [S, 512], BF16)
    qT_dram = scratch("qT_dram", [8, 128, S], BF16)
    kT_dram = scratch("kT_dram", [2, 128, S], BF16)
    v_dram = scratch("v_dram", [S, 256], BF16)
    gT_dram = scratch("gT_dram", [2048, S], BF16)
    Ys_dram = scratch("Ys_dram", [2, 64, 128, 512], BF16)
    XT_dram = scratch("XT_dram", [1024, S], BF16)
    OT_dram = scratch("OT_dram", [1024, S], BF16)
    x1_dram = scratch("x1_dram", [S, D], F32)
    u2_dram = scratch("u2_dram", [S, D], BF16)
    xs_dram = scratch("xs_dram", [NPAD, D], BF16)
    ys_dram = scratch("ys_dram", [NPAD, D], BF16)
    wgu_bf = scratch("wgu_bf", [NE * 128, 8 * 2 * D], BF16)
    wd_bf = scratch("wd_bf", [NE * 128, 8 * D], BF16)
    bgu_bf = scratch("bgu_bf", [NE, 2 * D], BF16)
    bd_bf = scratch("bd_bf", [NE, D], BF16)
    dbg6 = None
    if "dbg6" in debug.get("dump", ()):
        dbg6 = {"L": nc.dram_tensor("d6_L", [S, NE], F32, kind="ExternalOutput").ap(),
                "dest": nc.dram_tensor("d6_dest", [128, NT * 4], I32, kind="ExternalOutput").ap(),
                "blk": nc.dram_tensor("d6_blk", [1, NBLK], I32, kind="ExternalOutput").ap(),
                "chg": nc.dram_tensor("d6_chg", [1, NBLK], I32, kind="ExternalOutput").ap(),
                "w4": nc.dram_tensor("d6_w4", [128, NT * 4], F32, kind="ExternalOutput").ap()}
    mod_dump = scratch("mod_dump", [128, 6 * D], F32) if "mod_dump" in debug.get("dump", ()) else None

    with ExitStack() as top:
        kb = KB(nc, top)

        def T(es, name, shape, dt):
            return es.enter_context(nc.sbuf_tensor(name, shape, dt))

        def P(es, name, shape, dt):
            return es.enter_context(nc.psum_tensor(name, shape, dt))

        mod_bc = T(top, "mod_bc", [128, 6 * D], F32)
        b_mod = Buf("mod_bc")
        ident = T(top, "ident", [128, 128], BF16)
        b_ident = Buf("ident")
        kb.op("pool", lambda e: e.memset(ident[:], 0.0), writes=[b_ident])
        kb.op("pool", lambda e: e.affine_select(out=ident[:], in_=ident[:], pattern=[[-1, 128]], compare_op=ALU.not_equal,
                                                fill=1.0, base=0, channel_multiplier=1), writes=[b_ident])
        mhalf = T(top, "mhalf", [128, 16], F32)
        b_mhalf = Buf("mhalf")
        kb.op("pool", lambda e: e.memset(mhalf[:], -0.5), writes=[b_mhalf])

        with ExitStack() as es:
            c_sb = T(es, "c_sb", [128, 8], F32); cond = T(es, "cond", [128, 8], F32)
            cond_bc = T(es, "cond_bc", [128, 8, 128], F32)
            wblk = [T(es, f"wblk{i}", [128, 8, 512], F32) for i in range(2)]
            bada = T(es, "bada", [1, 6 * D], F32); ones1 = T(es, "ones1", [1, 128], F32)
            ps0 = [P(es, f"ps0_{i}", [128, 512], F32) for i in range(2)]
            b_c = Buf(); b_cond = Buf(); b_cbc = Buf(); b_wblk = [Buf(), Buf()]; b_bada = Buf(); b_ones1 = Buf(); b_ps0 = [Buf(), Buf()]
            s_c = kb.sem("p0c"); s_w = [kb.sem("p0w0"), kb.sem("p0w1")]
            kb.op("sp", lambda e: e.dma_start(out=c_sb[:], in_=I["c_fm"][:, :]), writes=[b_c], dsem=s_c)
            kb.op("sp", lambda e: e.dma_start(out=bada[:], in_=I["b_ada"][:, :]), writes=[b_bada], dsem=s_c)
            kb.op("pool", lambda e: e.memset(ones1[:], 1.0), writes=[b_ones1])
            kb.op("act", lambda e: e.activation(out=cond[:], in_=c_sb[:], func=AF.Silu), reads=[b_c], writes=[b_cond])
            kb.op("dve", lambda e: e.tensor_copy(out=cond_bc[:], in_=cond[:].unsqueeze(2).to_broadcast([128, 8, 128])),
                  reads=[b_cond], writes=[b_cbc])
            w_ada_v = I["w_ada"].rearrange("(dc p) c -> p dc c", p=128)
            for cb in range(12):
                sl = cb % 2
                kb.op("sp", lambda e: e.dma_start(out=wblk[sl][:], in_=w_ada_v[:, :, cb * 512:(cb + 1) * 512]),
                      writes=[b_wblk[sl]], dsem=s_w[sl])
                fns = []
                for dc in range(8):
                    fns.append(lambda e, dc=dc: e.matmul(ps0[sl][:], lhsT=cond_bc[:, dc, :], rhs=wblk[sl][:, dc, :],
                                                         start=(dc == 0), stop=False))
                fns.append(lambda e: e.matmul(ps0[sl][:], lhsT=ones1[0:1, :], rhs=bada[0:1, cb * 512:(cb + 1) * 512],
                                              start=False, stop=True))
                kb.group("pe", fns, reads=[b_cbc, b_wblk[sl], b_ones1, b_bada], writes=[b_ps0[sl]])
                addv = 1.0 if (cb // 2) in (1, 4) else 0.0
                kb.op("dve", lambda e: e.tensor_scalar(out=mod_bc[:, cb * 512:(cb + 1) * 512], in0=ps0[sl][:], scalar1=addv,
                                                       scalar2=None, op0=ALU.add), reads=[b_ps0[sl]], writes=[b_mod])
            if mod_dump is not None:
                s_d = kb.sem("p0d")
                kb.op("sp", lambda e: e.dma_start(out=mod_dump[:, :], in_=mod_bc[:]), reads=[b_mod], dsem=s_d)
            kb.barrier()
        SH1, SC1, G1, SH2, SC2, G2 = [mod_bc[:, i * D:(i + 1) * D] for i in range(6)]

        def ln_tile_g(src, b_src, dst, b_dst, st_, b_st_, mv_, b_mv_, rstd_, b_rstd_, eps, mul_ap, add_ap, b_aff, rstd_on_act=False):
            kb.op("dve", lambda e: e.bn_stats(out=st_[:, 0, :], in_=src[:, 0:512]), reads=[b_src], writes=[b_st_]); yield
            kb.op("dve", lambda e: e.bn_stats(out=st_[:, 1, :], in_=src[:, 512:1024]), reads=[b_src], writes=[b_st_]); yield
            kb.op("dve", lambda e: e.bn_aggr(out=mv_[:], in_=st_[:].rearrange("p a b -> p (a b)")), reads=[b_st_], writes=[b_mv_]); yield
            if rstd_on_act:
                kb.op("dve", lambda e: e.tensor_scalar(out=rstd_[:], in0=mv_[:, 1:2], scalar1=float(eps), scalar2=None, op0=ALU.add),
                      reads=[b_mv_], writes=[b_rstd_]); yield
                kb.op("act", lambda e: e.activation(out=rstd_[:], in_=rstd_[:], func=AF.Sqrt), reads=[b_rstd_], writes=[b_rstd_]); yield
                kb.op("dve", lambda e: e.reciprocal(out=rstd_[:], in_=rstd_[:]), reads=[b_rstd_], writes=[b_rstd_]); yield
            else:
                kb.op("pool", lambda e: e.tensor_scalar(out=rstd_[:], in0=mv_[:, 1:2], scalar1=1.0, scalar2=float(eps), op0=ALU.mult, op1=ALU.add),
                      reads=[b_mv_], writes=[b_rstd_]); yield
                kb.op("pool", lambda e: e.tensor_tensor(out=rstd_[:], in0=rstd_[:], in1=mhalf[:, 0:1], op=ALU.pow), reads=[b_rstd_, b_mhalf], writes=[b_rstd_]); yield
            kb.op("dve", lambda e: e.tensor_scalar(out=src[:], in0=src[:], scalar1=mv_[:, 0:1], scalar2=rstd_[:, 0:1], op0=ALU.subtract, op1=ALU.mult),
                  reads=[b_src, b_mv_, b_rstd_], writes=[b_src]); yield
            kb.op("dve", lambda e: e.tensor_tensor(out=src[:], in0=src[:], in1=mul_ap, op=ALU.mult), reads=[b_src, b_aff], writes=[b_src]); yield
            kb.op("dve", lambda e: e.tensor_tensor(out=dst[:], in0=src[:], in1=add_ap, op=ALU.add), reads=[b_src, b_aff], writes=[b_dst]); yield

        def ln_tile(src, b_src, dst, b_dst, st_, b_st_, mv_, b_mv_, rstd_, b_rstd_, eps, mul_ap, add_ap, b_aff, mul_first_pool):
            kb.op("dve", lambda e: e.bn_stats(out=st_[:, 0, :], in_=src[:, 0:512]), reads=[b_src], writes=[b_st_])
            kb.op("dve", lambda e: e.bn_stats(out=st_[:, 1, :], in_=src[:, 512:1024]), reads=[b_src], writes=[b_st_])
            kb.op("dve", lambda e: e.bn_aggr(out=mv_[:], in_=st_[:].rearrange("p a b -> p (a b)")), reads=[b_st_], writes=[b_mv_])
            kb.op("pool", lambda e: e.tensor_scalar(out=rstd_[:], in0=mv_[:, 1:2], scalar1=1.0, scalar2=float(eps), op0=ALU.mult, op1=ALU.add),
                  reads=[b_mv_], writes=[b_rstd_])
            kb.op("pool", lambda e: e.tensor_tensor(out=rstd_[:], in0=rstd_[:], in1=mhalf[:, 0:1], op=ALU.pow), reads=[b_rstd_, b_mhalf], writes=[b_rstd_])
            kb.op("dve", lambda e: e.tensor_scalar(out=src[:], in0=src[:], scalar1=mv_[:, 0:1], scalar2=rstd_[:, 0:1], op0=ALU.subtract, op1=ALU.mult),
                  reads=[b_src, b_mv_, b_rstd_], writes=[b_src])
            kb.op("dve", lambda e: e.tensor_tensor(out=src[:], in0=src[:], in1=mul_ap, op=ALU.mult), reads=[b_src, b_aff], writes=[b_src])
            kb.op("dve", lambda e: e.tensor_tensor(out=dst[:], in0=src[:], in1=add_ap, op=ALU.add), reads=[b_src, b_aff], writes=[b_dst])

        if stop_after >= 1:
          with ExitStack() as es:
            win = T(es, "win", [128, 8, 4096], BF16); b_win = Buf()
            xt = [T(es, f"xt{i}", [128, D], F32) for i in range(2)]; b_xt = [Buf(), Buf()]
            tab = [T(es, f"tab{i}", [128, 256], F32) for i in range(2)]; b_tab = [Buf(), Buf()]
            st_ = [T(es, f"st{i}", [128, 2, 6], F32) for i in range(2)]; mv_ = [T(es, f"mv{i}", [128, 2], F32) for i in range(2)]; rstd_ = [T(es, f"rstd{i}", [128, 1], F32) for i in range(2)]
            b_st_ = [Buf(), Buf()]; b_mv_ = [Buf(), Buf()]; b_rstd_ = [Buf(), Buf()]
            xn_ = [T(es, f"xn{i}", [128, D], F32) for i in range(2)]; b_xn_ = [Buf(), Buf()]
            ub = [T(es, f"ub{i}", [128, D], BF16) for i in range(2)]; b_ub = [Buf(), Buf()]
            uT = [T(es, f"uT{i}", [128, 8, 512], BF16) for i in range(2)]; b_uT = [Buf(), Buf()]
            qk_ = [T(es, f"qk{i}", [128, 1280], F32) for i in range(2)]; b_qk_ = [Buf(), Buf()]

            ss_ = [T(es, f"ss{i}", [128, 10], F32) for i in range(2)]; b_ss_ = [Buf(), Buf()]
            rs_ = [T(es, f"rs{i}", [128, 10], F32) for i in range(2)]; b_rs_ = [Buf(), Buf()]
            qn_ = [T(es, f"qn{i}", [128, 1280], F32) for i in range(2)]; b_qn_ = [Buf(), Buf()]
            t1_ = [T(es, f"t1{i}", [128, 1280], F32) for i in range(2)]; b_t1_ = [Buf(), Buf()]
            t2_ = [T(es, f"t2{i}", [128, 1280], F32) for i in range(2)]; b_t2_ = [Buf(), Buf()]
            rot_ = [T(es, f"rot{i}", [128, 1280], BF16) for i in range(2)]; b_rot_ = [Buf(), Buf()]
            gain = T(es, "gain", [128, 1280], F32); b_gain = Buf()
            fst = [T(es, f"fst{i}", [128, 512], BF16) for i in range(2)]; b_fst = [Buf(), Buf()]
            vst = [T(es, f"vst{i}", [128, 256], BF16) for i in range(2)]; b_vst = [Buf(), Buf()]
            qTs_ = [T(es, f"qTs{i}", [128, 8, 512], BF16) for i in range(2)]; b_qTs_ = [Buf(), Buf()]
            kTs_ = [T(es, f"kTs{i}", [128, 2, 512], BF16) for i in range(2)]; b_kTs_ = [Buf(), Buf()]
            gst = [T(es, f"gst{i}", [128, 4, 512], BF16) for i in range(2)]; b_gst = [Buf(), Buf()]
            pst = P(es, "pst", [128, 8, 128], BF16); b_pst = Buf()
            psz = P(es, "psz", [128, 2048], F32); b_psz = [Buf() for _ in range(4)]
            pstq = P(es, "pstq", [128, 8, 128], BF16); b_pstq = Buf()
            pstk = P(es, "pstk", [128, 2, 128], BF16); b_pstk = Buf()
            psg = P(es, "psg", [128, 512], F32); b_psg = Buf()
            s_win = kb.sem("p1win"); s_x = [kb.sem("p1x0"), kb.sem("p1x1")]; s_g = kb.sem("p1g")
            s_f = [kb.sem("p1f0"), kb.sem("p1f1")]; s_v = [kb.sem("p1v0"), kb.sem("p1v1")]
            s_q = kb.sem("p1q"); s_k = kb.sem("p1k"); s_gs = [kb.sem("p1gs0"), kb.sem("p1gs1")]
            w_in_v = I["w_in"].rearrange("(dc p) c -> p dc c", p=128)
            for dc in range(8):
                for hh in range(2):
                    kb.op("pool", lambda e, dc=dc, hh=hh: e.dma_start(out=win[:, dc, hh * 2048:(hh + 1) * 2048],
                                                                      in_=w_in_v[:, dc, hh * 2048:(hh + 1) * 2048]),
                          writes=[b_win], dsem=s_win)
            for h in range(10):
                src = I["q_norm_g"] if h < 8 else I["k_norm_g"]
                kb.op("sp", lambda e, h=h, src=src: e.dma_start(out=gain[:, h * 128:(h + 1) * 128],
                                                               in_=src[0:1, :].partition_broadcast(128)),
                      writes=[b_gain], dsem=s_g)
            kb.op("dve", lambda e: e.tensor_scalar(out=gain[:, 0:1024], in0=gain[:, 0:1024], scalar1=float(128.0 ** -0.5),
                                                   scalar2=None, op0=ALU.mult), reads=[b_gain], writes=[b_gain])
            x_v = I["x"].rearrange("(t p) d -> t p d", p=128)
            tab_v = I["rope_tab"].rearrange("(t p) d -> t p d", p=128)
            qT_v = qT_dram.rearrange("h d s -> d h s")
            kT_v = kT_dram.rearrange("h d s -> d h s")
            gT_v = gT_dram.rearrange("(g p) s -> p g s", p=128)

            s_tb = [kb.sem("p1t0"), kb.sem("p1t1")]

            def load_xt(t):
                sl = t % 2
                kb.op("sp", lambda e: e.dma_start(out=xt[sl][:], in_=x_v[t]), writes=[b_xt[sl]], dsem=s_x[sl])

            def load_tab(t):
                sl = t % 2
                kb.op("sp", lambda e: e.dma_start(out=tab[sl][:], in_=tab_v[t]), writes=[b_tab[sl]], dsem=s_tb[sl])

            load_xt(0); load_xt(1); load_tab(0); load_tab(1)
            def tile1(t):
                    sl = t % 2; blk = t // 4; sl4 = t % 4; ub_ = ub[sl]; uT_ = uT[blk % 2]
                    st = st_[sl]; mv = mv_[sl]; rstd = rstd_[sl]; b_st = b_st_[sl]; b_mv = b_mv_[sl]; b_rstd = b_rstd_[sl]
                    xn = xn_[sl]; b_xn = b_xn_[sl]; qk = qk_[sl]; b_qk = b_qk_[sl]; ss = ss_[sl]; b_ss = b_ss_[sl]; rs = rs_[sl]; b_rs = b_rs_[sl]
                    qn = qn_[sl]; b_qn = b_qn_[sl]; t1 = t1_[sl]; b_t1 = b_t1_[sl]; t2 = t2_[sl]; b_t2 = b_t2_[sl]; rot = rot_[sl]; b_rot = b_rot_[sl]
                    sq = t2; b_sq = b_t2
                    qTs = qTs_[blk % 2]; b_qTs = b_qTs_[blk % 2]; kTs = kTs_[blk % 2]; b_kTs = b_kTs_[blk % 2]
                    x_ = xt[sl]
                    kb.op("dve", lambda e: e.bn_stats(out=st[:, 0, :], in_=x_[:, 0:512]), reads=[b_xt[sl]], writes=[b_st])
                    yield
                    kb.op("dve", lambda e: e.bn_stats(out=st[:, 1, :], in_=x_[:, 512:1024]), reads=[b_xt[sl]], writes=[b_st])
                    yield
                    kb.op("dve", lambda e: e.bn_aggr(out=mv[:], in_=st[:].rearrange("p a b -> p (a b)")), reads=[b_st], writes=[b_mv])
                    yield
                    kb.op("pool", lambda e: e.tensor_scalar(out=rstd[:], in0=mv[:, 1:2], scalar1=1.0, scalar2=1e-6, op0=ALU.mult, op1=ALU.add),
                          reads=[b_mv], writes=[b_rstd])
                    yield
                    kb.op("pool", lambda e: e.tensor_tensor(out=rstd[:], in0=rstd[:], in1=mhalf[:, 0:1], op=ALU.pow),
                          reads=[b_rstd, b_mhalf], writes=[b_rstd])
                    yield
                    kb.op("dve", lambda e: e.tensor_scalar(out=xn[:], in0=x_[:], scalar1=mv[:, 0:1], scalar2=rstd[:, 0:1],
                                                           op0=ALU.subtract, op1=ALU.mult), reads=[b_xt[sl], b_mv, b_rstd], writes=[b_xn])
                    if t + 2 < NT:
                        load_xt(t + 2)
                    yield
                    kb.op("dve", lambda e: e.tensor_tensor(out=xn[:], in0=xn[:], in1=SC1, op=ALU.mult), reads=[b_xn, b_mod], writes=[b_xn])
                    yield
                    kb.op("dve", lambda e: e.tensor_tensor(out=ub_[:], in0=xn[:], in1=SH1, op=ALU.add), reads=[b_xn, b_mod], writes=[b_ub[sl]])
                    yield
                    kb.group("pe", [lambda e, dc=dc: e.transpose(out=pst[:, dc, :], in_=ub_[:, dc * 128:(dc + 1) * 128], identity=ident[:])
                                    for dc in range(8)], reads=[b_ub[sl], b_ident], writes=[b_pst])
                    kb.op("act", lambda e: e.copy(out=uT_[:, :, sl4 * 128:(sl4 + 1) * 128], in_=pst[:]), reads=[b_pst], writes=[b_uT[blk % 2]])
                    yield
                    for cc in range(4):
                        kb.group("pe", [lambda e, dc=dc, cc=cc: e.matmul(psz[:, cc * 512:(cc + 1) * 512], lhsT=uT_[:, dc, sl4 * 128:(sl4 + 1) * 128],
                                                                         rhs=win[:, dc, cc * 512:(cc + 1) * 512], start=(dc == 0), stop=(dc == 7))
                                        for dc in range(8)], reads=[b_uT[blk % 2], b_win], writes=[b_psz[cc]])
                    kb.op("act", lambda e: e.copy(out=fst[sl][:], in_=psz[:, 0:512]), reads=[b_psz[0]], writes=[b_fst[sl]])
                    kb.op("sp", lambda e: e.dma_start(out=f_dram[t * 128:(t + 1) * 128, :], in_=fst[sl][:]), reads=[b_fst[sl]], dsem=s_f[sl])
                    kb.op("act", lambda e: e.copy(out=vst[sl][:], in_=psz[:, 1792:2048]), reads=[b_psz[3]], writes=[b_vst[sl]])
                    kb.op("sp", lambda e: e.dma_start(out=v_dram[t * 128:(t + 1) * 128, :], in_=vst[sl][:]), reads=[b_vst[sl]], dsem=s_v[sl])
                    kb.op("act", lambda e: e.copy(out=qk[:], in_=psz[:, 512:1792]), reads=[b_psz[1], b_psz[2], b_psz[3]], writes=[b_qk])
                    yield
                    kb.op("dve", lambda e: e.tensor_tensor(out=sq[:], in0=qk[:], in1=qk[:], op=ALU.mult), reads=[b_qk], writes=[b_sq])
                    yield
                    kb.op("dve", lambda e: e.tensor_reduce(out=ss[:], in_=sq[:].rearrange("p (h d) -> p h d", d=128), axis=AX.X, op=ALU.add),
                          reads=[b_sq], writes=[b_ss])
                    yield
                    kb.op("pool", lambda e: e.tensor_scalar(out=rs[:], in0=ss[:], scalar1=1.0 / 128.0, scalar2=1e-6, op0=ALU.mult, op1=ALU.add),
                          reads=[b_ss], writes=[b_rs])
                    yield
                    kb.op("pool", lambda e: e.tensor_tensor(out=rs[:], in0=rs[:], in1=mhalf[:, 0:10], op=ALU.pow), reads=[b_rs, b_mhalf], writes=[b_rs])
                    yield
                    kb.op("dve", lambda e: e.tensor_tensor(out=qn[:].rearrange("p (h d) -> p h d", d=128), in0=qk[:].rearrange("p (h d) -> p h d", d=128),
                                                           in1=rs[:].unsqueeze(2).to_broadcast([128, 10, 128]), op=ALU.mult),
                          reads=[b_qk, b_rs], writes=[b_qn])
                    yield
                    kb.op("dve", lambda e: e.tensor_tensor(out=qn[:], in0=qn[:], in1=gain[:], op=ALU.mult), reads=[b_qn, b_gain], writes=[b_qn])
                    yield
                    tb_ = tab[sl]
                    kb.op("dve", lambda e: e.tensor_tensor(out=t1[:].rearrange("p (h d) -> p h d", d=128), in0=qn[:].rearrange("p (h d) -> p h d", d=128),
                                                           in1=tb_[:, 0:128].unsqueeze(1).to_broadcast([128, 10, 128]), op=ALU.mult),
                          reads=[b_qn, b_tab[sl]], writes=[b_t1])
                    yield
                    qn5 = qn[:].rearrange("p (h a t d) -> p h a t d", h=10, a=2, t=2, d=32)
                    t25 = t2[:].rearrange("p (h a t d) -> p h a t d", h=10, a=2, t=2, d=32)
                    sn4 = tb_[:, 128:256].rearrange("p (a t d) -> p a t d", a=2, t=2, d=32)
                    for half in range(2):
                        kb.op("pool", lambda e, half=half: e.tensor_tensor(out=t25[:, :, :, half, :], in0=qn5[:, :, :, 1 - half, :],
                                                                           in1=sn4[:, :, half, :].unsqueeze(1).to_broadcast([128, 10, 2, 32]), op=ALU.mult),
                              reads=[b_qn, b_tab[sl]], writes=[b_t2])
                        yield
                    if t + 2 < NT:
                        load_tab(t + 2)
                    kb.op("dve", lambda e: e.tensor_tensor(out=rot[:], in0=t1[:], in1=t2[:], op=ALU.add), reads=[b_t1, b_t2], writes=[b_rot])
                    yield
                    kb.group("pe", [lambda e, h=h: e.transpose(out=pstq[:, h, :], in_=rot[:, h * 128:(h + 1) * 128], identity=ident[:]) for h in range(8)],
                             reads=[b_rot, b_ident], writes=[b_pstq])
                    kb.group("pe", [lambda e, h=h: e.transpose(out=pstk[:, h, :], in_=rot[:, (8 + h) * 128:(9 + h) * 128], identity=ident[:]) for h in range(2)],
                             reads=[b_rot, b_ident], writes=[b_pstk])
                    kb.op("act", lambda e: e.copy(out=qTs[:, :, sl4 * 128:(sl4 + 1) * 128], in_=pstq[:]), reads=[b_pstq], writes=[b_qTs])
                    kb.op("act", lambda e: e.copy(out=kTs[:, :, sl4 * 128:(sl4 + 1) * 128], in_=pstk[:]), reads=[b_pstk], writes=[b_kTs])
                    yield
                    if sl4 == 3:
                        kb.op("sp", lambda e: e.dma_start(out=qT_v[:, :, blk * 512:(blk + 1) * 512], in_=qTs[:]), reads=[b_qTs], dsem=s_q)
                        yield
                        kb.op("sp", lambda e: e.dma_start(out=kT_v[:, :, blk * 512:(blk + 1) * 512], in_=kTs[:]), reads=[b_kTs], dsem=s_k)
                        yield
                        for gq in range(4):
                            gsl = gq % 2
                            for gi in range(4):
                                gc = gq * 4 + gi
                                kb.group("pe", [lambda e, dc=dc, gc=gc: e.matmul(psg[:], lhsT=win[:, dc, 2048 + gc * 128:2048 + (gc + 1) * 128],
                                                                                 rhs=uT_[:, dc, :], start=(dc == 0), stop=(dc == 7)) for dc in range(8)],
                                         reads=[b_uT[blk % 2], b_win], writes=[b_psg])
                                kb.op("act", lambda e, gi=gi: e.activation(out=gst[gsl][:, gi, :], in_=psg[:], func=AF.Sigmoid),
                                      reads=[b_psg], writes=[b_gst[gsl]])
                                yield
                            kb.op("sp", lambda e, gq=gq: e.dma_start(out=gT_v[:, gq * 4:(gq + 1) * 4, blk * 512:(blk + 1) * 512], in_=gst[gsl][:]),
                                  reads=[b_gst[gsl]], dsem=s_gs[gsl])
                            yield
            run_interleaved((tile1(t) for t in range(NT)), debug.get("il1", 2))
            kb.barrier()


        if stop_after >= 2:
          with ExitStack() as es:
            E = T(es, "fftE", [128, 64, 2, 128], BF16); b_E = Buf()
            f1 = [T(es, f"f1_{i}", [128, 8, 512], BF16) for i in range(2)]; b_f1 = [Buf(), Buf()]
            Ysb = [T(es, f"Ysb{i}", [128, 8, 2, 512], BF16) for i in range(2)]; b_Ysb = [Buf(), Buf()]
            psY = [P(es, f"psY{i}", [128, 512], F32) for i in range(4)]; b_psY = [Buf() for _ in range(4)]
            s_E = kb.sem("p2E"); s_f1 = [kb.sem("p2f0"), kb.sem("p2f1")]; s_y = [kb.sem("p2y0"), kb.sem("p2y1")]
            kb.op("sp", lambda e: e.dma_start(out=E[:], in_=I["fft_e"][:, :, :, :]), writes=[b_E], dsem=s_E)
            f1_v = f_dram.rearrange("(p n) c -> p n c", n=64)
            cnt = 0
            for ch in range(8):
                sl = ch % 2
                kb.op("sp", lambda e: e.dma_start(out=f1[sl][:], in_=f1_v[:, ch * 8:(ch + 1) * 8, :]), writes=[b_f1[sl]], dsem=s_f1[sl])
                for j in range(8):
                    n2 = ch * 8 + j
                    for ri in range(2):
                        pb = cnt % 4; cnt += 1
                        kb.op("pe", lambda e: e.matmul(psY[pb][:], lhsT=E[:, n2, ri, :], rhs=f1[sl][:, j, :], start=True, stop=True),
                              reads=[b_E, b_f1[sl]], writes=[b_psY[pb]])
                        if ri == 0:
                            kb.op("act", lambda e: e.copy(out=Ysb[sl][:, j, ri, :], in_=psY[pb][:]), reads=[b_psY[pb]], writes=[b_Ysb[sl]])
                        else:
                            kb.op("dve", lambda e: e.tensor_copy(out=Ysb[sl][:, j, ri, :], in_=psY[pb][:]), reads=[b_psY[pb]], writes=[b_Ysb[sl]])
                for ri in range(2):
                    kb.op("sp", lambda e, ri=ri: e.dma_start(out=Ys_dram[ri, ch * 8:(ch + 1) * 8].rearrange("n k c -> k n c"), in_=Ysb[sl][:, :, ri, :]),
                          reads=[b_Ysb[sl]], dsem=s_y[sl])
            kb.barrier()
          with ExitStack() as es:
            W3 = T(es, "W3", [128, 128], BF16); b_W3 = Buf()
            Y2 = [T(es, f"Y2_{i}", [128, 8, 256], BF16) for i in range(2)]; b_Y2 = [Buf(), Buf()]
            XT = T(es, "XT", [128, 2, 2, S], BF16); b_XT = Buf()
            psX = [P(es, f"psX{i}", [128, 4, 128], F32) for i in range(2)]; b_psX = [Buf(), Buf()]
            s_w3 = kb.sem("p2w3"); s_y2 = [kb.sem("p2y20"), kb.sem("p2y21")]; s_xo = kb.sem("p2xo")
            kb.op("sp", lambda e: e.dma_start(out=W3[:], in_=I["fft_w3"][:, :]), writes=[b_W3], dsem=s_w3)
            Y2_v = Ys_dram.rearrange("r n k c -> (r n) k c")
            cnt = 0
            for pp in range(2):
                for kc in range(16):
                    sl = kc % 2
                    kb.op("sp", lambda e: e.dma_start(out=Y2[sl][:], in_=Y2_v[:, kc * 8:(kc + 1) * 8, pp * 256:(pp + 1) * 256]),
                          writes=[b_Y2[sl]], dsem=s_y2[sl])
                    for ccl in range(2):
                        for g in range(2):
                            pb = cnt % 2; cnt += 1
                            kb.group("pe", [lambda e, j=j: e.matmul(psX[pb][:, j, :], lhsT=Y2[sl][:, g * 4 + j, ccl * 128:(ccl + 1) * 128], rhs=W3[:],
                                                                    start=True, stop=True) for j in range(4)],
                                     reads=[b_Y2[sl], b_W3], writes=[b_psX[pb]])
                            k10 = kc * 8 + g * 4
                            o_ap = XT[:, ccl, :, :].rearrange("c x (k2 k1) -> c x k2 k1", k1=128)[:, :, :, k10:k10 + 4]
                            i_ap = psX[pb][:].rearrange("c j (x k2) -> c x k2 j", x=2)
                            if cnt % 2 == 0:
                                kb.op("act", lambda e: e.copy(out=o_ap, in_=i_ap), reads=[b_psX[pb]], writes=[b_XT])
                            else:
                                kb.op("dve", lambda e: e.tensor_copy(out=o_ap, in_=i_ap), reads=[b_psX[pb]], writes=[b_XT])
                for ccl in range(2):
                    for xr in range(2):
                        r0 = xr * 512 + (pp * 2 + ccl) * 128
                        kb.op("sp", lambda e, ccl=ccl, xr=xr, r0=r0: e.dma_start(out=XT_dram[r0:r0 + 128, :], in_=XT[:, ccl, xr, :]),
                              reads=[b_XT], dsem=s_xo)
            kb.barrier()

        if stop_after >= 4:
          with ExitStack() as es:
            kT = T(es, "kT", [128, 2, S], BF16); b_kT = Buf()
            V = T(es, "V", [128, NT, 256], BF16); b_V = Buf()
            ones = T(es, "ones", [128, 128], BF16); b_ones = Buf()
            gqk = T(es, "gqk", [128, 2, 128], F32); b_gqk = Buf()
            gmx = T(es, "gmx", [128, 2], F32); b_gmx = Buf()
            nbias = T(es, "nbias", [128, 1], F32); b_nb = Buf()
            qb = [T(es, f"qb{i}", [128, 8, 512], BF16) for i in range(2)]; b_qb = [Buf(), Buf()]
            PT = [T(es, f"PT{i}", [128, 1024], BF16) for i in range(4)]; b_PT = [Buf() for _ in range(4)]
            acc1 = T(es, "acc1", [128, 512], F32); b_acc1 = Buf()
            Osb = T(es, "Osb", [128, 512], F32); b_Osb = Buf()
            ones32 = T(es, "ones32", [128, 32], BF16); b_ones32 = Buf()
            kb.op("pool", lambda e: e.memset(ones32[:], 1.0), writes=[b_ones32])
            onesF = T(es, "onesF", [128, 128], F32); b_onesF = Buf()
            rec = T(es, "rec", [128, 512], F32); b_rec = Buf()
            OTs = [T(es, f"OTs{i}", [128, 512], BF16) for i in range(2)]; b_OTs = [Buf(), Buf()]
            psS = [P(es, f"psS{i}", [128, 1024], F32) for i in range(3)]; b_psS = [Buf() for _ in range(3)]
            psO = [P(es, "psO0", [128, 512], F32)]; b_psO = [Buf()]
            psL = [P(es, "psL0", [128, 512], F32)]; b_psL = [Buf()]
            s_kv = kb.sem("p4kv"); s_qb = [kb.sem("p4q0"), kb.sem("p4q1")]; s_o = [kb.sem("p4o0"), kb.sem("p4o1")]; s_gq = kb.sem("p4g")
            kb.op("sp", lambda e: e.dma_start(out=kT[:], in_=kT_dram.rearrange("h d s -> d h s")), writes=[b_kT], dsem=s_kv)
            kb.op("sp", lambda e: e.dma_start(out=V[:], in_=v_dram.rearrange("(t p) c -> p t c", p=128)), writes=[b_V], dsem=s_kv)
            kb.op("pool", lambda e: e.memset(onesF[:], 1.0 / 32.0), writes=[b_onesF])
            kb.op("sp", lambda e: e.dma_start(out=gqk[:, 0, :], in_=I["q_norm_g"][0:1, :].partition_broadcast(128)), writes=[b_gqk], dsem=s_gq)
            kb.op("sp", lambda e: e.dma_start(out=gqk[:, 1, :], in_=I["k_norm_g"][0:1, :].partition_broadcast(128)), writes=[b_gqk], dsem=s_gq)
            gmn = T(es, "gmn", [128, 2], F32); b_gmn = Buf()
            kb.op("dve", lambda e: e.tensor_reduce(out=gmx[:], in_=gqk[:], axis=AX.X, op=ALU.max), reads=[b_gqk], writes=[b_gmx])
            kb.op("dve", lambda e: e.tensor_reduce(out=gmn[:], in_=gqk[:], axis=AX.X, op=ALU.min, negate=True), reads=[b_gqk], writes=[b_gmn])
            kb.op("dve", lambda e: e.tensor_tensor(out=gmx[:], in0=gmx[:], in1=gmn[:], op=ALU.max), reads=[b_gmx, b_gmn], writes=[b_gmx])
            kb.op("dve", lambda e: e.tensor_tensor(out=nbias[:], in0=gmx[:, 0:1], in1=gmx[:, 1:2], op=ALU.mult), reads=[b_gmx], writes=[b_nb])
            kb.op("dve", lambda e: e.tensor_scalar(out=nbias[:], in0=nbias[:], scalar1=float(-(128.0 ** 0.5)), scalar2=None, op0=ALU.mult),
                  reads=[b_nb], writes=[b_nb])
            qT_v2 = qT_dram.rearrange("h d s -> d h s")
            kb.op("sp", lambda e: e.dma_start(out=qb[0][:], in_=qT_v2[:, :, 0:512]), writes=[b_qb[0]], dsem=s_qb[0])
            pc_jobs = []
            if stop_after >= 7:
                stg = [T(es, f"stg{i}", [128, 4096], F32) for i in range(2)]; b_stg = [Buf(), Buf()]
                stb = [T(es, f"stb{i}", [128, 4096], BF16) for i in range(2)]; b_stb = [Buf(), Buf()]
                s_ci = [kb.sem("p3i0"), kb.sem("p3i1")]; s_co = [kb.sem("p3o0"), kb.sem("p3o1")]
                for ex in range(NE):
                    gsrc = I["w_gate_up"][ex].rearrange("(r p) f -> p r f", p=128)
                    dsrc = I["w_down"][ex].rearrange("(r p) f -> p r f", p=128)
                    for q in range(4):
                        pc_jobs.append((gsrc[:, q * 2:(q + 1) * 2, :], wgu_bf[ex * 128:(ex + 1) * 128, q * 4096:(q + 1) * 4096], 2, 128, 4096))
                    for q in range(2):
                        pc_jobs.append((dsrc[:, q * 4:(q + 1) * 4, :], wd_bf[ex * 128:(ex + 1) * 128, q * 4096:(q + 1) * 4096], 4, 128, 4096))
                pc_jobs.append((I["b_down"][:, :], bd_bf[:, :], 1, NE, D))
            pc_state = [0]

            def precast_step(n):
                for _ in range(n):
                    ci = pc_state[0]
                    if ci >= len(pc_jobs):
                        return
                    src_ap, dst_ap, nr, npart, w_ = pc_jobs[ci]
                    sl = ci % 2
                    o_ap = stg[sl][0:npart, 0:w_]
                    if nr > 1:
                        o_ap = o_ap.rearrange("p (r f) -> p r f", r=nr)
                    kb.op("sp", lambda e: e.dma_start(out=o_ap, in_=src_ap), writes=[b_stg[sl]], dsem=s_ci[sl])
                    ce = ("dve", "pool")[ci % 2]
                    kb.op(ce, lambda e: e.tensor_copy(out=stb[sl][0:npart, 0:w_], in_=stg[sl][0:npart, 0:w_]), reads=[b_stg[sl]], writes=[b_stb[sl]])
                    kb.op("sp", lambda e: e.dma_start(out=dst_ap, in_=stb[sl][0:npart, 0:w_]), reads=[b_stb[sl]], dsem=s_co[sl])
                    pc_state[0] += 1
            i_u = 0
            nblk4 = debug.get("p4_blocks", NB)
            NU = NT // 2
            for blk in range(nblk4):
                bs = blk % 2
                if blk + 1 < nblk4:
                    kb.op("sp", lambda e: e.dma_start(out=qb[1 - bs][:], in_=qT_v2[:, :, (blk + 1) * 512:(blk + 2) * 512]),
                          writes=[b_qb[1 - bs]], dsem=s_qb[1 - bs])
                for h in range(8):
                    kvh = h // 4
                    base = i_u

                    def emit_S(u):
                        ii = base + u
                        kb.group("pe", [lambda e, c=c: e.matmul(psS[ii % 3][:, c * 512:(c + 1) * 512], lhsT=kT[:, kvh, (2 * u + c) * 128:(2 * u + c + 1) * 128],
                                                                rhs=qb[bs][:, h, :], start=True, stop=True) for c in range(2)],
                                 reads=[b_kT, b_qb[bs]], writes=[b_psS[ii % 3]])
                    emit_S(0); emit_S(1)
                    for u in range(NU):
                        ii = base + u
                        pt = PT[ii % 4]
                        kb.op("act", lambda e: e.activation(out=pt[:], in_=psS[ii % 3][:], func=AF.Exp, bias=nbias[:, 0:1], scale=1.0),
                              reads=[b_psS[ii % 3], b_nb], writes=[b_PT[ii % 4]])
                        if u + 2 < NU:
                            emit_S(u + 2)
                        first = (u == 0); last = (u == NU - 1)
                        kb.group("pe", [lambda e, c=c: e.matmul(psO[0][:], lhsT=V[:, 2 * u + c, kvh * 128:(kvh + 1) * 128], rhs=pt[:, c * 512:(c + 1) * 512],
                                                                start=(first and c == 0), stop=(last and c == 1)) for c in range(2)],
                                 reads=[b_V, b_PT[ii % 4]], writes=([b_psO[0]] if (first or last) else []))
                        if u % 2 == 1:
                            ptp = PT[(ii - 1) % 4]
                            srcs = [ptp[:, 0:512], ptp[:, 512:1024], pt[:, 0:512], pt[:, 512:1024]]
                            kb.group("pe", [lambda e, jc=jc: e.matmul(psL[0][32 * jc:32 * (jc + 1), :], lhsT=ones32[:, :], rhs=srcs[jc],
                                                                      start=(u == 1), stop=(u == NU - 1), tile_position=(0, 32 * jc)) for jc in range(4)],
                                     reads=[b_ones32, b_PT[(ii - 1) % 4], b_PT[ii % 4]], writes=([b_psL[0]] if (u == 1 or u == NU - 1) else []))
                    i_u += NU
                    kb.op("dve", lambda e: e.tensor_copy(out=acc1[:], in_=psL[0][:]), reads=[b_psL[0]], writes=[b_acc1])
                    kb.op("dve", lambda e: e.tensor_copy(out=Osb[:], in_=psO[0][:]), reads=[b_psO[0]], writes=[b_Osb])
                    kb.op("pe", lambda e: e.matmul(psL[0][:], lhsT=onesF[:], rhs=acc1[:], start=True, stop=True), reads=[b_onesF, b_acc1], writes=[b_psL[0]])
                    jo = (blk * 8 + h) % 2
                    kb.op("dve", lambda e: e.reciprocal(out=rec[:], in_=psL[0][:]), reads=[b_psL[0]], writes=[b_rec])
                    kb.op("dve", lambda e: e.tensor_tensor(out=OTs[jo][:], in0=Osb[:], in1=rec[:], op=ALU.mult), reads=[b_Osb, b_rec], writes=[b_OTs[jo]])
                    kb.op("sp", lambda e: e.dma_start(out=OT_dram[h * 128:(h + 1) * 128, blk * 512:(blk + 1) * 512], in_=OTs[jo][:]),
                          reads=[b_OTs[jo]], dsem=s_o[jo])
                    precast_step(2)
            precast_step(len(pc_jobs))
            kb.barrier()

        if stop_after >= 5:
          with ExitStack() as es:
            Wf = T(es, "Wf", [128, 8, D], BF16); b_Wf = Buf()
            wao = T(es, "wao", [128, 8, D], BF16); b_wao = Buf()
            wout = T(es, "wout", [128, 8, D], BF16); b_wout = Buf()
            lng = T(es, "lng", [128, D], F32); lnb = T(es, "lnb", [128, D], F32); b_ln = Buf()
            s_w5 = kb.sem("p5w"); s_ln = kb.sem("p5ln")
            kb.op("sp", lambda e: e.dma_start(out=lng[:], in_=I["ln1_g"][0:1, :].partition_broadcast(128)), writes=[b_ln], dsem=s_ln)
            kb.op("sp", lambda e: e.dma_start(out=lnb[:], in_=I["ln1_b"][0:1, :].partition_broadcast(128)), writes=[b_ln], dsem=s_ln)
            wao_v = I["w_attn_o"].rearrange("(kc p) d -> p kc d", p=128); wout_v = I["w_out"].rearrange("(kc p) d -> p kc d", p=128)
            for kc in range(8):
                kb.op("pool", lambda e, kc=kc: e.dma_start(out=wao[:, kc, :], in_=wao_v[:, kc, :]), writes=[b_wao], dsem=s_w5)
                kb.op("pool", lambda e, kc=kc: e.dma_start(out=wout[:, kc, :], in_=wout_v[:, kc, :]), writes=[b_wout], dsem=s_w5)
            psF = [P(es, f"psF{i}", [128, 512], F32) for i in range(2)]; b_psF = [Buf(), Buf()]
            psA = [P(es, f"psA{i}", [128, 512], F32) for i in range(2)]; b_psA = [Buf(), Buf()]
            psH = P(es, "psH", [128, D], F32); b_psH = Buf()
            with ExitStack() as es2:
                wf_sb = T(es2, "wf_sb", [128, 4, D], BF16); b_wf = Buf()
                dftc = T(es2, "dftc", [128, 2, 128], BF16); b_dftc = Buf()
                s_f5 = kb.sem("p5f")
                kb.op("pool", lambda e: e.dma_start(out=wf_sb[:], in_=I["w_fourier"].rearrange("(g p) d -> p g d", p=128)), writes=[b_wf], dsem=s_f5)
                kb.op("sp", lambda e: e.dma_start(out=dftc[:], in_=I["dft_c"][:, :, :]), writes=[b_dftc], dsem=s_f5)
                cnt = 0
                for xr in range(2):
                    for g in range(4):
                        for half in range(2):
                            pb = cnt % 2; cnt += 1
                            kb.op("pe", lambda e: e.matmul(psF[pb][:], lhsT=dftc[:, xr, :], rhs=wf_sb[:, g, half * 512:(half + 1) * 512], start=True, stop=True),
                                  reads=[b_dftc, b_wf], writes=[b_psF[pb]])
                            kb.op("dve", lambda e: e.tensor_copy(out=Wf[:, xr * 4 + g, half * 512:(half + 1) * 512], in_=psF[pb][:]),
                                  reads=[b_psF[pb]], writes=[b_Wf])
                kb.barrier()
            XTb = [T(es, f"XTb{i}", [128, 8, 512], BF16) for i in range(2)]; b_XTb = [Buf(), Buf()]
            OTb = [T(es, f"OTb{i}", [128, 8, 512], BF16) for i in range(2)]; b_OTb = [Buf(), Buf()]
            gTb = [T(es, f"gTb{i}", [128, 16, 512], BF16) for i in range(2)]; b_gTb = [Buf(), Buf()]
            mT = T(es, "mT", [128, 8, 512], BF16); b_mT = Buf()
            ta = T(es, "ta", [128, 512], F32); tb2 = T(es, "tb2", [128, 512], F32); b_ta = Buf(); b_tb2 = Buf()
            x5 = [T(es, f"x5_{i}", [128, D], F32) for i in range(2)]; b_x5 = [Buf(), Buf()]
            r5 = T(es, "r5", [128, D], F32); b_r5 = Buf()
            st5 = T(es, "st5", [128, 2, 6], F32); mv5 = T(es, "mv5", [128, 2], F32); rstd5 = T(es, "rstd5", [128, 1], F32)
            b_st5 = Buf(); b_mv5 = Buf(); b_rstd5 = Buf()
            y5 = [T(es, f"y5_{i}", [128, D], F32) for i in range(2)]; b_y5 = [Buf(), Buf()]
            s_blk = [kb.sem("p5b0"), kb.sem("p5b1")]; s_x5 = [kb.sem("p5x0"), kb.sem("p5x1")]; s_y5 = [kb.sem("p5y0"), kb.sem("p5y1")]
            XT_v = XT_dram.rearrange("(kc p) s -> p kc s", p=128); OT_v = OT_dram.rearrange("(kc p) s -> p kc s", p=128)
            gT_v5 = gT_dram.rearrange("(g p) s -> p g s", p=128)
            x_v5 = I["x"].rearrange("(t p) d -> t p d", p=128); x1_v = x1_dram.rearrange("(t p) d -> t p d", p=128)

            def load_blk(tb):
                sl = tb % 2
                kb.op("sp", lambda e: e.dma_start(out=XTb[sl][:], in_=XT_v[:, :, tb * 512:(tb + 1) * 512]), writes=[b_XTb[sl]], dsem=s_blk[sl])
                kb.op("sp", lambda e: e.dma_start(out=OTb[sl][:], in_=OT_v[:, :, tb * 512:(tb + 1) * 512]), writes=[b_OTb[sl]], dsem=s_blk[sl])
                kb.op("sp", lambda e: e.dma_start(out=gTb[sl][:], in_=gT_v5[:, :, tb * 512:(tb + 1) * 512]), writes=[b_gTb[sl]], dsem=s_blk[sl])

            mT_ = [mT, T(es, "mT1", [128, 8, 512], BF16)]; b_mT_ = [b_mT, Buf()]
            ta_ = [ta, T(es, "ta1", [128, 512], F32)]; tb2_ = [tb2, T(es, "tb21", [128, 512], F32)]; b_ta_ = [b_ta, Buf()]; b_tb2_ = [b_tb2, Buf()]
            r5_ = [r5, T(es, "r5_1", [128, D], F32)]; b_r5_ = [b_r5, Buf()]
            st5_ = [st5, T(es, "st5_1", [128, 2, 6], F32)]; mv5_ = [mv5, T(es, "mv5_1", [128, 2], F32)]; rstd5_ = [rstd5, T(es, "rstd5_1", [128, 1], F32)]
            b_st5_ = [b_st5, Buf()]; b_mv5_ = [b_mv5, Buf()]; b_rstd5_ = [b_rstd5, Buf()]

            def gate5(tb):
                sl = tb % 2
                load_blk(tb)
                yield
                for Dc in range(8):
                    pb = Dc % 2
                    kb.group("pe", [lambda e, kc=kc: e.matmul(psF[pb][:], lhsT=Wf[:, kc, Dc * 128:(Dc + 1) * 128], rhs=XTb[sl][:, kc, :],
                                                              start=(kc == 0), stop=(kc == 7)) for kc in range(8)],
                             reads=[b_Wf, b_XTb[sl]], writes=[b_psF[pb]])
                    kb.group("pe", [lambda e, kc=kc: e.matmul(psA[pb][:], lhsT=wao[:, kc, Dc * 128:(Dc + 1) * 128], rhs=OTb[sl][:, kc, :],
                                                              start=(kc == 0), stop=(kc == 7)) for kc in range(8)],
                             reads=[b_wao, b_OTb[sl]], writes=[b_psA[pb]])
                    kb.op("dve", lambda e: e.tensor_tensor(out=ta_[pb][:], in0=psF[pb][:], in1=gTb[sl][:, Dc, :], op=ALU.mult),
                          reads=[b_psF[pb], b_gTb[sl]], writes=[b_ta_[pb]])
                    kb.op("dve", lambda e: e.tensor_tensor(out=tb2_[pb][:], in0=psA[pb][:], in1=gTb[sl][:, 8 + Dc, :], op=ALU.mult),
                          reads=[b_psA[pb], b_gTb[sl]], writes=[b_tb2_[pb]])
                    yield
                    kb.op("dve", lambda e: e.tensor_tensor(out=mT_[sl][:, Dc, :], in0=ta_[pb][:], in1=tb2_[pb][:], op=ALU.add),
                          reads=[b_ta_[pb], b_tb2_[pb]], writes=[b_mT_[sl]])
                    yield

            def tile5(tb, tt):
                sl = tb % 2; t = tb * 4 + tt; xs_ = t % 2
                r5t = r5_[xs_]; b_r5t = b_r5_[xs_]
                kb.op("sp", lambda e: e.dma_start(out=x5[xs_][:], in_=x_v5[t]), writes=[b_x5[xs_]], dsem=s_x5[xs_])
                yield
                for half in range(2):
                    kb.group("pe", [lambda e, Dc=Dc: e.matmul(psH[:, half * 512:(half + 1) * 512], lhsT=mT_[sl][:, Dc, tt * 128:(tt + 1) * 128],
                                                              rhs=wout[:, Dc, half * 512:(half + 1) * 512], start=(Dc == 0), stop=(Dc == 7)) for Dc in range(8)],
                             reads=[b_mT_[sl], b_wout], writes=[b_psH])
                kb.op("dve", lambda e: e.tensor_tensor(out=r5t[:], in0=psH[:], in1=G1, op=ALU.mult), reads=[b_psH, b_mod], writes=[b_r5t])
                yield
                kb.op("dve", lambda e: e.scalar_tensor_tensor(out=r5t[:], in0=x5[xs_][:], scalar=float(ALPHA), in1=r5t[:], op0=ALU.mult, op1=ALU.add),
                      reads=[b_x5[xs_], b_r5t], writes=[b_r5t])
                yield
                yield from ln_tile_g(r5t, b_r5t, y5[xs_], b_y5[xs_], st5_[xs_], b_st5_[xs_], mv5_[xs_], b_mv5_[xs_], rstd5_[xs_], b_rstd5_[xs_],
                                     1e-5, lng[:], lnb[:], b_ln)
                kb.op("sp", lambda e: e.dma_start(out=x1_v[t], in_=y5[xs_][:]), reads=[b_y5[xs_]], dsem=s_y5[xs_])
                yield

            items5 = [("g0", gate5(0), ()), ("g1", gate5(1), ("g0",))]
            for tb in range(NB):
                for tt in range(4):
                    items5.append((f"t{tb}_{tt}", tile5(tb, tt), (f"g{tb}",)))
                if tb + 2 < NB:
                    items5.append((f"g{tb + 2}", gate5(tb + 2), (f"t{tb}_3", f"g{tb + 1}")))
            run_interleaved(items5, 2)
            kb.barrier()


        if stop_after >= 6:
          with ExitStack() as es68:
            w4_all = T(es68, "w4_all", [128, NT, 4], F32); b_w4 = Buf()
            dest_i = T(es68, "dest_i", [128, NT * 4], I32); b_desti = Buf()
            blk_i = T(es68, "blk_i", [128, NBLK], I32); chg_i = T(es68, "chg_i", [128, NBLK], I32); b_blk = Buf()
            idx_w = T(es68, "idx_w", [128, NBLK], I32); idx_b = T(es68, "idx_b", [128, NBLK], I32)
            with ExitStack() as es:
                identF = T(es, "identF", [128, 128], F32); b_identF = Buf()
                kb.op("pool", lambda e: e.memset(identF[:], 0.0), writes=[b_identF])
                kb.op("pool", lambda e: e.affine_select(out=identF[:], in_=identF[:], pattern=[[-1, 128]], compare_op=ALU.not_equal,
                                                        fill=1.0, base=0, channel_multiplier=1), writes=[b_identF])
                ustr = T(es, "ustr", [128, 128], BF16); b_ustr = Buf()
                kb.op("pool", lambda e: e.memset(ustr[:], 1.0), writes=[b_ustr])
                kb.op("pool", lambda e: e.affine_select(out=ustr[:], in_=ustr[:], pattern=[[1, 128]], compare_op=ALU.is_gt,
                                                        fill=0.0, base=0, channel_multiplier=-1), writes=[b_ustr])
                onesb = T(es, "onesb6", [128, 128], BF16); b_onesb = Buf()
                kb.op("pool", lambda e: e.memset(onesb[:], 1.0), writes=[b_onesb])
                ones1f = T(es, "ones1f", [1, 128], F32); b_ones1f = Buf()
                kb.op("pool", lambda e: e.memset(ones1f[:], 1.0), writes=[b_ones1f])
                wr = T(es, "wr", [128, 8, NE], F32); br = T(es, "br", [1, NE], F32); b_wr = Buf()
                s_wr = kb.sem("p6wr")
                kb.op("sp", lambda e: e.dma_start(out=wr[:], in_=I["w_router"].rearrange("(dc p) n -> p dc n", p=128)), writes=[b_wr], dsem=s_wr)
                kb.op("sp", lambda e: e.dma_start(out=br[:], in_=I["b_router"][:, :]), writes=[b_wr], dsem=s_wr)
                x6 = [T(es, f"x6_{i}", [128, D], F32) for i in range(2)]; b_x6 = [Buf(), Buf()]
                u2 = T(es, "u2", [128, D], F32); b_u2 = Buf()
                u2b = [T(es, f"u2b{i}", [128, D], BF16) for i in range(2)]; b_u2b = [Buf(), Buf()]
                u2T = T(es, "u2T", [128, 8, 128], F32); b_u2T = Buf()
                st6 = T(es, "st6", [128, 2, 6], F32); mv6 = T(es, "mv6", [128, 2], F32); rstd6 = T(es, "rstd6", [128, 1], F32)
                b_st6 = Buf(); b_mv6 = Buf(); b_rstd6 = Buf()
                L_all = T(es, "L_all", [128, NT, NE], F32); b_L = Buf()
                top8 = T(es, "top8", [128, 8], F32); b_top8 = Buf()
                top4_all = T(es, "top4_all", [128, NT, 4], F32); b_top4 = Buf()
                negmax = T(es, "negmax", [128, 1], F32); b_negmax = Buf()
                e4 = T(es, "e4", [128, 4], F32); den = T(es, "den", [128, 1], F32); b_e4 = Buf(); b_den = Buf()
                maskb = T(es, "maskb", [128, NE], BF16); b_maskb = Buf()
                pos_all = T(es, "pos_all", [128, NT, NE], F32); b_pos = Buf()
                runcnt = T(es, "runcnt", [128, NE], F32); b_run = Buf()
                kb.op("dve", lambda e: e.memset(runcnt[:], 0.0), writes=[b_run])
                psT6 = P(es, "psT6", [128, 8, 128], F32); b_psT6 = Buf()
                psLg = P(es, "psLg", [128, NE], F32); b_psLg = Buf()
                psPos = P(es, "psPos", [128, NE], F32); b_psPos = Buf()
                psCnt = P(es, "psCnt", [128, NE], F32); b_psCnt = Buf()
                s_x6 = [kb.sem("p6x0"), kb.sem("p6x1")]; s_u6 = [kb.sem("p6u0"), kb.sem("p6u1")]
                x1_v6 = x1_dram.rearrange("(t p) d -> t p d", p=128); u2_v = u2_dram.rearrange("(t p) d -> t p d", p=128)
                u2_ = [u2, T(es, "u2_1", [128, D], F32)]; b_u2_ = [b_u2, Buf()]
                u2T_ = [u2T, T(es, "u2T_1", [128, 8, 128], F32)]; b_u2T_ = [b_u2T, Buf()]
                st6_ = [st6, T(es, "st6_1", [128, 2, 6], F32)]; mv6_ = [mv6, T(es, "mv6_1", [128, 2], F32)]; rstd6_ = [rstd6, T(es, "rstd6_1", [128, 1], F32)]
                b_st6_ = [b_st6, Buf()]; b_mv6_ = [b_mv6, Buf()]; b_rstd6_ = [b_rstd6, Buf()]
                top8_ = [top8, T(es, "top8_1", [128, 8], F32)]; b_top8_ = [b_top8, Buf()]
                negmax_ = [negmax, T(es, "negmax_1", [128, 1], F32)]; b_negmax_ = [b_negmax, Buf()]
                e4_ = [e4, T(es, "e4_1", [128, 4], F32)]; den_ = [den, T(es, "den_1", [128, 1], F32)]; b_e4_ = [b_e4, Buf()]; b_den_ = [b_den, Buf()]
                maskb_ = [maskb, T(es, "maskb_1", [128, NE], BF16)]; b_maskb_ = [b_maskb, Buf()]
                for i6 in range(2):
                    kb.op("sp", lambda e, i6=i6: e.dma_start(out=x6[i6][:], in_=x1_v6[i6]), writes=[b_x6[i6]], dsem=s_x6[i6])

                def route_a(t):
                    sl = t % 2
                    u2c = u2_[sl]; b_u2c = b_u2_[sl]; u2Tc = u2T_[sl]; b_u2Tc = b_u2T_[sl]
                    t8 = top8_[sl]; b_t8 = b_top8_[sl]; nm = negmax_[sl]; b_nm = b_negmax_[sl]
                    e4c = e4_[sl]; b_e4c = b_e4_[sl]; dn = den_[sl]; b_dn = b_den_[sl]; mk = maskb_[sl]; b_mk = b_maskb_[sl]
                    yield from ln_tile_g(x6[sl], b_x6[sl], u2c, b_u2c, st6_[sl], b_st6_[sl], mv6_[sl], b_mv6_[sl], rstd6_[sl], b_rstd6_[sl], 1e-6, SC2, SH2, b_mod)
                    if t + 2 < NT:
                        kb.op("sp", lambda e: e.dma_start(out=x6[sl][:], in_=x1_v6[t + 2]), writes=[b_x6[sl]], dsem=s_x6[sl])
                    kb.op("act", lambda e: e.copy(out=u2b[sl][:], in_=u2c[:]), reads=[b_u2c], writes=[b_u2b[sl]])
                    yield
                    kb.op("sp", lambda e: e.dma_start(out=u2_v[t], in_=u2b[sl][:]), reads=[b_u2b[sl]], dsem=s_u6[sl])
                    yield
                    kb.group("pe", [lambda e, dc=dc: e.transpose(out=psT6[:, dc, :], in_=u2c[:, dc * 128:(dc + 1) * 128], identity=identF[:]) for dc in range(8)],
                             reads=[b_u2c, b_identF], writes=[b_psT6])
                    kb.op("dve", lambda e: e.tensor_copy(out=u2Tc[:], in_=psT6[:]), reads=[b_psT6], writes=[b_u2Tc])
                    yield
                    fns = [lambda e, dc=dc: e.matmul(psLg[:], lhsT=u2Tc[:, dc, :], rhs=wr[:, dc, :], start=(dc == 0), stop=False) for dc in range(8)]
                    fns.append(lambda e: e.matmul(psLg[:], lhsT=ones1f[0:1, :], rhs=br[0:1, :], start=False, stop=True))
                    kb.group("pe", fns, reads=[b_u2Tc, b_wr, b_ones1f], writes=[b_psLg])
                    Lt = L_all[:, t, :]
                    kb.op("dve", lambda e: e.tensor_copy(out=Lt, in_=psLg[:]), reads=[b_psLg], writes=[b_L])
                    yield
                    kb.op("dve", lambda e: e.max(out=t8[:], in_=Lt), reads=[b_L], writes=[b_t8])
                    yield
                    kb.op("dve", lambda e: e.tensor_copy(out=top4_all[:, t, :], in_=t8[:, 0:4]), reads=[b_t8], writes=[b_top4])
                    yield
                    kb.op("dve", lambda e: e.tensor_scalar(out=mk[:], in0=Lt, scalar1=t8[:, 3:4], scalar2=None, op0=ALU.is_ge),
                          reads=[b_L, b_t8], writes=[b_mk])
                    yield
                    kb.op("dve", lambda e: e.tensor_scalar(out=nm[:], in0=t8[:, 0:1], scalar1=-1.0, scalar2=None, op0=ALU.mult),
                          reads=[b_t8], writes=[b_nm])
                    yield
                    kb.op("act", lambda e: e.activation(out=e4c[:], in_=t8[:, 0:4], func=AF.Exp, bias=nm[:, 0:1], scale=1.0, accum_out=dn[:, 0:1]),
                          reads=[b_t8, b_nm], writes=[b_e4c, b_dn])
                    yield
                    kb.op("dve", lambda e: e.reciprocal(out=dn[:], in_=dn[:]), reads=[b_dn], writes=[b_dn])
                    yield
                    kb.op("dve", lambda e: e.tensor_scalar(out=w4_all[:, t, :], in0=e4c[:], scalar1=dn[:, 0:1], scalar2=None, op0=ALU.mult),
                          reads=[b_e4c, b_dn], writes=[b_w4])
                    yield

                def route_b(t):
                    mk = maskb_[t % 2]; b_mk = b_maskb_[t % 2]
                    kb.op("pe", lambda e: e.matmul(psPos[:], lhsT=ustr[:], rhs=mk[:], start=True, stop=True), reads=[b_ustr, b_mk], writes=[b_psPos])
                    kb.op("pe", lambda e: e.matmul(psCnt[:], lhsT=onesb[:], rhs=mk[:], start=True, stop=True), reads=[b_onesb, b_mk], writes=[b_psCnt])
                    kb.op("dve", lambda e: e.tensor_tensor(out=pos_all[:, t, :], in0=psPos[:], in1=runcnt[:], op=ALU.add), reads=[b_psPos, b_run], writes=[b_pos])
                    kb.op("dve", lambda e: e.tensor_tensor(out=runcnt[:], in0=psCnt[:], in1=runcnt[:], op=ALU.add), reads=[b_psCnt, b_run], writes=[b_run])
                    yield

                items6 = []
                for t in range(NT):
                    items6.append((f"a{t}", route_a(t), ((f"b{t - 2}",) if t >= 2 else ())))
                    if t >= 1:
                        items6.append((f"b{t - 1}", route_b(t - 1), (f"a{t - 1}",) + ((f"b{t - 2}",) if t >= 2 else ())))
                items6.append((f"b{NT - 1}", route_b(NT - 1), (f"a{NT - 1}", f"b{NT - 2}")))
                run_interleaved(items6, 2)
                thr_i = T(es, "thr_i", [128, 64], I32); thr = T(es, "thr", [128, 64], F32); b_thr = Buf()
                kb.op("pool", lambda e: e.iota(thr_i[:], pattern=[[BS, 64]], base=0, channel_multiplier=0), writes=[b_thr])
                kb.op("dve", lambda e: e.tensor_copy(out=thr[:], in_=thr_i[:]), reads=[b_thr], writes=[b_thr])
                jf_i = T(es, "jf_i", [128, NBLK], I32); jf = T(es, "jf", [128, NBLK], F32); b_jf = Buf()
                kb.op("pool", lambda e: e.iota(jf_i[:], pattern=[[1, NBLK]], base=0, channel_multiplier=0), writes=[b_jf])
                kb.op("dve", lambda e: e.tensor_copy(out=jf[:], in_=jf_i[:]), reads=[b_jf], writes=[b_jf])
                junk = T(es, "junk", [128, 64], F32); b_junk = Buf()
                nblk = T(es, "nblk", [128, NE], F32); b_nblk = Buf()
                zeros32 = T(es, "zeros32", [128, NE], F32); b_z32 = Buf()
                kb.op("dve", lambda e: e.memset(zeros32[:], 0.0), writes=[b_z32])
                for ex in range(NE):
                    kb.op("dve", lambda e, ex=ex: e.tensor_scalar(out=junk[:], in0=thr[:], scalar1=runcnt[:, ex:ex + 1], scalar2=0.0, op0=ALU.is_lt, op1=ALU.add,
                                                                  accum_out=nblk[:, ex:ex + 1]), reads=[b_thr, b_run], writes=[b_junk, b_nblk])
                pend = T(es, "pend", [128, NE], F32); b_pend = Buf()
                kb.op("dve", lambda e: e.tensor_tensor_scan(out=pend[:], data0=nblk[:], data1=zeros32[:], initial=0.0, op0=ALU.add, op1=ALU.add),
                      reads=[b_nblk, b_z32], writes=[b_pend])
                pstart = T(es, "pstart", [128, NE], F32); b_pstart = Buf()
                kb.op("dve", lambda e: e.tensor_tensor(out=pstart[:], in0=pend[:], in1=nblk[:], op=ALU.subtract), reads=[b_pend, b_nblk], writes=[b_pstart])
                kb.op("dve", lambda e: e.tensor_scalar(out=pstart[:], in0=pstart[:], scalar1=float(BS), scalar2=None, op0=ALU.mult), reads=[b_pstart], writes=[b_pstart])
                acc = T(es, "acc6", [128, NBLK], F32); b_acc = Buf()
                chg = T(es, "chg6", [128, NBLK], F32); b_chg = Buf()
                kb.op("dve", lambda e: e.memset(acc[:], 0.0), writes=[b_acc])
                for ex in range(NE - 1):
                    kb.op("dve", lambda e, ex=ex: e.scalar_tensor_tensor(out=acc[:], in0=jf[:], scalar=pend[:, ex:ex + 1], in1=acc[:], op0=ALU.is_ge, op1=ALU.add),
                          reads=[b_jf, b_pend, b_acc], writes=[b_acc])
                kb.op("dve", lambda e: e.memset(chg[:], 1.0), writes=[b_chg])
                kb.op("dve", lambda e: e.tensor_tensor(out=chg[:, 2:NBLK], in0=acc[:, 2:NBLK], in1=acc[:, 0:NBLK - 2], op=ALU.not_equal), reads=[b_acc, b_chg], writes=[b_chg])
                kb.op("dve", lambda e: e.tensor_copy(out=blk_i[:], in_=acc[:]), reads=[b_acc], writes=[b_blk])
                kb.op("dve", lambda e: e.tensor_copy(out=chg_i[:], in_=chg[:]), reads=[b_chg], writes=[b_blk])
                BIG = float(1 << 20)
                pio_i = T(es, "pio_i", [128, 1], I32); pio = T(es, "pio", [128, 1], F32); b_pio = Buf()
                kb.op("pool", lambda e: e.iota(pio_i[:], pattern=[[0, 1]], base=0, channel_multiplier=1), writes=[b_pio])
                kb.op("dve", lambda e: e.tensor_copy(out=pio[:], in_=pio_i[:]), reads=[b_pio], writes=[b_pio])
                idxf = T(es, "idxf", [128, NBLK], F32); b_idxf = Buf()
                kb.op("dve", lambda e: e.tensor_scalar(out=idxf[:], in0=acc[:], scalar1=128.0, scalar2=None, op0=ALU.mult), reads=[b_acc], writes=[b_idxf])
                kb.op("dve", lambda e: e.tensor_scalar(out=idxf[:], in0=idxf[:], scalar1=pio[:, 0:1], scalar2=None, op0=ALU.add), reads=[b_idxf, b_pio], writes=[b_idxf])
                kb.op("dve", lambda e: e.tensor_copy(out=idx_w[:], in_=idxf[:]), reads=[b_idxf], writes=[b_blk])
                kb.op("dve", lambda e: e.tensor_copy(out=idx_b[:], in_=acc[:]), reads=[b_acc], writes=[b_blk])
                destf = T(es, "destf", [128, NT * 4], F32); b_destf = Buf()
                A6 = T(es, "A6", [128, NE], F32); b_A6 = Buf()
                junk2 = T(es, "junk2", [128, NE], F32); b_junk2 = Buf()
                s_sc = [kb.sem("p6s0"), kb.sem("p6s1")]; s_ul = [kb.sem("p6l0"), kb.sem("p6l1")]
                A6_ = [A6, T(es, "A6_1", [128, NE], F32)]; b_A6_ = [b_A6, Buf()]
                b_dt = [Buf() for _ in range(NT)]
                for t in range(NT):
                    sl = t % 2
                    kb.op("dve", lambda e: e.tensor_tensor(out=A6_[sl][:], in0=pos_all[:, t, :], in1=pstart[:], op=ALU.add), reads=[b_pos, b_pstart], writes=[b_A6_[sl]])
                    for j in range(4):
                        kb.op("dve", lambda e, j=j: e.scalar_tensor_tensor(out=junk2[:], in0=L_all[:, t, :], scalar=top4_all[:, t, j:j + 1], in1=A6_[sl][:],
                                                                           op0=ALU.is_equal, op1=ALU.mult, accum_out=destf[:, t * 4 + j:t * 4 + j + 1]),
                              reads=[b_L, b_top4, b_A6_[sl]], writes=[b_junk2, b_destf])
                    kb.op("dve", lambda e: e.tensor_copy(out=dest_i[:, t * 4:(t + 1) * 4], in_=destf[:, t * 4:(t + 1) * 4]), reads=[b_destf], writes=[b_dt[t]])
                    kb.op("sp", lambda e: e.dma_start(out=u2b[sl][:], in_=u2_v[t]), writes=[b_u2b[sl]], dsem=s_ul[sl])
                    for j in range(4):
                        kb.op("pool", lambda e, j=j: e.indirect_dma_start(out=xs_dram[:, :], out_offset=bass.IndirectOffsetOnAxis(ap=dest_i[:, t * 4 + j:t * 4 + j + 1], axis=0),
                                                                          in_=u2b[sl][:], in_offset=None), reads=[b_u2b[sl], b_dt[t]], dsem=s_sc[sl])
                if dbg6 is not None:
                    s_d6 = kb.sem("p6dbg")
                    kb.op("sp", lambda e: e.dma_start(out=dbg6["L"].rearrange("(t p) n -> p t n", p=128), in_=L_all[:]), reads=[b_L], dsem=s_d6)
                    kb.op("sp", lambda e: e.dma_start(out=dbg6["dest"][:, :], in_=dest_i[:]), reads=b_dt, dsem=s_d6)
                    kb.op("sp", lambda e: e.dma_start(out=dbg6["blk"][:, :], in_=blk_i[0:1, :]), reads=[b_blk], dsem=s_d6)
                    kb.op("sp", lambda e: e.dma_start(out=dbg6["chg"][:, :], in_=chg_i[0:1, :]), reads=[b_blk], dsem=s_d6)
                    kb.op("sp", lambda e: e.dma_start(out=dbg6["w4"][:, :], in_=w4_all[:].rearrange("p t j -> p (t j)")), reads=[b_w4], dsem=s_d6)
                kb.barrier()

            if stop_after >= 7:
              with ExitStack() as es:
                wgu = [T(es, f"wgu{i}", [128, 8 * 2 * D], BF16) for i in range(2)]
                wd = [T(es, f"wd{i}", [128, 8 * D], BF16) for i in range(2)]
                bgf = [T(es, f"bgf{i}", [128, 16], F32) for i in range(2)]
                bd = [T(es, f"bd{i}", [2, D], BF16) for i in range(2)]
                b_w7 = [Buf(), Buf()]
                ones7 = T(es, "ones7", [1, 128], BF16); b_ones7 = Buf()
                kb.op("pool", lambda e: e.memset(ones7[:], 1.0), writes=[b_ones7])
                xsb = [T(es, f"xsb{i}", [128, NSUB, D], BF16) for i in range(2)]; b_xsb = [Buf(), Buf()]
                xsT = [T(es, f"xsT{i}", [128, 8, BS], BF16) for i in range(2)]; b_xsT = [Buf(), Buf()]
                actT = [T(es, f"actT{i}", [128, 8, BS], BF16) for i in range(2)]; b_actT = [Buf(), Buf()]
                g7 = [T(es, f"g7_{i}", [128, BS], F32) for i in range(2)]; b_g7 = [Buf(), Buf()]
                sg = [T(es, f"sg_{i}", [128, BS], F32) for i in range(2)]; b_sg = [Buf(), Buf()]
                u1 = [T(es, f"u1_{i}", [128, BS], F32) for i in range(2)]; b_u1 = [Buf(), Buf()]
                a1 = [T(es, f"a1_{i}", [128, BS], F32) for i in range(2)]; b_a1 = [Buf(), Buf()]
                ysb = [T(es, f"ysb{i}", [128, 512], BF16) for i in range(3)]; b_ysb = [Buf() for _ in range(3)]
                pstx = P(es, "pstx", [128, 8, 128], BF16); b_pstx = Buf()
                psG = [P(es, f"psG{i}", [128, BS], F32) for i in range(2)]; b_psG = [Buf(), Buf()]
                psU = [P(es, f"psU{i}", [128, BS], F32) for i in range(2)]; b_psU = [Buf(), Buf()]
                psYb = [P(es, f"psYb{i}", [128, 512], F32) for i in range(3)]; b_psYb = [Buf() for _ in range(3)]
                s_w7 = [kb.sem("p7w0"), kb.sem("p7w1")]; s_xs = [kb.sem("p7x0"), kb.sem("p7x1")]; s_ys = [kb.sem(f"p7y{i}") for i in range(3)]
                nblk7 = debug.get("p7_blocks", NBLK)
                xs_v = xs_dram.rearrange("(j t p) d -> j p t d", t=NSUB, p=128)

                def load_w(j):
                    sl = j % 2
                    for dst, srcT in ((wgu[sl][:, :], wgu_bf), (wd[sl][:, :], wd_bf), (bgf[sl][:, :], I["bgu_fm"])):
                        kb.op("pool", lambda e, dst=dst, srcT=srcT: e.indirect_dma_start(out=dst, out_offset=None, in_=srcT[:, :],
                                                                                        in_offset=bass.IndirectOffsetOnAxis(ap=idx_w[:, j:j + 1], axis=0)),
                              reads=[b_blk], writes=[b_w7[sl]], dsem=s_w7[sl])
                    kb.op("pool", lambda e: e.indirect_dma_start(out=bd[sl][0:2, :], out_offset=None, in_=bd_bf[:, :],
                                                                 in_offset=bass.IndirectOffsetOnAxis(ap=idx_b[0:2, j:j + 1], axis=0)),
                          reads=[b_blk], writes=[b_w7[sl]], dsem=s_w7[sl])
                    kb.op("sp", lambda e: e.dma_start(out=xsb[sl][:], in_=xs_v[j]), writes=[b_xsb[sl]], dsem=s_xs[sl])

                def emit_tx(jj):
                    s2 = jj % 2
                    for st in range(NSUB):
                        kb.group("pe", [lambda e, dc=dc: e.transpose(out=pstx[:, dc, :], in_=xsb[s2][:, st, dc * 128:(dc + 1) * 128], identity=ident[:]) for dc in range(8)],
                                 reads=[b_xsb[s2], b_ident], writes=[b_pstx])
                        kb.op("act", lambda e: e.copy(out=xsT[s2][:, :, st * 128:(st + 1) * 128], in_=pstx[:]), reads=[b_pstx], writes=[b_xsT[s2]])

                load_w(0)
                yk = 0
                for j in range(nblk7):
                    sl = j % 2
                    if j + 1 < nblk7:
                        load_w(j + 1)
                    if j == 0:
                        emit_tx(0)
                    for fc in range(8):
                        pb = fc % 2
                        kb.group("pe", [lambda e, r=r: e.matmul(psG[pb][:], lhsT=wgu[sl][:, r * 2048 + fc * 128:r * 2048 + (fc + 1) * 128], rhs=xsT[sl][:, r, :],
                                                                start=(r == 0), stop=(r == 7)) for r in range(8)],
                                 reads=[b_xsT[sl], b_w7[sl]], writes=[b_psG[pb]])
                        kb.group("pe", [lambda e, r=r: e.matmul(psU[pb][:], lhsT=wgu[sl][:, r * 2048 + 1024 + fc * 128:r * 2048 + 1024 + (fc + 1) * 128], rhs=xsT[sl][:, r, :],
                                                                start=(r == 0), stop=(r == 7)) for r in range(8)],
                                 reads=[b_xsT[sl], b_w7[sl]], writes=[b_psU[pb]])
                        kb.op("dve", lambda e: e.tensor_scalar(out=g7[pb][:], in0=psG[pb][:], scalar1=bgf[sl][:, fc:fc + 1], scalar2=7.0, op0=ALU.add, op1=ALU.min),
                              reads=[b_psG[pb], b_w7[sl]], writes=[b_g7[pb]])
                        kb.op("act", lambda e: e.activation(out=sg[pb][:], in_=g7[pb][:], func=AF.Sigmoid, scale=1.702), reads=[b_g7[pb]], writes=[b_sg[pb]])
                        kb.op("dve", lambda e: e.tensor_scalar(out=u1[pb][:], in0=psU[pb][:], scalar1=bgf[sl][:, 8 + fc:9 + fc], scalar2=7.0, op0=ALU.add, op1=ALU.min),
                              reads=[b_psU[pb], b_w7[sl]], writes=[b_u1[pb]])
                        kb.op("dve", lambda e: e.tensor_scalar(out=u1[pb][:], in0=u1[pb][:], scalar1=-7.0, scalar2=1.0, op0=ALU.max, op1=ALU.add),
                              reads=[b_u1[pb]], writes=[b_u1[pb]])
                        kb.op("dve", lambda e: e.tensor_tensor(out=a1[pb][:], in0=u1[pb][:], in1=g7[pb][:], op=ALU.mult), reads=[b_u1[pb], b_g7[pb]], writes=[b_a1[pb]])
                        kb.op("dve", lambda e: e.tensor_tensor(out=actT[sl][:, fc, :], in0=a1[pb][:], in1=sg[pb][:], op=ALU.mult), reads=[b_a1[pb], b_sg[pb]], writes=[b_actT[sl]])
                    if j + 1 < nblk7:
                        emit_tx(j + 1)
                    for st in range(NSUB):
                        r0 = j * BS + st * 128
                        for half in range(2):
                            yb = yk % 3; yk += 1
                            fns = [lambda e, fc=fc: e.matmul(psYb[yb][:], lhsT=actT[sl][:, fc, st * 128:(st + 1) * 128],
                                                             rhs=wd[sl][:, fc * 1024 + half * 512:fc * 1024 + (half + 1) * 512], start=(fc == 0), stop=False) for fc in range(8)]
                            fns.append(lambda e: e.matmul(psYb[yb][:], lhsT=ones7[0:1, :], rhs=bd[sl][0:1, half * 512:(half + 1) * 512], start=False, stop=True))
                            kb.group("pe", fns, reads=[b_actT[sl], b_w7[sl], b_ones7], writes=[b_psYb[yb]])
                            kb.op("act", lambda e: e.copy(out=ysb[yb][:], in_=psYb[yb][:]), reads=[b_psYb[yb]], writes=[b_ysb[yb]])
                            kb.op("act", lambda e: e.dma_start(out=ys_dram[r0:r0 + 128, half * 512:(half + 1) * 512], in_=ysb[yb][:]), reads=[b_ysb[yb]], dsem=s_ys[yb])
                kb.barrier()

            if stop_after >= 8:
              with ExitStack() as es:
                lng2 = T(es, "lng2", [128, D], F32); lnb2 = T(es, "lnb2", [128, D], F32); b_ln2 = Buf()
                s_ln2 = kb.sem("p8ln")
                kb.op("sp", lambda e: e.dma_start(out=lng2[:], in_=I["ln2_g"][0:1, :].partition_broadcast(128)), writes=[b_ln2], dsem=s_ln2)
                kb.op("sp", lambda e: e.dma_start(out=lnb2[:], in_=I["ln2_b"][0:1, :].partition_broadcast(128)), writes=[b_ln2], dsem=s_ln2)
                yg = [[T(es, f"yg{i}_{j}", [128, D], BF16) for j in range(4)] for i in range(2)]; b_yg = [[Buf() for _ in range(4)] for _ in range(2)]
                x8 = [T(es, f"x8_{i}", [128, D], F32) for i in range(2)]; b_x8 = [Buf(), Buf()]
                h8 = T(es, "h8", [128, D], F32); b_h8 = Buf()
                o8 = [T(es, f"o8_{i}", [128, D], F32) for i in range(2)]; b_o8 = [Buf(), Buf()]
                st8 = T(es, "st8", [128, 2, 6], F32); mv8 = T(es, "mv8", [128, 2], F32); rstd8 = T(es, "rstd8", [128, 1], F32)
                b_st8 = Buf(); b_mv8 = Buf(); b_rstd8 = Buf()
                s_g8 = [kb.sem("p8g0"), kb.sem("p8g1")]; s_x8 = [kb.sem("p8x0"), kb.sem("p8x1")]; s_o8 = [kb.sem("p8o0"), kb.sem("p8o1")]
                x1_v8 = x1_dram.rearrange("(t p) d -> t p d", p=128); out_v = out.rearrange("(t p) d -> t p d", p=128)

                def load8(t):
                    sl = t % 2
                    kb.op("sp", lambda e: e.dma_start(out=x8[sl][:], in_=x1_v8[t]), writes=[b_x8[sl]], dsem=s_x8[sl])
                    for j in range(4):
                        kb.op("pool", lambda e, j=j: e.indirect_dma_start(out=yg[sl][j][:], out_offset=None, in_=ys_dram[:, :],
                                                                          in_offset=bass.IndirectOffsetOnAxis(ap=dest_i[:, t * 4 + j:t * 4 + j + 1], axis=0)),
                              reads=[b_desti], writes=[b_yg[sl][j]], dsem=s_g8[sl])
                h8_ = [h8, T(es, "h8_1", [128, D], F32)]; b_h8_ = [b_h8, Buf()]
                st8_ = [st8, T(es, "st8_1", [128, 2, 6], F32)]; mv8_ = [mv8, T(es, "mv8_1", [128, 2], F32)]; rstd8_ = [rstd8, T(es, "rstd8_1", [128, 1], F32)]
                b_st8_ = [b_st8, Buf()]; b_mv8_ = [b_mv8, Buf()]; b_rstd8_ = [b_rstd8, Buf()]
                load8(0); load8(1)

                def tile8(t):
                    sl = t % 2
                    hh = h8_[sl]; b_hh = b_h8_[sl]
                    kb.op("dve", lambda e: e.tensor_scalar(out=hh[:], in0=yg[sl][0][:], scalar1=w4_all[:, t, 0:1], scalar2=None, op0=ALU.mult),
                          reads=[b_yg[sl][0], b_w4], writes=[b_hh])
                    yield
                    for j in range(1, 4):
                        kb.op("dve", lambda e, j=j: e.scalar_tensor_tensor(out=hh[:], in0=yg[sl][j][:], scalar=w4_all[:, t, j:j + 1], in1=hh[:], op0=ALU.mult, op1=ALU.add),
                              reads=[b_yg[sl][j], b_w4, b_hh], writes=[b_hh])
                        yield
                    kb.op("dve", lambda e: e.tensor_tensor(out=hh[:], in0=hh[:], in1=G2, op=ALU.mult), reads=[b_hh, b_mod], writes=[b_hh])
                    yield
                    kb.op("dve", lambda e: e.scalar_tensor_tensor(out=hh[:], in0=x8[sl][:], scalar=float(ALPHA), in1=hh[:], op0=ALU.mult, op1=ALU.add),
                          reads=[b_x8[sl], b_hh], writes=[b_hh])
                    if t + 2 < NT:
                        load8(t + 2)
                    yield
                    yield from ln_tile_g(hh, b_hh, o8[sl], b_o8[sl], st8_[sl], b_st8_[sl], mv8_[sl], b_mv8_[sl], rstd8_[sl], b_rstd8_[sl],
                                         1e-5, lng2[:], lnb2[:], b_ln2, rstd_on_act=True)
                    kb.op("sp", lambda e: e.dma_start(out=out_v[t], in_=o8[sl][:]), reads=[b_o8[sl]], dsem=s_o8[sl])
                    yield

                run_interleaved((tile8(t) for t in range(NT)), 2)
                kb.barrier()

        kb.barrier()
    return nc, dbg_out, I


def _tables():
    rows = S // 64
    row_ids = np.repeat(np.arange(rows, dtype=np.float32), 64)
    col_ids = np.tile(np.arange(64, dtype=np.float32), rows)
    freqs = (np.float32(10000.0) ** (-np.arange(0, 64, 2, dtype=np.float32) / np.float32(64))).astype(np.float32)
    ang_r = (row_ids[:, None] * freqs).astype(np.float32)
    ang_c = (col_ids[:, None] * freqs).astype(np.float32)
    cr, sr, cc, sc = np.cos(ang_r), np.sin(ang_r), np.cos(ang_c), np.sin(ang_c)
    rope = np.concatenate([cr, cr, cc, cc, -sr, sr, -sc, sc], axis=1).astype(np.float32)
    n1 = np.arange(128)[:, None, None]; n2 = np.arange(64)[None, :, None]; k1 = np.arange(128)[None, None, :]
    ang = 2.0 * np.pi * ((k1 * (64 * n1 + n2)) % 8192) / 8192.0
    fft_e = np.stack([np.cos(ang), -np.sin(ang)], axis=2)
    n2v = np.arange(64)[:, None]; k2 = np.arange(64)[None, :]
    th = 2.0 * np.pi * ((n2v * k2) % 64) / 64.0
    sc_ = 1.0 / np.sqrt(8192.0)
    w3 = np.zeros((128, 128))
    w3[0:64, 0:64] = np.cos(th); w3[64:128, 0:64] = np.sin(th)
    w3[0:64, 64:128] = -np.sin(th); w3[64:128, 64:128] = np.cos(th)
    w3 *= sc_
    cch = np.arange(128)[:, None] * np.arange(128)[None, :]
    thc = 2.0 * np.pi * (cch % 128) / 128.0
    dft_c = np.stack([np.cos(thc), np.sin(thc)], axis=1) / np.sqrt(128.0)
    bf = ml_dtypes.bfloat16
    return {"rope_tab": rope, "fft_e": fft_e.astype(np.float32).astype(bf), "fft_w3": w3.astype(np.float32).astype(bf),
            "dft_c": dft_c.astype(np.float32).astype(bf)}


def make_in_maps(inputs):
    tabs = _tables()
    shared = {}
    for k in ("w_ada", "w_in", "w_fourier", "w_attn_o", "w_out", "w_router", "w_gate_up", "w_down"):
        shared[k] = np.ascontiguousarray(np.asarray(inputs[k], dtype=np.float32)[0])
    for k in ("b_ada", "q_norm_g", "k_norm_g", "ln1_g", "ln1_b", "b_router", "ln2_g", "ln2_b"):
        shared[k] = np.ascontiguousarray(np.asarray(inputs[k], dtype=np.float32)[0][None, :])
    shared["b_down"] = np.ascontiguousarray(np.asarray(inputs["b_down"], dtype=np.float32)[0])
    shared["bgu_fm"] = np.ascontiguousarray(np.asarray(inputs["b_gate_up"], dtype=np.float32)[0].reshape(NE, 16, 128).transpose(0, 2, 1).reshape(NE * 128, 16))
    shared.update(tabs)
    x = np.asarray(inputs["x"], dtype=np.float32)
    c = np.asarray(inputs["c"], dtype=np.float32)
    maps = []
    for b in range(8):
        m = dict(shared)
        m["x"] = np.ascontiguousarray(x[b])
        m["c_fm"] = np.ascontiguousarray(c[b].reshape(8, 128).T)
        maps.append(m)
    return maps


def kernel(**inputs):
    nc, _, _ = build()
    maps = make_in_maps(inputs)
    res = run_bass_kernel_spmd(nc, maps, core_ids=list(range(8)))
    return np.stack([np.asarray(r["out"], dtype=np.float32) for r in res.results], axis=0)
```

```python
import numpy as np
import ml_dtypes
from contextlib import ExitStack
import concourse.bass as bass
import concourse.mybir as mybir
from concourse.bass_utils import run_bass_kernel_spmd

F32 = mybir.dt.float32
BF16 = mybir.dt.bfloat16
I32 = mybir.dt.int32
AF = mybir.ActivationFunctionType
ALU = mybir.AluOpType
AX = mybir.AxisListType

S = 8192
D = 1024
NT = S // 128
NB = S // 512
NE = 32
BS = 512
NSUB = BS // 128
NBLK = (S * 4 + NE * (BS - 1)) // BS
NPAD = NBLK * BS
ALPHA = 2.0 ** 0.25


class Sem:
    def __init__(self, h, is_dma):
        self.h = h
        self.v = 0
        self.is_dma = is_dma


class Tok:
    __slots__ = ("sem", "val")

    def __init__(self, sem, val):
        self.sem = sem
        self.val = val


class Buf:
    def __init__(self, name=""):
        self.name = name
        self.w = None
        self.r = {}


class KB:
    def __init__(self, nc, es):
        self.nc = nc
        self.es = es
        self.engs = {"pe": nc.tensor, "act": nc.scalar, "dve": nc.vector, "pool": nc.gpsimd, "sp": nc.sync}
        self.sems = []
        self.esem = {}
        for e in ("pe", "act", "dve", "pool"):
            self.esem[e] = self.sem("e_" + e, is_dma=False)
        self.waited = {e: {} for e in self.engs}
        self.n_ins = 0

    def sem(self, name, is_dma=True):
        s = Sem(self.es.enter_context(self.nc.semaphore(name)), is_dma)
        self.sems.append(s)
        return s

    def wait(self, eng, tok):
        if tok is None:
            return
        val = tok.sem.v if tok.sem.is_dma else tok.val
        w = self.waited[eng]
        if w.get(id(tok.sem), 0) >= val:
            return
        self.engs[eng].wait_ge(tok.sem.h, val)
        w[id(tok.sem)] = val

    def _deps(self, eng, reads, writes):
        for b in reads:
            self.wait(eng, b.w)
        for b in writes:
            self.wait(eng, b.w)
            for t in b.r.values():
                self.wait(eng, t)

    def _done(self, tok, reads, writes):
        for b in reads:
            b.r[id(tok.sem)] = tok
        for b in writes:
            b.w = tok
            b.r = {}

    def op(self, eng, fn, reads=(), writes=(), dsem=None):
        self._deps(eng, reads, writes)
        ins = fn(self.engs[eng])
        self.n_ins += 1
        if dsem is not None:
            ins.then_inc(dsem.h, 16)
            dsem.v += 16
            tok = Tok(dsem, dsem.v)
        else:
            s = self.esem[eng]
            ins.then_inc(s.h, 1)
            s.v += 1
            tok = Tok(s, s.v)
        self._done(tok, reads, writes)
        return tok

    def group(self, eng, fns, reads=(), writes=()):
        self._deps(eng, reads, writes)
        ins = None
        for fn in fns:
            ins = fn(self.engs[eng])
            self.n_ins += 1
        s = self.esem[eng]
        ins.then_inc(s.h, 1)
        s.v += 1
        tok = Tok(s, s.v)
        self._done(tok, reads, writes)
        return tok

    def barrier(self):
        for e in self.engs:
            for s in self.sems:
                if s.v > 0:
                    self.wait(e, Tok(s, s.v))


def run_interleaved(gens, width=2):
    items = []
    for g in gens:
        items.append(g if isinstance(g, tuple) else (None, g, ()))
    done = set()
    active = []
    pos = 0
    while True:
        while len(active) < width and pos < len(items):
            name, g, deps = items[pos]
            if any(d not in done for d in deps):
                break
            active.append((name, g))
            pos += 1
        if not active:
            assert pos >= len(items), "interleave deadlock"
            break
        for ent in list(active):
            try:
                next(ent[1])
            except StopIteration:
                active.remove(ent)
                if ent[0] is not None:
                    done.add(ent[0])


def build(debug=None):
    debug = debug or {}
    stop_after = debug.get("stop_after", 99)
    nc = bass.Bass("TRN2", target_bir_lowering=False)
    I = {}

    def inp(name, shape, dt=F32):
        I[name] = nc.dram_tensor(name, shape, dt, kind="ExternalInput").ap()
        return I[name]

    inp("x", [S, D]); inp("c_fm", [128, 8]); inp("w_ada", [D, 6 * D]); inp("b_ada", [1, 6 * D])
    inp("w_in", [D, 4096]); inp("q_norm_g", [1, 128]); inp("k_norm_g", [1, 128])
    inp("w_fourier", [512, D]); inp("w_attn_o", [D, D]); inp("w_out", [D, D])
    inp("ln1_g", [1, D]); inp("ln1_b", [1, D]); inp("w_router", [D, NE]); inp("b_router", [1, NE])
    if stop_after >= 7:
        inp("w_gate_up", [NE, D, 2 * D]); inp("bgu_fm", [NE * 128, 16]); inp("w_down", [NE, D, D]); inp("b_down", [NE, D])
    inp("ln2_g", [1, D]); inp("ln2_b", [1, D])
    inp("rope_tab", [S, 256])
    inp("fft_e", [128, 64, 2, 128], BF16)
    inp("fft_w3", [128, 128], BF16)
    inp("dft_c", [128, 2, 128], BF16)
    out = nc.dram_tensor("out", [S, D], F32, kind="ExternalOutput").ap()

    dbg_out = {}

    def scratch(name, shape, dt):
        if name in debug.get("dump", ()):
            t = nc.dram_tensor(name, shape, dt, kind="ExternalOutput").ap()
            dbg_out[name] = t
            return t
        return nc.dram_tensor(name, shape, dt, kind="Internal").ap()

    f_dram = scratch("f_dram", [S, 512], BF16)
    qT_dram = scratch("qT_dram", [8, 128, S], BF16)
    kT_dram = scratch("kT_dram", [2, 128, S], BF16)
    v_dram = scratch("v_dram", [S, 256], BF16)
    gT_dram = scratch("gT_dram", [2048, S], BF16)
    Ys_dram = scratch("Ys_dram", [2, 64, 128, 512], BF16)
    XT_dram = scratch("XT_dram", [1024, S], BF16)
    OT_dram = scratch("OT_dram", [1024, S], BF16)
    x1_dram = scratch("x1_dram", [S, D], F32)
    u2_dram = scratch("u2_dram", [S, D], BF16)
    xs_dram = scratch("xs_dram", [NPAD, D], BF16)
    ys_dram = scratch("ys_dram", [NPAD, D], BF16)
    wgu_bf = scratch("wgu_bf", [NE * 128, 8 * 2 * D], BF16)
    wd_bf = scratch("wd_bf", [NE * 128, 8 * D], BF16)
    bgu_bf = scratch("bgu_bf", [NE, 2 * D], BF16)
    bd_bf = scratch("bd_bf", [NE, D], BF16)
    dbg6 = None
    if "dbg6" in debug.get("dump", ()):
        dbg6 = {"L": nc.dram_tensor("d6_L", [S, NE], F32, kind="ExternalOutput").ap(),
                "dest": nc.dram_tensor("d6_dest", [128, NT * 4], I32, kind="ExternalOutput").ap(),
                "blk": nc.dram_tensor("d6_blk", [1, NBLK], I32, kind="ExternalOutput").ap(),
                "chg": nc.dram_tensor("d6_chg", [1, NBLK], I32, kind="ExternalOutput").ap(),
                "w4": nc.dram_tensor("d6_w4", [128, NT * 4], F32, kind="ExternalOutput").ap()}
    mod_dump = scratch("mod_dump", [128, 6 * D], F32) if "mod_dump" in debug.get("dump", ()) else None

    with ExitStack() as top:
        kb = KB(nc, top)

        def T(es, name, shape, dt):
            return es.enter_context(nc.sbuf_tensor(name, shape, dt))

        def P(es, name, shape, dt):
            return es.enter_context(nc.psum_tensor(name, shape, dt))

        mod_bc = T(top, "mod_bc", [128, 6 * D], F32)
        b_mod = Buf("mod_bc")
        ident = T(top, "ident", [128, 128], BF16)
        b_ident = Buf("ident")
        kb.op("pool", lambda e: e.memset(ident[:], 0.0), writes=[b_ident])
        kb.op("pool", lambda e: e.affine_select(out=ident[:], in_=ident[:], pattern=[[-1, 128]], compare_op=ALU.not_equal,
                                                fill=1.0, base=0, channel_multiplier=1), writes=[b_ident])
        mhalf = T(top, "mhalf", [128, 16], F32)
        b_mhalf = Buf("mhalf")
        kb.op("pool", lambda e: e.memset(mhalf[:], -0.5), writes=[b_mhalf])

        with ExitStack() as es:
            c_sb = T(es, "c_sb", [128, 8], F32); cond = T(es, "cond", [128, 8], F32)
            cond_bc = T(es, "cond_bc", [128, 8, 128], F32)
            wblk = [T(es, f"wblk{i}", [128, 8, 512], F32) for i in range(2)]
            bada = T(es, "bada", [1, 6 * D], F32); ones1 = T(es, "ones1", [1, 128], F32)
            ps0 = [P(es, f"ps0_{i}", [128, 512], F32) for i in range(2)]
            b_c = Buf(); b_cond = Buf(); b_cbc = Buf(); b_wblk = [Buf(), Buf()]; b_bada = Buf(); b_ones1 = Buf(); b_ps0 = [Buf(), Buf()]
            s_c = kb.sem("p0c"); s_w = [kb.sem("p0w0"), kb.sem("p0w1")]
            kb.op("sp", lambda e: e.dma_start(out=c_sb[:], in_=I["c_fm"][:, :]), writes=[b_c], dsem=s_c)
            kb.op("sp", lambda e: e.dma_start(out=bada[:], in_=I["b_ada"][:, :]), writes=[b_bada], dsem=s_c)
            kb.op("pool", lambda e: e.memset(ones1[:], 1.0), writes=[b_ones1])
            kb.op("act", lambda e: e.activation(out=cond[:], in_=c_sb[:], func=AF.Silu), reads=[b_c], writes=[b_cond])
            kb.op("dve", lambda e: e.tensor_copy(out=cond_bc[:], in_=cond[:].unsqueeze(2).to_broadcast([128, 8, 128])),
                  reads=[b_cond], writes=[b_cbc])
            w_ada_v = I["w_ada"].rearrange("(dc p) c -> p dc c", p=128)
            for cb in range(12):
                sl = cb % 2
                kb.op("sp", lambda e: e.dma_start(out=wblk[sl][:], in_=w_ada_v[:, :, cb * 512:(cb + 1) * 512]),
                      writes=[b_wblk[sl]], dsem=s_w[sl])
                fns = []
                for dc in range(8):
                    fns.append(lambda e, dc=dc: e.matmul(ps0[sl][:], lhsT=cond_bc[:, dc, :], rhs=wblk[sl][:, dc, :],
                                                         start=(dc == 0), stop=False))
                fns.append(lambda e: e.matmul(ps0[sl][:], lhsT=ones1[0:1, :], rhs=bada[0:1, cb * 512:(cb + 1) * 512],
                                              start=False, stop=True))
                kb.group("pe", fns, reads=[b_cbc, b_wblk[sl], b_ones1, b_bada], writes=[b_ps0[sl]])
                addv = 1.0 if (cb // 2) in (1, 4) else 0.0
                kb.op("dve", lambda e: e.tensor_scalar(out=mod_bc[:, cb * 512:(cb + 1) * 512], in0=ps0[sl][:], scalar1=addv,
                                                       scalar2=None, op0=ALU.add), reads=[b_ps0[sl]], writes=[b_mod])
            if mod_dump is not None:
                s_d = kb.sem("p0d")
                kb.op("sp", lambda e: e.dma_start(out=mod_dump[:, :], in_=mod_bc[:]), reads=[b_mod], dsem=s_d)
            kb.barrier()
        SH1, SC1, G1, SH2, SC2, G2 = [mod_bc[:, i * D:(i + 1) * D] for i in range(6)]

        def ln_tile_g(src, b_src, dst, b_dst, st_, b_st_, mv_, b_mv_, rstd_, b_rstd_, eps, mul_ap, add_ap, b_aff):
            kb.op("dve", lambda e: e.bn_stats(out=st_[:, 0, :], in_=src[:, 0:512]), reads=[b_src], writes=[b_st_]); yield
            kb.op("dve", lambda e: e.bn_stats(out=st_[:, 1, :], in_=src[:, 512:1024]), reads=[b_src], writes=[b_st_]); yield
            kb.op("dve", lambda e: e.bn_aggr(out=mv_[:], in_=st_[:].rearrange("p a b -> p (a b)")), reads=[b_st_], writes=[b_mv_]); yield
            kb.op("pool", lambda e: e.tensor_scalar(out=rstd_[:], in0=mv_[:, 1:2], scalar1=1.0, scalar2=float(eps), op0=ALU.mult, op1=ALU.add),
                  reads=[b_mv_], writes=[b_rstd_]); yield
            kb.op("pool", lambda e: e.tensor_tensor(out=rstd_[:], in0=rstd_[:], in1=mhalf[:, 0:1], op=ALU.pow), reads=[b_rstd_, b_mhalf], writes=[b_rstd_]); yield
            kb.op("dve", lambda e: e.tensor_scalar(out=src[:], in0=src[:], scalar1=mv_[:, 0:1], scalar2=rstd_[:, 0:1], op0=ALU.subtract, op1=ALU.mult),
                  reads=[b_src, b_mv_, b_rstd_], writes=[b_src]); yield
            kb.op("dve", lambda e: e.tensor_tensor(out=src[:], in0=src[:], in1=mul_ap, op=ALU.mult), reads=[b_src, b_aff], writes=[b_src]); yield
            kb.op("dve", lambda e: e.tensor_tensor(out=dst[:], in0=src[:], in1=add_ap, op=ALU.add), reads=[b_src, b_aff], writes=[b_dst]); yield

        def ln_tile(src, b_src, dst, b_dst, st_, b_st_, mv_, b_mv_, rstd_, b_rstd_, eps, mul_ap, add_ap, b_aff, mul_first_pool):
            kb.op("dve", lambda e: e.bn_stats(out=st_[:, 0, :], in_=src[:, 0:512]), reads=[b_src], writes=[b_st_])
            kb.op("dve", lambda e: e.bn_stats(out=st_[:, 1, :], in_=src[:, 512:1024]), reads=[b_src], writes=[b_st_])
            kb.op("dve", lambda e: e.bn_aggr(out=mv_[:], in_=st_[:].rearrange("p a b -> p (a b)")), reads=[b_st_], writes=[b_mv_])
            kb.op("pool", lambda e: e.tensor_scalar(out=rstd_[:], in0=mv_[:, 1:2], scalar1=1.0, scalar2=float(eps), op0=ALU.mult, op1=ALU.add),
                  reads=[b_mv_], writes=[b_rstd_])
            kb.op("pool", lambda e: e.tensor_tensor(out=rstd_[:], in0=rstd_[:], in1=mhalf[:, 0:1], op=ALU.pow), reads=[b_rstd_, b_mhalf], writes=[b_rstd_])
            kb.op("dve", lambda e: e.tensor_scalar(out=src[:], in0=src[:], scalar1=mv_[:, 0:1], scalar2=rstd_[:, 0:1], op0=ALU.subtract, op1=ALU.mult),
                  reads=[b_src, b_mv_, b_rstd_], writes=[b_src])
            kb.op("dve", lambda e: e.tensor_tensor(out=src[:], in0=src[:], in1=mul_ap, op=ALU.mult), reads=[b_src, b_aff], writes=[b_src])
            kb.op("dve", lambda e: e.tensor_tensor(out=dst[:], in0=src[:], in1=add_ap, op=ALU.add), reads=[b_src, b_aff], writes=[b_dst])

        if stop_after >= 1:
          with ExitStack() as es:
            win = T(es, "win", [128, 8, 4096], BF16); b_win = Buf()
            xt = [T(es, f"xt{i}", [128, D], F32) for i in range(2)]; b_xt = [Buf(), Buf()]
            tab = [T(es, f"tab{i}", [128, 256], F32) for i in range(2)]; b_tab = [Buf(), Buf()]
            st_ = [T(es, f"st{i}", [128, 2, 6], F32) for i in range(2)]; mv_ = [T(es, f"mv{i}", [128, 2], F32) for i in range(2)]; rstd_ = [T(es, f"rstd{i}", [128, 1], F32) for i in range(2)]
            b_st_ = [Buf(), Buf()]; b_mv_ = [Buf(), Buf()]; b_rstd_ = [Buf(), Buf()]
            xn_ = [T(es, f"xn{i}", [128, D], F32) for i in range(2)]; b_xn_ = [Buf(), Buf()]
            ub = [T(es, f"ub{i}", [128, D], BF16) for i in range(2)]; b_ub = [Buf(), Buf()]
            uT = [T(es, f"uT{i}", [128, 8, 512], BF16) for i in range(2)]; b_uT = [Buf(), Buf()]
            qk_ = [T(es, f"qk{i}", [128, 1280], F32) for i in range(2)]; b_qk_ = [Buf(), Buf()]

            ss_ = [T(es, f"ss{i}", [128, 10], F32) for i in range(2)]; b_ss_ = [Buf(), Buf()]
            rs_ = [T(es, f"rs{i}", [128, 10], F32) for i in range(2)]; b_rs_ = [Buf(), Buf()]
            qn_ = [T(es, f"qn{i}", [128, 1280], F32) for i in range(2)]; b_qn_ = [Buf(), Buf()]
            t1_ = [T(es, f"t1{i}", [128, 1280], F32) for i in range(2)]; b_t1_ = [Buf(), Buf()]
            t2_ = [T(es, f"t2{i}", [128, 1280], F32) for i in range(2)]; b_t2_ = [Buf(), Buf()]
            rot_ = [T(es, f"rot{i}", [128, 1280], BF16) for i in range(2)]; b_rot_ = [Buf(), Buf()]
            gain = T(es, "gain", [128, 1280], F32); b_gain = Buf()
            fst = [T(es, f"fst{i}", [128, 512], BF16) for i in range(2)]; b_fst = [Buf(), Buf()]
            vst = [T(es, f"vst{i}", [128, 256], BF16) for i in range(2)]; b_vst = [Buf(), Buf()]
            qTs_ = [T(es, f"qTs{i}", [128, 8, 512], BF16) for i in range(2)]; b_qTs_ = [Buf(), Buf()]
            kTs_ = [T(es, f"kTs{i}", [128, 2, 512], BF16) for i in range(2)]; b_kTs_ = [Buf(), Buf()]
            gst = [T(es, f"gst{i}", [128, 4, 512], BF16) for i in range(2)]; b_gst = [Buf(), Buf()]
            pst = P(es, "pst", [128, 8, 128], BF16); b_pst = Buf()
            psz = P(es, "psz", [128, 2048], F32); b_psz = [Buf() for _ in range(4)]
            pstq = P(es, "pstq", [128, 8, 128], BF16); b_pstq = Buf()
            pstk = P(es, "pstk", [128, 2, 128], BF16); b_pstk = Buf()
            psg = P(es, "psg", [128, 512], F32); b_psg = Buf()
            s_win = kb.sem("p1win"); s_x = [kb.sem("p1x0"), kb.sem("p1x1")]; s_g = kb.sem("p1g")
            s_f = [kb.sem("p1f0"), kb.sem("p1f1")]; s_v = [kb.sem("p1v0"), kb.sem("p1v1")]
            s_q = kb.sem("p1q"); s_k = kb.sem("p1k"); s_gs = [kb.sem("p1gs0"), kb.sem("p1gs1")]
            w_in_v = I["w_in"].rearrange("(dc p) c -> p dc c", p=128)
            for dc in range(8):
                for hh in range(2):
                    kb.op("pool", lambda e, dc=dc, hh=hh: e.dma_start(out=win[:, dc, hh * 2048:(hh + 1) * 2048],
                                                                      in_=w_in_v[:, dc, hh * 2048:(hh + 1) * 2048]),
                          writes=[b_win], dsem=s_win)
            for h in range(10):
                src = I["q_norm_g"] if h < 8 else I["k_norm_g"]
                kb.op("sp", lambda e, h=h, src=src: e.dma_start(out=gain[:, h * 128:(h + 1) * 128],
                                                               in_=src[0:1, :].partition_broadcast(128)),
                      writes=[b_gain], dsem=s_g)
            kb.op("dve", lambda e: e.tensor_scalar(out=gain[:, 0:1024], in0=gain[:, 0:1024], scalar1=float(128.0 ** -0.5),
                                                   scalar2=None, op0=ALU.mult), reads=[b_gain], writes=[b_gain])
            x_v = I["x"].rearrange("(t p) d -> t p d", p=128)
            tab_v = I["rope_tab"].rearrange("(t p) d -> t p d", p=128)
            qT_v = qT_dram.rearrange("h d s -> d h s")
            kT_v = kT_dram.rearrange("h d s -> d h s")
            gT_v = gT_dram.rearrange("(g p) s -> p g s", p=128)

            s_tb = [kb.sem("p1t0"), kb.sem("p1t1")]

            def load_xt(t):
                sl = t % 2
                kb.op("sp", lambda e: e.dma_start(out=xt[sl][:], in_=x_v[t]), writes=[b_xt[sl]], dsem=s_x[sl])

            def load_tab(t):
                sl = t % 2
                kb.op("sp", lambda e: e.dma_start(out=tab[sl][:], in_=tab_v[t]), writes=[b_tab[sl]], dsem=s_tb[sl])

            load_xt(0); load_xt(1); load_tab(0); load_tab(1)
            def tile1(t):
                    sl = t % 2; blk = t // 4; sl4 = t % 4; ub_ = ub[sl]; uT_ = uT[blk % 2]
                    st = st_[sl]; mv = mv_[sl]; rstd = rstd_[sl]; b_st = b_st_[sl]; b_mv = b_mv_[sl]; b_rstd = b_rstd_[sl]
                    xn = xn_[sl]; b_xn = b_xn_[sl]; qk = qk_[sl]; b_qk = b_qk_[sl]; ss = ss_[sl]; b_ss = b_ss_[sl]; rs = rs_[sl]; b_rs = b_rs_[sl]
                    qn = qn_[sl]; b_qn = b_qn_[sl]; t1 = t1_[sl]; b_t1 = b_t1_[sl]; t2 = t2_[sl]; b_t2 = b_t2_[sl]; rot = rot_[sl]; b_rot = b_rot_[sl]
                    sq = t2; b_sq = b_t2
                    qTs = qTs_[blk % 2]; b_qTs = b_qTs_[blk % 2]; kTs = kTs_[blk % 2]; b_kTs = b_kTs_[blk % 2]
                    x_ = xt[sl]
                    kb.op("dve", lambda e: e.bn_stats(out=st[:, 0, :], in_=x_[:, 0:512]), reads=[b_xt[sl]], writes=[b_st])
                    yield
                    kb.op("dve", lambda e: e.bn_stats(out=st[:, 1, :], in_=x_[:, 512:1024]), reads=[b_xt[sl]], writes=[b_st])
                    yield
                    kb.op("dve", lambda e: e.bn_aggr(out=mv[:], in_=st[:].rearrange("p a b -> p (a b)")), reads=[b_st], writes=[b_mv])
                    yield
                    kb.op("pool", lambda e: e.tensor_scalar(out=rstd[:], in0=mv[:, 1:2], scalar1=1.0, scalar2=1e-6, op0=ALU.mult, op1=ALU.add),
                          reads=[b_mv], writes=[b_rstd])
                    yield
                    kb.op("pool", lambda e: e.tensor_tensor(out=rstd[:], in0=rstd[:], in1=mhalf[:, 0:1], op=ALU.pow),
                          reads=[b_rstd, b_mhalf], writes=[b_rstd])
                    yield
                    kb.op("dve", lambda e: e.tensor_scalar(out=xn[:], in0=x_[:], scalar1=mv[:, 0:1], scalar2=rstd[:, 0:1],
                                                           op0=ALU.subtract, op1=ALU.mult), reads=[b_xt[sl], b_mv, b_rstd], writes=[b_xn])
                    if t + 2 < NT:
                        load_xt(t + 2)
                    yield
                    kb.op("dve", lambda e: e.tensor_tensor(out=xn[:], in0=xn[:], in1=SC1, op=ALU.mult), reads=[b_xn, b_mod], writes=[b_xn])
                    yield
                    kb.op("dve", lambda e: e.tensor_tensor(out=ub_[:], in0=xn[:], in1=SH1, op=ALU.add), reads=[b_xn, b_mod], writes=[b_ub[sl]])
                    yield
                    kb.group("pe", [lambda e, dc=dc: e.transpose(out=pst[:, dc, :], in_=ub_[:, dc * 128:(dc + 1) * 128], identity=ident[:])
                                    for dc in range(8)], reads=[b_ub[sl], b_ident], writes=[b_pst])
                    kb.op("act", lambda e: e.copy(out=uT_[:, :, sl4 * 128:(sl4 + 1) * 128], in_=pst[:]), reads=[b_pst], writes=[b_uT[blk % 2]])
                    yield
                    for cc in range(4):
                        kb.group("pe", [lambda e, dc=dc, cc=cc: e.matmul(psz[:, cc * 512:(cc + 1) * 512], lhsT=uT_[:, dc, sl4 * 128:(sl4 + 1) * 128],
                                                                         rhs=win[:, dc, cc * 512:(cc + 1) * 512], start=(dc == 0), stop=(dc == 7))
                                        for dc in range(8)], reads=[b_uT[blk % 2], b_win], writes=[b_psz[cc]])
                    kb.op("act", lambda e: e.copy(out=fst[sl][:], in_=psz[:, 0:512]), reads=[b_psz[0]], writes=[b_fst[sl]])
                    kb.op("sp", lambda e: e.dma_start(out=f_dram[t * 128:(t + 1) * 128, :], in_=fst[sl][:]), reads=[b_fst[sl]], dsem=s_f[sl])
                    kb.op("act", lambda e: e.copy(out=vst[sl][:], in_=psz[:, 1792:2048]), reads=[b_psz[3]], writes=[b_vst[sl]])
                    kb.op("sp", lambda e: e.dma_start(out=v_dram[t * 128:(t + 1) * 128, :], in_=vst[sl][:]), reads=[b_vst[sl]], dsem=s_v[sl])
                    kb.op("act", lambda e: e.copy(out=qk[:], in_=psz[:, 512:1792]), reads=[b_psz[1], b_psz[2], b_psz[3]], writes=[b_qk])
                    yield
                    kb.op("dve", lambda e: e.tensor_tensor(out=sq[:], in0=qk[:], in1=qk[:], op=ALU.mult), reads=[b_qk], writes=[b_sq])
                    yield
                    kb.op("dve", lambda e: e.tensor_reduce(out=ss[:], in_=sq[:].rearrange("p (h d) -> p h d", d=128), axis=AX.X, op=ALU.add),
                          reads=[b_sq], writes=[b_ss])
                    yield
                    kb.op("pool", lambda e: e.tensor_scalar(out=rs[:], in0=ss[:], scalar1=1.0 / 128.0, scalar2=1e-6, op0=ALU.mult, op1=ALU.add),
                          reads=[b_ss], writes=[b_rs])
                    yield
                    kb.op("pool", lambda e: e.tensor_tensor(out=rs[:], in0=rs[:], in1=mhalf[:, 0:10], op=ALU.pow), reads=[b_rs, b_mhalf], writes=[b_rs])
                    yield
                    kb.op("dve", lambda e: e.tensor_tensor(out=qn[:].rearrange("p (h d) -> p h d", d=128), in0=qk[:].rearrange("p (h d) -> p h d", d=128),
                                                           in1=rs[:].unsqueeze(2).to_broadcast([128, 10, 128]), op=ALU.mult),
                          reads=[b_qk, b_rs], writes=[b_qn])
                    yield
                    kb.op("dve", lambda e: e.tensor_tensor(out=qn[:], in0=qn[:], in1=gain[:], op=ALU.mult), reads=[b_qn, b_gain], writes=[b_qn])
                    yield
                    tb_ = tab[sl]
                    kb.op("dve", lambda e: e.tensor_tensor(out=t1[:].rearrange("p (h d) -> p h d", d=128), in0=qn[:].rearrange("p (h d) -> p h d", d=128),
                                                           in1=tb_[:, 0:128].unsqueeze(1).to_broadcast([128, 10, 128]), op=ALU.mult),
                          reads=[b_qn, b_tab[sl]], writes=[b_t1])
                    yield
                    qn5 = qn[:].rearrange("p (h a t d) -> p h a t d", h=10, a=2, t=2, d=32)
                    t25 = t2[:].rearrange("p (h a t d) -> p h a t d", h=10, a=2, t=2, d=32)
                    sn4 = tb_[:, 128:256].rearrange("p (a t d) -> p a t d", a=2, t=2, d=32)
                    for half in range(2):
                        kb.op("pool", lambda e, half=half: e.tensor_tensor(out=t25[:, :, :, half, :], in0=qn5[:, :, :, 1 - half, :],
                                                                           in1=sn4[:, :, half, :].unsqueeze(1).to_broadcast([128, 10, 2, 32]), op=ALU.mult),
                              reads=[b_qn, b_tab[sl]], writes=[b_t2])
                        yield
                    if t + 2 < NT:
                        load_tab(t + 2)
                    kb.op("dve", lambda e: e.tensor_tensor(out=rot[:], in0=t1[:], in1=t2[:], op=ALU.add), reads=[b_t1, b_t2], writes=[b_rot])
                    yield
                    kb.group("pe", [lambda e, h=h: e.transpose(out=pstq[:, h, :], in_=rot[:, h * 128:(h + 1) * 128], identity=ident[:]) for h in range(8)],
                             reads=[b_rot, b_ident], writes=[b_pstq])
                    kb.group("pe", [lambda e, h=h: e.transpose(out=pstk[:, h, :], in_=rot[:, (8 + h) * 128:(9 + h) * 128], identity=ident[:]) for h in range(2)],
                             reads=[b_rot, b_ident], writes=[b_pstk])
                    kb.op("act", lambda e: e.copy(out=qTs[:, :, sl4 * 128:(sl4 + 1) * 128], in_=pstq[:]), reads=[b_pstq], writes=[b_qTs])
                    kb.op("act", lambda e: e.copy(out=kTs[:, :, sl4 * 128:(sl4 + 1) * 128], in_=pstk[:]), reads=[b_pstk], writes=[b_kTs])
                    yield
                    if sl4 == 3:
                        kb.op("sp", lambda e: e.dma_start(out=qT_v[:, :, blk * 512:(blk + 1) * 512], in_=qTs[:]), reads=[b_qTs], dsem=s_q)
                        yield
                        kb.op("sp", lambda e: e.dma_start(out=kT_v[:, :, blk * 512:(blk + 1) * 512], in_=kTs[:]), reads=[b_kTs], dsem=s_k)
                        yield
                        for gq in range(4):
                            gsl = gq % 2
                            for gi in range(4):
                                gc = gq * 4 + gi
                                kb.group("pe", [lambda e, dc=dc, gc=gc: e.matmul(psg[:], lhsT=win[:, dc, 2048 + gc * 128:2048 + (gc + 1) * 128],
                                                                                 rhs=uT_[:, dc, :], start=(dc == 0), stop=(dc == 7)) for dc in range(8)],
                                         reads=[b_uT[blk % 2], b_win], writes=[b_psg])
                                kb.op("act", lambda e, gi=gi: e.activation(out=gst[gsl][:, gi, :], in_=psg[:], func=AF.Sigmoid),
                                      reads=[b_psg], writes=[b_gst[gsl]])
                                yield
                            kb.op("sp", lambda e, gq=gq: e.dma_start(out=gT_v[:, gq * 4:(gq + 1) * 4, blk * 512:(blk + 1) * 512], in_=gst[gsl][:]),
                                  reads=[b_gst[gsl]], dsem=s_gs[gsl])
                            yield
            run_interleaved((tile1(t) for t in range(NT)), debug.get("il1", 2))
            kb.barrier()


        if stop_after >= 2:
          with ExitStack() as es:
            E = T(es, "fftE", [128, 64, 2, 128], BF16); b_E = Buf()
            f1 = [T(es, f"f1_{i}", [128, 8, 512], BF16) for i in range(2)]; b_f1 = [Buf(), Buf()]
            Ysb = [T(es, f"Ysb{i}", [128, 8, 2, 512], BF16) for i in range(2)]; b_Ysb = [Buf(), Buf()]
            psY = [P(es, f"psY{i}", [128, 512], F32) for i in range(4)]; b_psY = [Buf() for _ in range(4)]
            s_E = kb.sem("p2E"); s_f1 = [kb.sem("p2f0"), kb.sem("p2f1")]; s_y = [kb.sem("p2y0"), kb.sem("p2y1")]
            kb.op("sp", lambda e: e.dma_start(out=E[:], in_=I["fft_e"][:, :, :, :]), writes=[b_E], dsem=s_E)
            f1_v = f_dram.rearrange("(p n) c -> p n c", n=64)
            cnt = 0
            for ch in range(8):
                sl = ch % 2
                kb.op("sp", lambda e: e.dma_start(out=f1[sl][:], in_=f1_v[:, ch * 8:(ch + 1) * 8, :]), writes=[b_f1[sl]], dsem=s_f1[sl])
                for j in range(8):
                    n2 = ch * 8 + j
                    for ri in range(2):
                        pb = cnt % 4; cnt += 1
                        kb.op("pe", lambda e: e.matmul(psY[pb][:], lhsT=E[:, n2, ri, :], rhs=f1[sl][:, j, :], start=True, stop=True),
                              reads=[b_E, b_f1[sl]], writes=[b_psY[pb]])
                        if ri == 0:
                            kb.op("act", lambda e: e.copy(out=Ysb[sl][:, j, ri, :], in_=psY[pb][:]), reads=[b_psY[pb]], writes=[b_Ysb[sl]])
                        else:
                            kb.op("dve", lambda e: e.tensor_copy(out=Ysb[sl][:, j, ri, :], in_=psY[pb][:]), reads=[b_psY[pb]], writes=[b_Ysb[sl]])
                for ri in range(2):
                    kb.op("sp", lambda e, ri=ri: e.dma_start(out=Ys_dram[ri, ch * 8:(ch + 1) * 8].rearrange("n k c -> k n c"), in_=Ysb[sl][:, :, ri, :]),
                          reads=[b_Ysb[sl]], dsem=s_y[sl])
            kb.barrier()
          with ExitStack() as es:
            W3 = T(es, "W3", [128, 128], BF16); b_W3 = Buf()
            Y2 = [T(es, f"Y2_{i}", [128, 8, 256], BF16) for i in range(2)]; b_Y2 = [Buf(), Buf()]
            XT = T(es, "XT", [128, 2, 2, S], BF16); b_XT = Buf()
            psX = [P(es, f"psX{i}", [128, 4, 128], F32) for i in range(2)]; b_psX = [Buf(), Buf()]
            s_w3 = kb.sem("p2w3"); s_y2 = [kb.sem("p2y20"), kb.sem("p2y21")]; s_xo = kb.sem("p2xo")
            kb.op("sp", lambda e: e.dma_start(out=W3[:], in_=I["fft_w3"][:, :]), writes=[b_W3], dsem=s_w3)
            Y2_v = Ys_dram.rearrange("r n k c -> (r n) k c")
            cnt = 0
            for pp in range(2):
                for kc in range(16):
                    sl = kc % 2
                    kb.op("sp", lambda e: e.dma_start(out=Y2[sl][:], in_=Y2_v[:, kc * 8:(kc + 1) * 8, pp * 256:(pp + 1) * 256]),
                          writes=[b_Y2[sl]], dsem=s_y2[sl])
                    for ccl in range(2):
                        for g in range(2):
                            pb = cnt % 2; cnt += 1
                            kb.group("pe", [lambda e, j=j: e.matmul(psX[pb][:, j, :], lhsT=Y2[sl][:, g * 4 + j, ccl * 128:(ccl + 1) * 128], rhs=W3[:],
                                                                    start=True, stop=True) for j in range(4)],
                                     reads=[b_Y2[sl], b_W3], writes=[b_psX[pb]])
                            k10 = kc * 8 + g * 4
                            o_ap = XT[:, ccl, :, :].rearrange("c x (k2 k1) -> c x k2 k1", k1=128)[:, :, :, k10:k10 + 4]
                            i_ap = psX[pb][:].rearrange("c j (x k2) -> c x k2 j", x=2)
                            if cnt % 2 == 0:
                                kb.op("act", lambda e: e.copy(out=o_ap, in_=i_ap), reads=[b_psX[pb]], writes=[b_XT])
                            else:
                                kb.op("dve", lambda e: e.tensor_copy(out=o_ap, in_=i_ap), reads=[b_psX[pb]], writes=[b_XT])
                for ccl in range(2):
                    for xr in range(2):
                        r0 = xr * 512 + (pp * 2 + ccl) * 128
                        kb.op("sp", lambda e, ccl=ccl, xr=xr, r0=r0: e.dma_start(out=XT_dram[r0:r0 + 128, :], in_=XT[:, ccl, xr, :]),
                              reads=[b_XT], dsem=s_xo)
            kb.barrier()

        if stop_after >= 4:
          with ExitStack() as es:
            kT = T(es, "kT", [128, 2, S], BF16); b_kT = Buf()
            V = T(es, "V", [128, NT, 256], BF16); b_V = Buf()
            ones = T(es, "ones", [128, 128], BF16); b_ones = Buf()
            gqk = T(es, "gqk", [128, 2, 128], F32); b_gqk = Buf()
            gmx = T(es, "gmx", [128, 2], F32); b_gmx = Buf()
            nbias = T(es, "nbias", [128, 1], F32); b_nb = Buf()
            qb = [T(es, f"qb{i}", [128, 8, 512], BF16) for i in range(2)]; b_qb = [Buf(), Buf()]
            PT = [T(es, f"PT{i}", [128, 1024], BF16) for i in range(4)]; b_PT = [Buf() for _ in range(4)]
            acc1 = T(es, "acc1", [128, 512], F32); b_acc1 = Buf()
            Osb = T(es, "Osb", [128, 512], F32); b_Osb = Buf()
            ones32 = T(es, "ones32", [128, 32], BF16); b_ones32 = Buf()
            kb.op("pool", lambda e: e.memset(ones32[:], 1.0), writes=[b_ones32])
            onesF = T(es, "onesF", [128, 128], F32); b_onesF = Buf()
            rec = T(es, "rec", [128, 512], F32); b_rec = Buf()
            OTs = [T(es, f"OTs{i}", [128, 512], BF16) for i in range(2)]; b_OTs = [Buf(), Buf()]
            psS = [P(es, f"psS{i}", [128, 1024], F32) for i in range(3)]; b_psS = [Buf() for _ in range(3)]
            psO = [P(es, "psO0", [128, 512], F32)]; b_psO = [Buf()]
            psL = [P(es, "psL0", [128, 512], F32)]; b_psL = [Buf()]
            s_kv = kb.sem("p4kv"); s_qb = [kb.sem("p4q0"), kb.sem("p4q1")]; s_o = [kb.sem("p4o0"), kb.sem("p4o1")]; s_gq = kb.sem("p4g")
            kb.op("sp", lambda e: e.dma_start(out=kT[:], in_=kT_dram.rearrange("h d s -> d h s")), writes=[b_kT], dsem=s_kv)
            kb.op("sp", lambda e: e.dma_start(out=V[:], in_=v_dram.rearrange("(t p) c -> p t c", p=128)), writes=[b_V], dsem=s_kv)
            kb.op("pool", lambda e: e.memset(onesF[:], 1.0 / 32.0), writes=[b_onesF])
            kb.op("sp", lambda e: e.dma_start(out=gqk[:, 0, :], in_=I["q_norm_g"][0:1, :].partition_broadcast(128)), writes=[b_gqk], dsem=s_gq)
            kb.op("sp", lambda e: e.dma_start(out=gqk[:, 1, :], in_=I["k_norm_g"][0:1, :].partition_broadcast(128)), writes=[b_gqk], dsem=s_gq)
            gmn = T(es, "gmn", [128, 2], F32); b_gmn = Buf()
            kb.op("dve", lambda e: e.tensor_reduce(out=gmx[:], in_=gqk[:], axis=AX.X, op=ALU.max), reads=[b_gqk], writes=[b_gmx])
            kb.op("dve", lambda e: e.tensor_reduce(out=gmn[:], in_=gqk[:], axis=AX.X, op=ALU.min, negate=True), reads=[b_gqk], writes=[b_gmn])
            kb.op("dve", lambda e: e.tensor_tensor(out=gmx[:], in0=gmx[:], in1=gmn[:], op=ALU.max), reads=[b_gmx, b_gmn], writes=[b_gmx])
            kb.op("dve", lambda e: e.tensor_tensor(out=nbias[:], in0=gmx[:, 0:1], in1=gmx[:, 1:2], op=ALU.mult), reads=[b_gmx], writes=[b_nb])
            kb.op("dve", lambda e: e.tensor_scalar(out=nbias[:], in0=nbias[:], scalar1=float(-(128.0 ** 0.5)), scalar2=None, op0=ALU.mult),
                  reads=[b_nb], writes=[b_nb])
            qT_v2 = qT_dram.rearrange("h d s -> d h s")
            kb.op("sp", lambda e: e.dma_start(out=qb[0][:], in_=qT_v2[:, :, 0:512]), writes=[b_qb[0]], dsem=s_qb[0])
            pc_jobs = []
            if stop_after >= 7:
                stg = [T(es, f"stg{i}", [128, 4096], F32) for i in range(2)]; b_stg = [Buf(), Buf()]
                stb = [T(es, f"stb{i}", [128, 4096], BF16) for i in range(2)]; b_stb = [Buf(), Buf()]
                s_ci = [kb.sem("p3i0"), kb.sem("p3i1")]; s_co = [kb.sem("p3o0"), kb.sem("p3o1")]
                for ex in range(NE):
                    gsrc = I["w_gate_up"][ex].rearrange("(r p) f -> p r f", p=128)
                    dsrc = I["w_down"][ex].rearrange("(r p) f -> p r f", p=128)
                    for q in range(4):
                        pc_jobs.append((gsrc[:, q * 2:(q + 1) * 2, :], wgu_bf[ex * 128:(ex + 1) * 128, q * 4096:(q + 1) * 4096], 2, 128, 4096))
                    for q in range(2):
                        pc_jobs.append((dsrc[:, q * 4:(q + 1) * 4, :], wd_bf[ex * 128:(ex + 1) * 128, q * 4096:(q + 1) * 4096], 4, 128, 4096))
                pc_jobs.append((I["b_down"][:, :], bd_bf[:, :], 1, NE, D))
            pc_state = [0]

            def precast_step(n):
                for _ in range(n):
                    ci = pc_state[0]
                    if ci >= len(pc_jobs):
                        return
                    src_ap, dst_ap, nr, npart, w_ = pc_jobs[ci]
                    sl = ci % 2
                    o_ap = stg[sl][0:npart, 0:w_]
                    if nr > 1:
                        o_ap = o_ap.rearrange("p (r f) -> p r f", r=nr)
                    kb.op("sp", lambda e: e.dma_start(out=o_ap, in_=src_ap), writes=[b_stg[sl]], dsem=s_ci[sl])
                    ce = ("dve", "pool")[ci % 2]
                    kb.op(ce, lambda e: e.tensor_copy(out=stb[sl][0:npart, 0:w_], in_=stg[sl][0:npart, 0:w_]), reads=[b_stg[sl]], writes=[b_stb[sl]])
                    kb.op("sp", lambda e: e.dma_start(out=dst_ap, in_=stb[sl][0:npart, 0:w_]), reads=[b_stb[sl]], dsem=s_co[sl])
                    pc_state[0] += 1
            i_u = 0
            nblk4 = debug.get("p4_blocks", NB)
            NU = NT // 2
            for blk in range(nblk4):
                bs = blk % 2
                if blk + 1 < nblk4:
                    kb.op("sp", lambda e: e.dma_start(out=qb[1 - bs][:], in_=qT_v2[:, :, (blk + 1) * 512:(blk + 2) * 512]),
                          writes=[b_qb[1 - bs]], dsem=s_qb[1 - bs])
                for h in range(8):
                    kvh = h // 4
                    base = i_u

                    def emit_S(u):
                        ii = base + u
                        kb.group("pe", [lambda e, c=c: e.matmul(psS[ii % 3][:, c * 512:(c + 1) * 512], lhsT=kT[:, kvh, (2 * u + c) * 128:(2 * u + c + 1) * 128],
                                                                rhs=qb[bs][:, h, :], start=True, stop=True) for c in range(2)],
                                 reads=[b_kT, b_qb[bs]], writes=[b_psS[ii % 3]])
                    emit_S(0); emit_S(1)
                    for u in range(NU):
                        ii = base + u
                        pt = PT[ii % 4]
                        kb.op("act", lambda e: e.activation(out=pt[:], in_=psS[ii % 3][:], func=AF.Exp, bias=nbias[:, 0:1], scale=1.0),
                              reads=[b_psS[ii % 3], b_nb], writes=[b_PT[ii % 4]])
                        if u + 2 < NU:
                            emit_S(u + 2)
                        first = (u == 0); last = (u == NU - 1)
                        kb.group("pe", [lambda e, c=c: e.matmul(psO[0][:], lhsT=V[:, 2 * u + c, kvh * 128:(kvh + 1) * 128], rhs=pt[:, c * 512:(c + 1) * 512],
                                                                start=(first and c == 0), stop=(last and c == 1)) for c in range(2)],
                                 reads=[b_V, b_PT[ii % 4]], writes=([b_psO[0]] if (first or last) else []))
                        if u % 2 == 1:
                            ptp = PT[(ii - 1) % 4]
                            srcs = [ptp[:, 0:512], ptp[:, 512:1024], pt[:, 0:512], pt[:, 512:1024]]
                            kb.group("pe", [lambda e, jc=jc: e.matmul(psL[0][32 * jc:32 * (jc + 1), :], lhsT=ones32[:, :], rhs=srcs[jc],
                                                                      start=(u == 1), stop=(u == NU - 1), tile_position=(0, 32 * jc)) for jc in range(4)],
                                     reads=[b_ones32, b_PT[(ii - 1) % 4], b_PT[ii % 4]], writes=([b_psL[0]] if (u == 1 or u == NU - 1) else []))
                    i_u += NU
                    kb.op("dve", lambda e: e.tensor_copy(out=acc1[:], in_=psL[0][:]), reads=[b_psL[0]], writes=[b_acc1])
                    kb.op("dve", lambda e: e.tensor_copy(out=Osb[:], in_=psO[0][:]), reads=[b_psO[0]], writes=[b_Osb])
                    kb.op("pe", lambda e: e.matmul(psL[0][:], lhsT=onesF[:], rhs=acc1[:], start=True, stop=True), reads=[b_onesF, b_acc1], writes=[b_psL[0]])
                    jo = (blk * 8 + h) % 2
                    kb.op("dve", lambda e: e.reciprocal(out=rec[:], in_=psL[0][:]), reads=[b_psL[0]], writes=[b_rec])
                    kb.op("dve", lambda e: e.tensor_tensor(out=OTs[jo][:], in0=Osb[:], in1=rec[:], op=ALU.mult), reads=[b_Osb, b_rec], writes=[b_OTs[jo]])
                    kb.op("sp", lambda e: e.dma_start(out=OT_dram[h * 128:(h + 1) * 128, blk * 512:(blk + 1) * 512], in_=OTs[jo][:]),
                          reads=[b_OTs[jo]], dsem=s_o[jo])
                    precast_step(2)
            precast_step(len(pc_jobs))
            kb.barrier()

        if stop_after >= 5:
          with ExitStack() as es:
            Wf = T(es, "Wf", [128, 8, D], BF16); b_Wf = Buf()
            wao = T(es, "wao", [128, 8, D], BF16); b_wao = Buf()
            wout = T(es, "wout", [128, 8, D], BF16); b_wout = Buf()
            lng = T(es, "lng", [128, D], F32); lnb = T(es, "lnb", [128, D], F32); b_ln = Buf()
            s_w5 = kb.sem("p5w"); s_ln = kb.sem("p5ln")
            kb.op("sp", lambda e: e.dma_start(out=lng[:], in_=I["ln1_g"][0:1, :].partition_broadcast(128)), writes=[b_ln], dsem=s_ln)
            kb.op("sp", lambda e: e.dma_start(out=lnb[:], in_=I["ln1_b"][0:1, :].partition_broadcast(128)), writes=[b_ln], dsem=s_ln)
            wao_v = I["w_attn_o"].rearrange("(kc p) d -> p kc d", p=128); wout_v = I["w_out"].rearrange("(kc p) d -> p kc d", p=128)
            for kc in range(8):
                kb.op("pool", lambda e, kc=kc: e.dma_start(out=wao[:, kc, :], in_=wao_v[:, kc, :]), writes=[b_wao], dsem=s_w5)
                kb.op("pool", lambda e, kc=kc: e.dma_start(out=wout[:, kc, :], in_=wout_v[:, kc, :]), writes=[b_wout], dsem=s_w5)
            psF = [P(es, f"psF{i}", [128, 512], F32) for i in range(2)]; b_psF = [Buf(), Buf()]
            psA = [P(es, f"psA{i}", [128, 512], F32) for i in range(2)]; b_psA = [Buf(), Buf()]
            psH = P(es, "psH", [128, D], F32); b_psH = Buf()
            with ExitStack() as es2:
                wf_sb = T(es2, "wf_sb", [128, 4, D], BF16); b_wf = Buf()
                dftc = T(es2, "dftc", [128, 2, 128], BF16); b_dftc = Buf()
                s_f5 = kb.sem("p5f")
                kb.op("pool", lambda e: e.dma_start(out=wf_sb[:], in_=I["w_fourier"].rearrange("(g p) d -> p g d", p=128)), writes=[b_wf], dsem=s_f5)
                kb.op("sp", lambda e: e.dma_start(out=dftc[:], in_=I["dft_c"][:, :, :]), writes=[b_dftc], dsem=s_f5)
                cnt = 0
                for xr in range(2):
                    for g in range(4):
                        for half in range(2):
                            pb = cnt % 2; cnt += 1
                            kb.op("pe", lambda e: e.matmul(psF[pb][:], lhsT=dftc[:, xr, :], rhs=wf_sb[:, g, half * 512:(half + 1) * 512], start=True, stop=True),
                                  reads=[b_dftc, b_wf], writes=[b_psF[pb]])
                            kb.op("dve", lambda e: e.tensor_copy(out=Wf[:, xr * 4 + g, half * 512:(half + 1) * 512], in_=psF[pb][:]),
                                  reads=[b_psF[pb]], writes=[b_Wf])
                kb.barrier()
            XTb = [T(es, f"XTb{i}", [128, 8, 512], BF16) for i in range(2)]; b_XTb = [Buf(), Buf()]
            OTb = [T(es, f"OTb{i}", [128, 8, 512], BF16) for i in range(2)]; b_OTb = [Buf(), Buf()]
            gTb = [T(es, f"gTb{i}", [128, 16, 512], BF16) for i in range(2)]; b_gTb = [Buf(), Buf()]
            mT = T(es, "mT", [128, 8, 512], BF16); b_mT = Buf()
            ta = T(es, "ta", [128, 512], F32); tb2 = T(es, "tb2", [128, 512], F32); b_ta = Buf(); b_tb2 = Buf()
            x5 = [T(es, f"x5_{i}", [128, D], F32) for i in range(2)]; b_x5 = [Buf(), Buf()]
            r5 = T(es, "r5", [128, D], F32); b_r5 = Buf()
            st5 = T(es, "st5", [128, 2, 6], F32); mv5 = T(es, "mv5", [128, 2], F32); rstd5 = T(es, "rstd5", [128, 1], F32)
            b_st5 = Buf(); b_mv5 = Buf(); b_rstd5 = Buf()
            y5 = [T(es, f"y5_{i}", [128, D], F32) for i in range(2)]; b_y5 = [Buf(), Buf()]
            s_blk = [kb.sem("p5b0"), kb.sem("p5b1")]; s_x5 = [kb.sem("p5x0"), kb.sem("p5x1")]; s_y5 = [kb.sem("p5y0"), kb.sem("p5y1")]
            XT_v = XT_dram.rearrange("(kc p) s -> p kc s", p=128); OT_v = OT_dram.rearrange("(kc p) s -> p kc s", p=128)
            gT_v5 = gT_dram.rearrange("(g p) s -> p g s", p=128)
            x_v5 = I["x"].rearrange("(t p) d -> t p d", p=128); x1_v = x1_dram.rearrange("(t p) d -> t p d", p=128)

            def load_blk(tb):
                sl = tb % 2
                kb.op("sp", lambda e: e.dma_start(out=XTb[sl][:], in_=XT_v[:, :, tb * 512:(tb + 1) * 512]), writes=[b_XTb[sl]], dsem=s_blk[sl])
                kb.op("sp", lambda e: e.dma_start(out=OTb[sl][:], in_=OT_v[:, :, tb * 512:(tb + 1) * 512]), writes=[b_OTb[sl]], dsem=s_blk[sl])
                kb.op("sp", lambda e: e.dma_start(out=gTb[sl][:], in_=gT_v5[:, :, tb * 512:(tb + 1) * 512]), writes=[b_gTb[sl]], dsem=s_blk[sl])

            mT_ = [mT, T(es, "mT1", [128, 8, 512], BF16)]; b_mT_ = [b_mT, Buf()]
            ta_ = [ta, T(es, "ta1", [128, 512], F32)]; tb2_ = [tb2, T(es, "tb21", [128, 512], F32)]; b_ta_ = [b_ta, Buf()]; b_tb2_ = [b_tb2, Buf()]
            r5_ = [r5, T(es, "r5_1", [128, D], F32)]; b_r5_ = [b_r5, Buf()]
            st5_ = [st5, T(es, "st5_1", [128, 2, 6], F32)]; mv5_ = [mv5, T(es, "mv5_1", [128, 2], F32)]; rstd5_ = [rstd5, T(es, "rstd5_1", [128, 1], F32)]
            b_st5_ = [b_st5, Buf()]; b_mv5_ = [b_mv5, Buf()]; b_rstd5_ = [b_rstd5, Buf()]

            def gate5(tb):
                sl = tb % 2
                if tb < 2:
                    load_blk(tb)
                yield
                for Dc in range(8):
                    pb = Dc % 2
                    kb.group("pe", [lambda e, kc=kc: e.matmul(psF[pb][:], lhsT=Wf[:, kc, Dc * 128:(Dc + 1) * 128], rhs=XTb[sl][:, kc, :],
                                                              start=(kc == 0), stop=(kc == 7)) for kc in range(8)],
                             reads=[b_Wf, b_XTb[sl]], writes=[b_psF[pb]])
                    kb.group("pe", [lambda e, kc=kc: e.matmul(psA[pb][:], lhsT=wao[:, kc, Dc * 128:(Dc + 1) * 128], rhs=OTb[sl][:, kc, :],
                                                              start=(kc == 0), stop=(kc == 7)) for kc in range(8)],
                             reads=[b_wao, b_OTb[sl]], writes=[b_psA[pb]])
                    kb.op("dve", lambda e: e.tensor_tensor(out=ta_[pb][:], in0=psF[pb][:], in1=gTb[sl][:, Dc, :], op=ALU.mult),
                          reads=[b_psF[pb], b_gTb[sl]], writes=[b_ta_[pb]])
                    kb.op("dve", lambda e: e.tensor_tensor(out=tb2_[pb][:], in0=psA[pb][:], in1=gTb[sl][:, 8 + Dc, :], op=ALU.mult),
                          reads=[b_psA[pb], b_gTb[sl]], writes=[b_tb2_[pb]])
                    yield
                    kb.op("dve", lambda e: e.tensor_tensor(out=mT_[sl][:, Dc, :], in0=ta_[pb][:], in1=tb2_[pb][:], op=ALU.add),
                          reads=[b_ta_[pb], b_tb2_[pb]], writes=[b_mT_[sl]])
                    yield
                if tb + 2 < NB:
                    load_blk(tb + 2)

            def tile5(tb, tt):
                sl = tb % 2; t = tb * 4 + tt; xs_ = t % 2
                r5t = r5_[xs_]; b_r5t = b_r5_[xs_]
                kb.op("sp", lambda e: e.dma_start(out=x5[xs_][:], in_=x_v5[t]), writes=[b_x5[xs_]], dsem=s_x5[xs_])
                yield
                for half in range(2):
                    kb.group("pe", [lambda e, Dc=Dc: e.matmul(psH[:, half * 512:(half + 1) * 512], lhsT=mT_[sl][:, Dc, tt * 128:(tt + 1) * 128],
                                                              rhs=wout[:, Dc, half * 512:(half + 1) * 512], start=(Dc == 0), stop=(Dc == 7)) for Dc in range(8)],
                             reads=[b_mT_[sl], b_wout], writes=[b_psH])
                kb.op("dve", lambda e: e.tensor_tensor(out=r5t[:], in0=psH[:], in1=G1, op=ALU.mult), reads=[b_psH, b_mod], writes=[b_r5t])
                yield
                kb.op("dve", lambda e: e.scalar_tensor_tensor(out=r5t[:], in0=x5[xs_][:], scalar=float(ALPHA), in1=r5t[:], op0=ALU.mult, op1=ALU.add),
                      reads=[b_x5[xs_], b_r5t], writes=[b_r5t])
                yield
                yield from ln_tile_g(r5t, b_r5t, y5[xs_], b_y5[xs_], st5_[xs_], b_st5_[xs_], mv5_[xs_], b_mv5_[xs_], rstd5_[xs_], b_rstd5_[xs_],
                                     1e-5, lng[:], lnb[:], b_ln)
                kb.op("sp", lambda e: e.dma_start(out=x1_v[t], in_=y5[xs_][:]), reads=[b_y5[xs_]], dsem=s_y5[xs_])
                yield

            items5 = [("g0", gate5(0), ()), ("g1", gate5(1), ("g0",))]
            for tb in range(NB):
                for tt in range(4):
                    items5.append((f"t{tb}_{tt}", tile5(tb, tt), (f"g{tb}",)))
                if tb + 2 < NB:
                    items5.append((f"g{tb + 2}", gate5(tb + 2), (f"t{tb}_3", f"g{tb + 1}")))
            run_interleaved(items5, 2)
            kb.barrier()


        if stop_after >= 6:
          with ExitStack() as es68:
            w4_all = T(es68, "w4_all", [128, NT, 4], F32); b_w4 = Buf()
            dest_i = T(es68, "dest_i", [128, NT * 4], I32); b_desti = Buf()
            blk_i = T(es68, "blk_i", [128, NBLK], I32); chg_i = T(es68, "chg_i", [128, NBLK], I32); b_blk = Buf()
            idx_w = T(es68, "idx_w", [128, NBLK], I32); idx_b = T(es68, "idx_b", [128, NBLK], I32)
            with ExitStack() as es:
                identF = T(es, "identF", [128, 128], F32); b_identF = Buf()
                kb.op("pool", lambda e: e.memset(identF[:], 0.0), writes=[b_identF])
                kb.op("pool", lambda e: e.affine_select(out=identF[:], in_=identF[:], pattern=[[-1, 128]], compare_op=ALU.not_equal,
                                                        fill=1.0, base=0, channel_multiplier=1), writes=[b_identF])
                ustr = T(es, "ustr", [128, 128], BF16); b_ustr = Buf()
                kb.op("pool", lambda e: e.memset(ustr[:], 1.0), writes=[b_ustr])
                kb.op("pool", lambda e: e.affine_select(out=ustr[:], in_=ustr[:], pattern=[[1, 128]], compare_op=ALU.is_gt,
                                                        fill=0.0, base=0, channel_multiplier=-1), writes=[b_ustr])
                onesb = T(es, "onesb6", [128, 128], BF16); b_onesb = Buf()
                kb.op("pool", lambda e: e.memset(onesb[:], 1.0), writes=[b_onesb])
                ones1f = T(es, "ones1f", [1, 128], F32); b_ones1f = Buf()
                kb.op("pool", lambda e: e.memset(ones1f[:], 1.0), writes=[b_ones1f])
                wr = T(es, "wr", [128, 8, NE], F32); br = T(es, "br", [1, NE], F32); b_wr = Buf()
                s_wr = kb.sem("p6wr")
                kb.op("sp", lambda e: e.dma_start(out=wr[:], in_=I["w_router"].rearrange("(dc p) n -> p dc n", p=128)), writes=[b_wr], dsem=s_wr)
                kb.op("sp", lambda e: e.dma_start(out=br[:], in_=I["b_router"][:, :]), writes=[b_wr], dsem=s_wr)
                x6 = [T(es, f"x6_{i}", [128, D], F32) for i in range(2)]; b_x6 = [Buf(), Buf()]
                u2 = T(es, "u2", [128, D], F32); b_u2 = Buf()
                u2b = [T(es, f"u2b{i}", [128, D], BF16) for i in range(2)]; b_u2b = [Buf(), Buf()]
                u2T = T(es, "u2T", [128, 8, 128], F32); b_u2T = Buf()
                st6 = T(es, "st6", [128, 2, 6], F32); mv6 = T(es, "mv6", [128, 2], F32); rstd6 = T(es, "rstd6", [128, 1], F32)
                b_st6 = Buf(); b_mv6 = Buf(); b_rstd6 = Buf()
                L_all = T(es, "L_all", [128, NT, NE], F32); b_L = Buf()
                top8 = T(es, "top8", [128, 8], F32); b_top8 = Buf()
                top4_all = T(es, "top4_all", [128, NT, 4], F32); b_top4 = Buf()
                negmax = T(es, "negmax", [128, 1], F32); b_negmax = Buf()
                e4 = T(es, "e4", [128, 4], F32); den = T(es, "den", [128, 1], F32); b_e4 = Buf(); b_den = Buf()
                maskb = T(es, "maskb", [128, NE], BF16); b_maskb = Buf()
                pos_all = T(es, "pos_all", [128, NT, NE], F32); b_pos = Buf()
                runcnt = T(es, "runcnt", [128, NE], F32); b_run = Buf()
                kb.op("dve", lambda e: e.memset(runcnt[:], 0.0), writes=[b_run])
                psT6 = P(es, "psT6", [128, 8, 128], F32); b_psT6 = Buf()
                psLg = P(es, "psLg", [128, NE], F32); b_psLg = Buf()
                psPos = P(es, "psPos", [128, NE], F32); b_psPos = Buf()
                psCnt = P(es, "psCnt", [128, NE], F32); b_psCnt = Buf()
                s_x6 = [kb.sem("p6x0"), kb.sem("p6x1")]; s_u6 = [kb.sem("p6u0"), kb.sem("p6u1")]
                x1_v6 = x1_dram.rearrange("(t p) d -> t p d", p=128); u2_v = u2_dram.rearrange("(t p) d -> t p d", p=128)
                u2_ = [u2, T(es, "u2_1", [128, D], F32)]; b_u2_ = [b_u2, Buf()]
                u2T_ = [u2T, T(es, "u2T_1", [128, 8, 128], F32)]; b_u2T_ = [b_u2T, Buf()]
                st6_ = [st6, T(es, "st6_1", [128, 2, 6], F32)]; mv6_ = [mv6, T(es, "mv6_1", [128, 2], F32)]; rstd6_ = [rstd6, T(es, "rstd6_1", [128, 1], F32)]
                b_st6_ = [b_st6, Buf()]; b_mv6_ = [b_mv6, Buf()]; b_rstd6_ = [b_rstd6, Buf()]
                top8_ = [top8, T(es, "top8_1", [128, 8], F32)]; b_top8_ = [b_top8, Buf()]
                negmax_ = [negmax, T(es, "negmax_1", [128, 1], F32)]; b_negmax_ = [b_negmax, Buf()]
                e4_ = [e4, T(es, "e4_1", [128, 4], F32)]; den_ = [den, T(es, "den_1", [128, 1], F32)]; b_e4_ = [b_e4, Buf()]; b_den_ = [b_den, Buf()]
                maskb_ = [maskb, T(es, "maskb_1", [128, NE], BF16)]; b_maskb_ = [b_maskb, Buf()]
                for i6 in range(2):
                    kb.op("sp", lambda e, i6=i6: e.dma_start(out=x6[i6][:], in_=x1_v6[i6]), writes=[b_x6[i6]], dsem=s_x6[i6])

                def route_a(t):
                    sl = t % 2
                    u2c = u2_[sl]; b_u2c = b_u2_[sl]; u2Tc = u2T_[sl]; b_u2Tc = b_u2T_[sl]
                    t8 = top8_[sl]; b_t8 = b_top8_[sl]; nm = negmax_[sl]; b_nm = b_negmax_[sl]
                    e4c = e4_[sl]; b_e4c = b_e4_[sl]; dn = den_[sl]; b_dn = b_den_[sl]; mk = maskb_[sl]; b_mk = b_maskb_[sl]
                    yield from ln_tile_g(x6[sl], b_x6[sl], u2c, b_u2c, st6_[sl], b_st6_[sl], mv6_[sl], b_mv6_[sl], rstd6_[sl], b_rstd6_[sl], 1e-6, SC2, SH2, b_mod)
                    if t + 2 < NT:
                        kb.op("sp", lambda e: e.dma_start(out=x6[sl][:], in_=x1_v6[t + 2]), writes=[b_x6[sl]], dsem=s_x6[sl])
                    kb.op("act", lambda e: e.copy(out=u2b[sl][:], in_=u2c[:]), reads=[b_u2c], writes=[b_u2b[sl]])
                    yield
                    kb.op("sp", lambda e: e.dma_start(out=u2_v[t], in_=u2b[sl][:]), reads=[b_u2b[sl]], dsem=s_u6[sl])
                    yield
                    kb.group("pe", [lambda e, dc=dc: e.transpose(out=psT6[:, dc, :], in_=u2c[:, dc * 128:(dc + 1) * 128], identity=identF[:]) for dc in range(8)],
                             reads=[b_u2c, b_identF], writes=[b_psT6])
                    kb.op("dve", lambda e: e.tensor_copy(out=u2Tc[:], in_=psT6[:]), reads=[b_psT6], writes=[b_u2Tc])
                    yield
                    fns = [lambda e, dc=dc: e.matmul(psLg[:], lhsT=u2Tc[:, dc, :], rhs=wr[:, dc, :], start=(dc == 0), stop=False) for dc in range(8)]
                    fns.append(lambda e: e.matmul(psLg[:], lhsT=ones1f[0:1, :], rhs=br[0:1, :], start=False, stop=True))
                    kb.group("pe", fns, reads=[b_u2Tc, b_wr, b_ones1f], writes=[b_psLg])
                    Lt = L_all[:, t, :]
                    kb.op("dve", lambda e: e.tensor_copy(out=Lt, in_=psLg[:]), reads=[b_psLg], writes=[b_L])
                    yield
                    kb.op("dve", lambda e: e.max(out=t8[:], in_=Lt), reads=[b_L], writes=[b_t8])
                    yield
                    kb.op("dve", lambda e: e.tensor_copy(out=top4_all[:, t, :], in_=t8[:, 0:4]), reads=[b_t8], writes=[b_top4])
                    yield
                    kb.op("dve", lambda e: e.tensor_scalar(out=mk[:], in0=Lt, scalar1=t8[:, 3:4], scalar2=None, op0=ALU.is_ge),
                          reads=[b_L, b_t8], writes=[b_mk])
                    yield
                    kb.op("dve", lambda e: e.tensor_scalar(out=nm[:], in0=t8[:, 0:1], scalar1=-1.0, scalar2=None, op0=ALU.mult),
                          reads=[b_t8], writes=[b_nm])
                    yield
                    kb.op("act", lambda e: e.activation(out=e4c[:], in_=t8[:, 0:4], func=AF.Exp, bias=nm[:, 0:1], scale=1.0, accum_out=dn[:, 0:1]),
                          reads=[b_t8, b_nm], writes=[b_e4c, b_dn])
                    yield
                    kb.op("dve", lambda e: e.reciprocal(out=dn[:], in_=dn[:]), reads=[b_dn], writes=[b_dn])
                    yield
                    kb.op("dve", lambda e: e.tensor_scalar(out=w4_all[:, t, :], in0=e4c[:], scalar1=dn[:, 0:1], scalar2=None, op0=ALU.mult),
                          reads=[b_e4c, b_dn], writes=[b_w4])
                    yield

                def route_b(t):
                    mk = maskb_[t % 2]; b_mk = b_maskb_[t % 2]
                    kb.op("pe", lambda e: e.matmul(psPos[:], lhsT=ustr[:], rhs=mk[:], start=True, stop=True), reads=[b_ustr, b_mk], writes=[b_psPos])
                    kb.op("pe", lambda e: e.matmul(psCnt[:], lhsT=onesb[:], rhs=mk[:], start=True, stop=True), reads=[b_onesb, b_mk], writes=[b_psCnt])
                    kb.op("dve", lambda e: e.tensor_tensor(out=pos_all[:, t, :], in0=psPos[:], in1=runcnt[:], op=ALU.add), reads=[b_psPos, b_run], writes=[b_pos])
                    kb.op("dve", lambda e: e.tensor_tensor(out=runcnt[:], in0=psCnt[:], in1=runcnt[:], op=ALU.add), reads=[b_psCnt, b_run], writes=[b_run])
                    yield

                items6 = []
                for t in range(NT):
                    items6.append((f"a{t}", route_a(t), ((f"b{t - 2}",) if t >= 2 else ())))
                    if t >= 1:
                        items6.append((f"b{t - 1}", route_b(t - 1), (f"a{t - 1}",) + ((f"b{t - 2}",) if t >= 2 else ())))
                items6.append((f"b{NT - 1}", route_b(NT - 1), (f"a{NT - 1}", f"b{NT - 2}")))
                run_interleaved(items6, 2)
                thr_i = T(es, "thr_i", [128, 64], I32); thr = T(es, "thr", [128, 64], F32); b_thr = Buf()
                kb.op("pool", lambda e: e.iota(thr_i[:], pattern=[[BS, 64]], base=0, channel_multiplier=0), writes=[b_thr])
                kb.op("dve", lambda e: e.tensor_copy(out=thr[:], in_=thr_i[:]), reads=[b_thr], writes=[b_thr])
                jf_i = T(es, "jf_i", [128, NBLK], I32); jf = T(es, "jf", [128, NBLK], F32); b_jf = Buf()
                kb.op("pool", lambda e: e.iota(jf_i[:], pattern=[[1, NBLK]], base=0, channel_multiplier=0), writes=[b_jf])
                kb.op("dve", lambda e: e.tensor_copy(out=jf[:], in_=jf_i[:]), reads=[b_jf], writes=[b_jf])
                junk = T(es, "junk", [128, 64], F32); b_junk = Buf()
                nblk = T(es, "nblk", [128, NE], F32); b_nblk = Buf()
                zeros32 = T(es, "zeros32", [128, NE], F32); b_z32 = Buf()
                kb.op("dve", lambda e: e.memset(zeros32[:], 0.0), writes=[b_z32])
                for ex in range(NE):
                    kb.op("dve", lambda e, ex=ex: e.tensor_scalar(out=junk[:], in0=thr[:], scalar1=runcnt[:, ex:ex + 1], scalar2=0.0, op0=ALU.is_lt, op1=ALU.add,
                                                                  accum_out=nblk[:, ex:ex + 1]), reads=[b_thr, b_run], writes=[b_junk, b_nblk])
                pend = T(es, "pend", [128, NE], F32); b_pend = Buf()
                kb.op("dve", lambda e: e.tensor_tensor_scan(out=pend[:], data0=nblk[:], data1=zeros32[:], initial=0.0, op0=ALU.add, op1=ALU.add),
                      reads=[b_nblk, b_z32], writes=[b_pend])
                pstart = T(es, "pstart", [128, NE], F32); b_pstart = Buf()
                kb.op("dve", lambda e: e.tensor_tensor(out=pstart[:], in0=pend[:], in1=nblk[:], op=ALU.subtract), reads=[b_pend, b_nblk], writes=[b_pstart])
                kb.op("dve", lambda e: e.tensor_scalar(out=pstart[:], in0=pstart[:], scalar1=float(BS), scalar2=None, op0=ALU.mult), reads=[b_pstart], writes=[b_pstart])
                acc = T(es, "acc6", [128, NBLK], F32); b_acc = Buf()
                chg = T(es, "chg6", [128, NBLK], F32); b_chg = Buf()
                kb.op("dve", lambda e: e.memset(acc[:], 0.0), writes=[b_acc])
                for ex in range(NE - 1):
                    kb.op("dve", lambda e, ex=ex: e.scalar_tensor_tensor(out=acc[:], in0=jf[:], scalar=pend[:, ex:ex + 1], in1=acc[:], op0=ALU.is_ge, op1=ALU.add),
                          reads=[b_jf, b_pend, b_acc], writes=[b_acc])
                kb.op("dve", lambda e: e.memset(chg[:], 1.0), writes=[b_chg])
                kb.op("dve", lambda e: e.tensor_tensor(out=chg[:, 2:NBLK], in0=acc[:, 2:NBLK], in1=acc[:, 0:NBLK - 2], op=ALU.not_equal), reads=[b_acc, b_chg], writes=[b_chg])
                kb.op("dve", lambda e: e.tensor_copy(out=blk_i[:], in_=acc[:]), reads=[b_acc], writes=[b_blk])
                kb.op("dve", lambda e: e.tensor_copy(out=chg_i[:], in_=chg[:]), reads=[b_chg], writes=[b_blk])
                BIG = float(1 << 20)
                pio_i = T(es, "pio_i", [128, 1], I32); pio = T(es, "pio", [128, 1], F32); b_pio = Buf()
                kb.op("pool", lambda e: e.iota(pio_i[:], pattern=[[0, 1]], base=0, channel_multiplier=1), writes=[b_pio])
                kb.op("dve", lambda e: e.tensor_copy(out=pio[:], in_=pio_i[:]), reads=[b_pio], writes=[b_pio])
                idxf = T(es, "idxf", [128, NBLK], F32); b_idxf = Buf()
                kb.op("dve", lambda e: e.tensor_scalar(out=idxf[:], in0=acc[:], scalar1=128.0, scalar2=None, op0=ALU.mult), reads=[b_acc], writes=[b_idxf])
                kb.op("dve", lambda e: e.tensor_scalar(out=idxf[:], in0=idxf[:], scalar1=pio[:, 0:1], scalar2=None, op0=ALU.add), reads=[b_idxf, b_pio], writes=[b_idxf])
                kb.op("dve", lambda e: e.tensor_copy(out=idx_w[:], in_=idxf[:]), reads=[b_idxf], writes=[b_blk])
                kb.op("dve", lambda e: e.tensor_copy(out=idx_b[:], in_=acc[:]), reads=[b_acc], writes=[b_blk])
                destf = T(es, "destf", [128, NT * 4], F32); b_destf = Buf()
                A6 = T(es, "A6", [128, NE], F32); b_A6 = Buf()
                junk2 = T(es, "junk2", [128, NE], F32); b_junk2 = Buf()
                s_sc = [kb.sem("p6s0"), kb.sem("p6s1")]; s_ul = [kb.sem("p6l0"), kb.sem("p6l1")]
                A6_ = [A6, T(es, "A6_1", [128, NE], F32)]; b_A6_ = [b_A6, Buf()]
                b_dt = [Buf() for _ in range(NT)]
                for t in range(NT):
                    sl = t % 2
                    kb.op("dve", lambda e: e.tensor_tensor(out=A6_[sl][:], in0=pos_all[:, t, :], in1=pstart[:], op=ALU.add), reads=[b_pos, b_pstart], writes=[b_A6_[sl]])
                    for j in range(4):
                        kb.op("dve", lambda e, j=j: e.scalar_tensor_tensor(out=junk2[:], in0=L_all[:, t, :], scalar=top4_all[:, t, j:j + 1], in1=A6_[sl][:],
                                                                           op0=ALU.is_equal, op1=ALU.mult, accum_out=destf[:, t * 4 + j:t * 4 + j + 1]),
                              reads=[b_L, b_top4, b_A6_[sl]], writes=[b_junk2, b_destf])
                    kb.op("dve", lambda e: e.tensor_copy(out=dest_i[:, t * 4:(t + 1) * 4], in_=destf[:, t * 4:(t + 1) * 4]), reads=[b_destf], writes=[b_dt[t]])
                    kb.op("sp", lambda e: e.dma_start(out=u2b[sl][:], in_=u2_v[t]), writes=[b_u2b[sl]], dsem=s_ul[sl])
                    for j in range(4):
                        kb.op("pool", lambda e, j=j: e.indirect_dma_start(out=xs_dram[:, :], out_offset=bass.IndirectOffsetOnAxis(ap=dest_i[:, t * 4 + j:t * 4 + j + 1], axis=0),
                                                                          in_=u2b[sl][:], in_offset=None), reads=[b_u2b[sl], b_dt[t]], dsem=s_sc[sl])
                if dbg6 is not None:
                    s_d6 = kb.sem("p6dbg")
                    kb.op("sp", lambda e: e.dma_start(out=dbg6["L"].rearrange("(t p) n -> p t n", p=128), in_=L_all[:]), reads=[b_L], dsem=s_d6)
                    kb.op("sp", lambda e: e.dma_start(out=dbg6["dest"][:, :], in_=dest_i[:]), reads=b_dt, dsem=s_d6)
                    kb.op("sp", lambda e: e.dma_start(out=dbg6["blk"][:, :], in_=blk_i[0:1, :]), reads=[b_blk], dsem=s_d6)
                    kb.op("sp", lambda e: e.dma_start(out=dbg6["chg"][:, :], in_=chg_i[0:1, :]), reads=[b_blk], dsem=s_d6)
                    kb.op("sp", lambda e: e.dma_start(out=dbg6["w4"][:, :], in_=w4_all[:].rearrange("p t j -> p (t j)")), reads=[b_w4], dsem=s_d6)
                kb.barrier()

            if stop_after >= 7:
              with ExitStack() as es:
                wgu = [T(es, f"wgu{i}", [128, 8 * 2 * D], BF16) for i in range(2)]
                wd = [T(es, f"wd{i}", [128, 8 * D], BF16) for i in range(2)]
                bgf = [T(es, f"bgf{i}", [128, 16], F32) for i in range(2)]
                bd = [T(es, f"bd{i}", [2, D], BF16) for i in range(2)]
                b_w7 = [Buf(), Buf()]
                ones7 = T(es, "ones7", [1, 128], BF16); b_ones7 = Buf()
                kb.op("pool", lambda e: e.memset(ones7[:], 1.0), writes=[b_ones7])
                xsb = [T(es, f"xsb{i}", [128, NSUB, D], BF16) for i in range(2)]; b_xsb = [Buf(), Buf()]
                xsT = [T(es, f"xsT{i}", [128, 8, BS], BF16) for i in range(2)]; b_xsT = [Buf(), Buf()]
                actT = [T(es, f"actT{i}", [128, 8, BS], BF16) for i in range(2)]; b_actT = [Buf(), Buf()]
                g7 = [T(es, f"g7_{i}", [128, BS], F32) for i in range(2)]; b_g7 = [Buf(), Buf()]
                sg = [T(es, f"sg_{i}", [128, BS], F32) for i in range(2)]; b_sg = [Buf(), Buf()]
                u1 = [T(es, f"u1_{i}", [128, BS], F32) for i in range(2)]; b_u1 = [Buf(), Buf()]
                a1 = [T(es, f"a1_{i}", [128, BS], F32) for i in range(2)]; b_a1 = [Buf(), Buf()]
                ysb = [T(es, f"ysb{i}", [128, 512], BF16) for i in range(3)]; b_ysb = [Buf() for _ in range(3)]
                pstx = P(es, "pstx", [128, 8, 128], BF16); b_pstx = Buf()
                psG = [P(es, f"psG{i}", [128, BS], F32) for i in range(2)]; b_psG = [Buf(), Buf()]
                psU = [P(es, f"psU{i}", [128, BS], F32) for i in range(2)]; b_psU = [Buf(), Buf()]
                psYb = [P(es, f"psYb{i}", [128, 512], F32) for i in range(3)]; b_psYb = [Buf() for _ in range(3)]
                s_w7 = [kb.sem("p7w0"), kb.sem("p7w1")]; s_xs = [kb.sem("p7x0"), kb.sem("p7x1")]; s_ys = [kb.sem(f"p7y{i}") for i in range(3)]
                nblk7 = debug.get("p7_blocks", NBLK)
                xs_v = xs_dram.rearrange("(j t p) d -> j p t d", t=NSUB, p=128)

                def load_w(j):
                    sl = j % 2
                    for dst, srcT in ((wgu[sl][:, :], wgu_bf), (wd[sl][:, :], wd_bf), (bgf[sl][:, :], I["bgu_fm"])):
                        kb.op("pool", lambda e, dst=dst, srcT=srcT: e.indirect_dma_start(out=dst, out_offset=None, in_=srcT[:, :],
                                                                                        in_offset=bass.IndirectOffsetOnAxis(ap=idx_w[:, j:j + 1], axis=0)),
                              reads=[b_blk], writes=[b_w7[sl]], dsem=s_w7[sl])
                    kb.op("pool", lambda e: e.indirect_dma_start(out=bd[sl][0:2, :], out_offset=None, in_=bd_bf[:, :],
                                                                 in_offset=bass.IndirectOffsetOnAxis(ap=idx_b[0:2, j:j + 1], axis=0)),
                          reads=[b_blk], writes=[b_w7[sl]], dsem=s_w7[sl])
                    kb.op("sp", lambda e: e.dma_start(out=xsb[sl][:], in_=xs_v[j]), writes=[b_xsb[sl]], dsem=s_xs[sl])

                def emit_tx(jj):
                    s2 = jj % 2
                    for st in range(NSUB):
                        kb.group("pe", [lambda e, dc=dc: e.transpose(out=pstx[:, dc, :], in_=xsb[s2][:, st, dc * 128:(dc + 1) * 128], identity=ident[:]) for dc in range(8)],
                                 reads=[b_xsb[s2], b_ident], writes=[b_pstx])
                        kb.op("act", lambda e: e.copy(out=xsT[s2][:, :, st * 128:(st + 1) * 128], in_=pstx[:]), reads=[b_pstx], writes=[b_xsT[s2]])

                load_w(0)
                yk = 0
                for j in range(nblk7):
                    sl = j % 2
                    if j + 1 < nblk7:
                        load_w(j + 1)
                    if j == 0:
                        emit_tx(0)
                    for fc in range(8):
                        pb = fc % 2
                        kb.group("pe", [lambda e, r=r: e.matmul(psG[pb][:], lhsT=wgu[sl][:, r * 2048 + fc * 128:r * 2048 + (fc + 1) * 128], rhs=xsT[sl][:, r, :],
                                                                start=(r == 0), stop=(r == 7)) for r in range(8)],
                                 reads=[b_xsT[sl], b_w7[sl]], writes=[b_psG[pb]])
                        kb.group("pe", [lambda e, r=r: e.matmul(psU[pb][:], lhsT=wgu[sl][:, r * 2048 + 1024 + fc * 128:r * 2048 + 1024 + (fc + 1) * 128], rhs=xsT[sl][:, r, :],
                                                                start=(r == 0), stop=(r == 7)) for r in range(8)],
                                 reads=[b_xsT[sl], b_w7[sl]], writes=[b_psU[pb]])
                        kb.op("dve", lambda e: e.tensor_scalar(out=g7[pb][:], in0=psG[pb][:], scalar1=bgf[sl][:, fc:fc + 1], scalar2=7.0, op0=ALU.add, op1=ALU.min),
                              reads=[b_psG[pb], b_w7[sl]], writes=[b_g7[pb]])
                        kb.op("act", lambda e: e.activation(out=sg[pb][:], in_=g7[pb][:], func=AF.Sigmoid, scale=1.702), reads=[b_g7[pb]], writes=[b_sg[pb]])
                        kb.op("dve", lambda e: e.tensor_scalar(out=u1[pb][:], in0=psU[pb][:], scalar1=bgf[sl][:, 8 + fc:9 + fc], scalar2=7.0, op0=ALU.add, op1=ALU.min),
                              reads=[b_psU[pb], b_w7[sl]], writes=[b_u1[pb]])
                        kb.op("dve", lambda e: e.tensor_scalar(out=u1[pb][:], in0=u1[pb][:], scalar1=-7.0, scalar2=1.0, op0=ALU.max, op1=ALU.add),
                              reads=[b_u1[pb]], writes=[b_u1[pb]])
                        kb.op("dve", lambda e: e.tensor_tensor(out=a1[pb][:], in0=u1[pb][:], in1=g7[pb][:], op=ALU.mult), reads=[b_u1[pb], b_g7[pb]], writes=[b_a1[pb]])
                        kb.op("dve", lambda e: e.tensor_tensor(out=actT[sl][:, fc, :], in0=a1[pb][:], in1=sg[pb][:], op=ALU.mult), reads=[b_a1[pb], b_sg[pb]], writes=[b_actT[sl]])
                    if j + 1 < nblk7:
                        emit_tx(j + 1)
                    for st in range(NSUB):
                        r0 = j * BS + st * 128
                        for half in range(2):
                            yb = yk % 3; yk += 1
                            fns = [lambda e, fc=fc: e.matmul(psYb[yb][:], lhsT=actT[sl][:, fc, st * 128:(st + 1) * 128],
                                                             rhs=wd[sl][:, fc * 1024 + half * 512:fc * 1024 + (half + 1) * 512], start=(fc == 0), stop=False) for fc in range(8)]
                            fns.append(lambda e: e.matmul(psYb[yb][:], lhsT=ones7[0:1, :], rhs=bd[sl][0:1, half * 512:(half + 1) * 512], start=False, stop=True))
                            kb.group("pe", fns, reads=[b_actT[sl], b_w7[sl], b_ones7], writes=[b_psYb[yb]])
                            kb.op("act", lambda e: e.copy(out=ysb[yb][:], in_=psYb[yb][:]), reads=[b_psYb[yb]], writes=[b_ysb[yb]])
                            kb.op("act", lambda e: e.dma_start(out=ys_dram[r0:r0 + 128, half * 512:(half + 1) * 512], in_=ysb[yb][:]), reads=[b_ysb[yb]], dsem=s_ys[yb])
                kb.barrier()

            if stop_after >= 8:
              with ExitStack() as es:
                lng2 = T(es, "lng2", [128, D], F32); lnb2 = T(es, "lnb2", [128, D], F32); b_ln2 = Buf()
                s_ln2 = kb.sem("p8ln")
                kb.op("sp", lambda e: e.dma_start(out=lng2[:], in_=I["ln2_g"][0:1, :].partition_broadcast(128)), writes=[b_ln2], dsem=s_ln2)
                kb.op("sp", lambda e: e.dma_start(out=lnb2[:], in_=I["ln2_b"][0:1, :].partition_broadcast(128)), writes=[b_ln2], dsem=s_ln2)
                yg = [[T(es, f"yg{i}_{j}", [128, D], BF16) for j in range(4)] for i in range(2)]; b_yg = [[Buf() for _ in range(4)] for _ in range(2)]
                x8 = [T(es, f"x8_{i}", [128, D], F32) for i in range(2)]; b_x8 = [Buf(), Buf()]
                h8 = T(es, "h8", [128, D], F32); b_h8 = Buf()
                o8 = [T(es, f"o8_{i}", [128, D], F32) for i in range(2)]; b_o8 = [Buf(), Buf()]
                st8 = T(es, "st8", [128, 2, 6], F32); mv8 = T(es, "mv8", [128, 2], F32); rstd8 = T(es, "rstd8", [128, 1], F32)
                b_st8 = Buf(); b_mv8 = Buf(); b_rstd8 = Buf()
                s_g8 = [kb.sem("p8g0"), kb.sem("p8g1")]; s_x8 = [kb.sem("p8x0"), kb.sem("p8x1")]; s_o8 = [kb.sem("p8o0"), kb.sem("p8o1")]
                x1_v8 = x1_dram.rearrange("(t p) d -> t p d", p=128); out_v = out.rearrange("(t p) d -> t p d", p=128)

                def load8(t):
                    sl = t % 2
                    kb.op("sp", lambda e: e.dma_start(out=x8[sl][:], in_=x1_v8[t]), writes=[b_x8[sl]], dsem=s_x8[sl])
                    for j in range(4):
                        kb.op("pool", lambda e, j=j: e.indirect_dma_start(out=yg[sl][j][:], out_offset=None, in_=ys_dram[:, :],
                                                                          in_offset=bass.IndirectOffsetOnAxis(ap=dest_i[:, t * 4 + j:t * 4 + j + 1], axis=0)),
                              reads=[b_desti], writes=[b_yg[sl][j]], dsem=s_g8[sl])
                h8_ = [h8, T(es, "h8_1", [128, D], F32)]; b_h8_ = [b_h8, Buf()]
                st8_ = [st8, T(es, "st8_1", [128, 2, 6], F32)]; mv8_ = [mv8, T(es, "mv8_1", [128, 2], F32)]; rstd8_ = [rstd8, T(es, "rstd8_1", [128, 1], F32)]
                b_st8_ = [b_st8, Buf()]; b_mv8_ = [b_mv8, Buf()]; b_rstd8_ = [b_rstd8, Buf()]
                load8(0); load8(1)

                def tile8(t):
                    sl = t % 2
                    hh = h8_[sl]; b_hh = b_h8_[sl]
                    kb.op("dve", lambda e: e.tensor_scalar(out=hh[:], in0=yg[sl][0][:], scalar1=w4_all[:, t, 0:1], scalar2=None, op0=ALU.mult),
                          reads=[b_yg[sl][0], b_w4], writes=[b_hh])
                    yield
                    for j in range(1, 4):
                        kb.op("dve", lambda e, j=j: e.scalar_tensor_tensor(out=hh[:], in0=yg[sl][j][:], scalar=w4_all[:, t, j:j + 1], in1=hh[:], op0=ALU.mult, op1=ALU.add),
                              reads=[b_yg[sl][j], b_w4, b_hh], writes=[b_hh])
                        yield
                    kb.op("dve", lambda e: e.tensor_tensor(out=hh[:], in0=hh[:], in1=G2, op=ALU.mult), reads=[b_hh, b_mod], writes=[b_hh])
                    yield
                    kb.op("dve", lambda e: e.scalar_tensor_tensor(out=hh[:], in0=x8[sl][:], scalar=float(ALPHA), in1=hh[:], op0=ALU.mult, op1=ALU.add),
                          reads=[b_x8[sl], b_hh], writes=[b_hh])
                    if t + 2 < NT:
                        load8(t + 2)
                    yield
                    yield from ln_tile_g(hh, b_hh, o8[sl], b_o8[sl], st8_[sl], b_st8_[sl], mv8_[sl], b_mv8_[sl], rstd8_[sl], b_rstd8_[sl],
                                         1e-5, lng2[:], lnb2[:], b_ln2)
                    kb.op("sp", lambda e: e.dma_start(out=out_v[t], in_=o8[sl][:]), reads=[b_o8[sl]], dsem=s_o8[sl])
                    yield

                run_interleaved((tile8(t) for t in range(NT)), 2)
                kb.barrier()

        kb.barrier()
    return nc, dbg_out, I


def _tables():
    rows = S // 64
    row_ids = np.repeat(np.arange(rows, dtype=np.float32), 64)
    col_ids = np.tile(np.arange(64, dtype=np.float32), rows)
    freqs = (np.float32(10000.0) ** (-np.arange(0, 64, 2, dtype=np.float32) / np.float32(64))).astype(np.float32)
    ang_r = (row_ids[:, None] * freqs).astype(np.float32)
    ang_c = (col_ids[:, None] * freqs).astype(np.float32)
    cr, sr, cc, sc = np.cos(ang_r), np.sin(ang_r), np.cos(ang_c), np.sin(ang_c)
    rope = np.concatenate([cr, cr, cc, cc, -sr, sr, -sc, sc], axis=1).astype(np.float32)
    n1 = np.arange(128)[:, None, None]; n2 = np.arange(64)[None, :, None]; k1 = np.arange(128)[None, None, :]
    ang = 2.0 * np.pi * ((k1 * (64 * n1 + n2)) % 8192) / 8192.0
    fft_e = np.stack([np.cos(ang), -np.sin(ang)], axis=2)
    n2v = np.arange(64)[:, None]; k2 = np.arange(64)[None, :]
    th = 2.0 * np.pi * ((n2v * k2) % 64) / 64.0
    sc_ = 1.0 / np.sqrt(8192.0)
    w3 = np.zeros((128, 128))
    w3[0:64, 0:64] = np.cos(th); w3[64:128, 0:64] = np.sin(th)
    w3[0:64, 64:128] = -np.sin(th); w3[64:128, 64:128] = np.cos(th)
    w3 *= sc_
    cch = np.arange(128)[:, None] * np.arange(128)[None, :]
    thc = 2.0 * np.pi * (cch % 128) / 128.0
    dft_c = np.stack([np.cos(thc), np.sin(thc)], axis=1) / np.sqrt(128.0)
    bf = ml_dtypes.bfloat16
    return {"rope_tab": rope, "fft_e": fft_e.astype(np.float32).astype(bf), "fft_w3": w3.astype(np.float32).astype(bf),
            "dft_c": dft_c.astype(np.float32).astype(bf)}


def make_in_maps(inputs):
    tabs = _tables()
    shared = {}
    for k in ("w_ada", "w_in", "w_fourier", "w_attn_o", "w_out", "w_router", "w_gate_up", "w_down"):
        shared[k] = np.ascontiguousarray(np.asarray(inputs[k], dtype=np.float32)[0])
    for k in ("b_ada", "q_norm_g", "k_norm_g", "ln1_g", "ln1_b", "b_router", "ln2_g", "ln2_b"):
        shared[k] = np.ascontiguousarray(np.asarray(inputs[k], dtype=np.float32)[0][None, :])
    shared["b_down"] = np.ascontiguousarray(np.asarray(inputs["b_down"], dtype=np.float32)[0])
    shared["bgu_fm"] = np.ascontiguousarray(np.asarray(inputs["b_gate_up"], dtype=np.float32)[0].reshape(NE, 16, 128).transpose(0, 2, 1).reshape(NE * 128, 16))
    shared.update(tabs)
    x = np.asarray(inputs["x"], dtype=np.float32)
    c = np.asarray(inputs["c"], dtype=np.float32)
    maps = []
    for b in range(8):
        m = dict(shared)
        m["x"] = np.ascontiguousarray(x[b])
        m["c_fm"] = np.ascontiguousarray(c[b].reshape(8, 128).T)
        maps.append(m)
    return maps


def kernel(**inputs):
    nc, _, _ = build()
    maps = make_in_maps(inputs)
    res = run_bass_kernel_spmd(nc, maps, core_ids=list(range(8)))
    return np.stack([np.asarray(r["out"], dtype=np.float32) for r in res.results], axis=0)
```
